# Optimizing a Trainium2 kernel written in Bass

```python
import math
import jax
import jax.numpy as jnp
from jax import lax
import numpy as np

D_MODEL = 1024
BATCH = 4
SEQ = 4096
DEPTH = 1

N_META = 16
GLA_HEADS = 4
GLA_DK = 128
GLA_DV = 256
GLA_QK = GLA_HEADS * GLA_DK
GLA_VW = GLA_HEADS * GLA_DV
GLA_GATE_RANK = 16
GLA_GATE_TAU = 16.0
GLA_CHUNK = 64
MLA_HEADS = 16
MLA_Q_RANK = 384
MLA_KV_RANK = 256
MLA_NOPE = 64
MLA_ROPE = 32
MLA_DV = 64
MLA_QDIM = MLA_NOPE + MLA_ROPE
MLA_VW = MLA_HEADS * MLA_DV
ROPE_BASE = 10000.0
Q_BLOCK = 128
N_GROUPS = 4
EXPERTS_PER_GROUP = 8
EXPERT_FF = 256
TOP_K = 2
ALPHA = (2.0 * DEPTH) ** 0.25
BETA = (8.0 * DEPTH) ** -0.25
LN_EPS = 1e-5
RMS_EPS = 1e-6
IN_SIZES = (GLA_QK, GLA_QK, GLA_VW, GLA_VW, GLA_GATE_RANK, MLA_Q_RANK, MLA_KV_RANK, MLA_ROPE, D_MODEL, D_MODEL)
IN_WIDTH = sum(IN_SIZES)
IN_OFFSETS = tuple(int(o) for o in np.cumsum(IN_SIZES)[:-1])

kernel_name = "hybrid_gla_mla_hmoe_deepnorm_meta"


def layer_norm(x, g, b):
    xf = x.astype(jnp.float32)
    mu = jnp.mean(xf, axis=-1, keepdims=True)
    var = jnp.mean(jnp.square(xf - mu), axis=-1, keepdims=True)
    return ((xf - mu) * lax.rsqrt(var + LN_EPS) * g.astype(jnp.float32) + b.astype(jnp.float32)).astype(x.dtype)


def rms_norm(x, g):
    xf = x.astype(jnp.float32)
    ms = jnp.mean(jnp.square(xf), axis=-1, keepdims=True)
    return (xf * lax.rsqrt(ms + RMS_EPS) * g.astype(jnp.float32)).astype(x.dtype)


def apply_rope(x, cos, sin):
    half = x.shape[-1] // 2
    x1, x2 = x[..., :half], x[..., half:]
    cos = cos.astype(x.dtype)
    sin = sin.astype(x.dtype)
    return jnp.concatenate([x1 * cos - x2 * sin, x1 * sin + x2 * cos], axis=-1)


def gla_mixer(q, k, v, log_a, r, norm_g):
    B, L, _ = q.shape
    pad = GLA_CHUNK - N_META
    padw = ((0, 0), (pad, 0), (0, 0))
    q, k, v, log_a = (jnp.pad(t, padw) for t in (q, k, v, log_a))
    Lp = L + pad
    n = Lp // GLA_CHUNK

    def to_chunks(t, d):
        return t.reshape(B, n, GLA_CHUNK, GLA_HEADS, d).transpose(0, 3, 1, 2, 4)

    qc = to_chunks(q, GLA_DK).astype(jnp.float32) * (GLA_DK ** -0.5)
    kc = to_chunks(k, GLA_DK).astype(jnp.float32)
    vc = to_chunks(v, GLA_DV).astype(jnp.float32)
    bc = jnp.cumsum(to_chunks(log_a, GLA_DK).astype(jnp.float32), axis=3)
    b_last = bc[:, :, :, -1:, :]
    q_t = qc * jnp.exp(bc)
    k_t = kc * jnp.exp(-bc)
    k_end = kc * jnp.exp(b_last - bc)
    causal = jnp.tril(jnp.ones((GLA_CHUNK, GLA_CHUNK), dtype=bool))
    att = jnp.einsum('bhncd,bhnsd->bhncs', q_t, k_t)
    att = jnp.where(causal, att, 0.0)
    o_intra = jnp.einsum('bhncs,bhnsv->bhncv', att, vc)
    upd = jnp.einsum('bhncd,bhncv->nbhdv', k_end, vc)
    dec = jnp.exp(b_last[:, :, :, 0, :]).transpose(2, 0, 1, 3)

    def step(state, inp):
        u, d = inp
        return d[..., None] * state + u, state

    s0 = jnp.zeros((B, GLA_HEADS, GLA_DK, GLA_DV), dtype=upd.dtype)
    _, s_before = lax.scan(step, s0, (upd, dec))
    o_inter = jnp.einsum('bhncd,nbhdv->bhncv', q_t, s_before)
    o = (o_intra + o_inter).transpose(0, 2, 3, 1, 4).reshape(B, Lp, GLA_HEADS, GLA_DV)[:, pad:]
    o = rms_norm(o, norm_g) * jax.nn.silu(r.reshape(B, L, GLA_HEADS, GLA_DV).astype(jnp.float32))
    return o.reshape(B, L, GLA_VW).astype(r.dtype)


def mla_mixer(c_q, c_kv, k_r, q_norm_g, w_uq, kv_norm_g, w_uk, w_uv, cos, sin):
    B, L, _ = c_q.shape
    q = (rms_norm(c_q, q_norm_g) @ w_uq).reshape(B, L, MLA_HEADS, MLA_QDIM)
    q_nope = q[..., :MLA_NOPE]
    q_rope = apply_rope(q[..., MLA_NOPE:], cos[:, None, :], sin[:, None, :])
    ckv = rms_norm(c_kv, kv_norm_g)
    k_nope = (ckv @ w_uk).reshape(B, L, MLA_HEADS, MLA_NOPE)
    v = (ckv @ w_uv).reshape(B, L, MLA_HEADS, MLA_DV)
    k_rope = apply_rope(k_r, cos, sin)
    nb = -(-L // Q_BLOCK)
    Lq = nb * Q_BLOCK
    padw = ((0, 0), (0, Lq - L), (0, 0), (0, 0))

    def to_blocks(t):
        return jnp.pad(t, padw).reshape(B, nb, Q_BLOCK, MLA_HEADS, t.shape[-1]).transpose(1, 0, 2, 3, 4)

    qn_b = to_blocks(q_nope)
    qr_b = to_blocks(q_rope)
    qpos_b = jnp.arange(Lq, dtype=jnp.int32).reshape(nb, Q_BLOCK)
    kpos = jnp.arange(L, dtype=jnp.int32)
    scale = MLA_QDIM ** -0.5

    def attend(args):
        qn, qr, qpos = args
        s = jnp.einsum('bqhd,bkhd->bhqk', qn, k_nope) + jnp.einsum('bqhd,bkd->bhqk', qr, k_rope)
        s = s.astype(jnp.float32) * scale
        s = jnp.where(qpos[:, None] >= kpos[None, :], s, jnp.finfo(jnp.float32).min)
        p = jax.nn.softmax(s, axis=-1).astype(v.dtype)
        return jnp.einsum('bhqk,bkhd->bqhd', p, v)

    o = lax.map(attend, (qn_b, qr_b, qpos_b))
    o = o.transpose(1, 0, 2, 3, 4).reshape(B, Lq, MLA_HEADS, MLA_DV)[:, :L]
    return o.reshape(B, L, MLA_VW)


def moe_ffn(h, w_rg, b_rg, w_re, b_re, w_gate, w_up, w_down):
    B, L, D = h.shape
    t = h.reshape(B * L, D)
    p_group = jax.nn.softmax((t @ w_rg + b_rg).astype(jnp.float32), axis=-1)
    g_idx = jnp.argmax(p_group, axis=-1)
    p_g = jnp.max(p_group, axis=-1)
    e_logits = (t @ w_re + b_re).astype(jnp.float32).reshape(-1, N_GROUPS, EXPERTS_PER_GROUP)
    e_sel = jnp.take_along_axis(e_logits, g_idx[:, None, None], axis=1)[:, 0]
    top_v, top_i = lax.top_k(e_sel, TOP_K)
    top_p = jax.nn.softmax(top_v, axis=-1)
    w_e = jnp.einsum('nk,nke->ne', top_p, jax.nn.one_hot(top_i, EXPERTS_PER_GROUP, dtype=jnp.float32))
    combine = (p_g[:, None, None] * jax.nn.one_hot(g_idx, N_GROUPS, dtype=jnp.float32)[:, :, None]
               * w_e[:, None, :]).astype(h.dtype)
    y = jnp.zeros_like(t)
    for grp in range(N_GROUPS):
        a = jnp.einsum('nd,edf->nef', t, w_gate[grp])
        u = jnp.einsum('nd,edf->nef', t, w_up[grp])
        hid = jax.nn.silu(a) * u * combine[:, grp, :, None]
        y = y + jnp.einsum('nef,efd->nd', hid, w_down[grp])
    return y.reshape(B, L, D)


def setup_inputs(seed: int = 0) -> dict:
    key = jax.random.key(seed)
    ks = jax.random.split(key, 32)
    f32 = jnp.float32
    nrm = lambda k, shape, s: jax.random.normal(k, shape, dtype=f32) * s
    gain = lambda k, shape: 1.0 + 0.02 * jax.random.normal(k, shape, dtype=f32)
    G, E, F, D = N_GROUPS, EXPERTS_PER_GROUP, EXPERT_FF, D_MODEL
    col_scale = jnp.ones((IN_WIDTH,), f32).at[2 * GLA_QK:2 * GLA_QK + GLA_VW].set(BETA)
    return {
        "x": nrm(ks[0], (BATCH, SEQ, D), 1.0),
        "meta_tokens": nrm(ks[1], (N_META, D), 1.0),
        "ln_emb_g": gain(ks[2], (D,)),
        "ln_emb_b": nrm(ks[3], (D,), 0.02),
        "w_in": nrm(ks[4], (DEPTH, D, IN_WIDTH), D ** -0.5) * col_scale,
        "gla_gate_w2": nrm(ks[5], (DEPTH, GLA_GATE_RANK, GLA_QK), GLA_GATE_RANK ** -0.5),
        "gla_gate_b": nrm(ks[6], (DEPTH, GLA_QK), 0.02),
        "gla_norm_g": gain(ks[7], (DEPTH, GLA_DV)),
        "mla_q_norm_g": gain(ks[8], (DEPTH, MLA_Q_RANK)),
        "mla_w_uq": nrm(ks[9], (DEPTH, MLA_Q_RANK, MLA_HEADS * MLA_QDIM), MLA_Q_RANK ** -0.5),
        "mla_kv_norm_g": gain(ks[10], (DEPTH, MLA_KV_RANK)),
        "mla_w_uk": nrm(ks[11], (DEPTH, MLA_KV_RANK, MLA_HEADS * MLA_NOPE), MLA_KV_RANK ** -0.5),
        "mla_w_uv": nrm(ks[12], (DEPTH, MLA_KV_RANK, MLA_VW), MLA_KV_RANK ** -0.5 * BETA),
        "w_branch_gla": nrm(ks[13], (DEPTH, GLA_VW, D), GLA_VW ** -0.5 * BETA),
        "w_branch_mla": nrm(ks[14], (DEPTH, MLA_VW, D), MLA_VW ** -0.5 * BETA),
        "w_out": nrm(ks[15], (DEPTH, D, D), D ** -0.5 * BETA),
        "ln_mix_g": gain(ks[16], (DEPTH, D)),
        "ln_mix_b": nrm(ks[17], (DEPTH, D), 0.02),
        "router_group_w": nrm(ks[18], (DEPTH, D, G), D ** -0.5),
        "router_group_b": nrm(ks[19], (DEPTH, G), 0.01),
        "router_expert_w": nrm(ks[20], (DEPTH, D, G * E), D ** -0.5),
        "router_expert_b": nrm(ks[21], (DEPTH, G * E), 0.01),
        "expert_w_gate": nrm(ks[22], (DEPTH, G, E, D, F), D ** -0.5),
        "expert_w_up": nrm(ks[23], (DEPTH, G, E, D, F), D ** -0.5 * BETA),
        "expert_w_down": nrm(ks[24], (DEPTH, G, E, F, D), F ** -0.5 * BETA),
        "ln_ffn_g": gain(ks[25], (DEPTH, D)),
        "ln_ffn_b": nrm(ks[26], (DEPTH, D), 0.02),
    }


def reference(x, meta_tokens, ln_emb_g, ln_emb_b, w_in, gla_gate_w2, gla_gate_b, gla_norm_g,
              mla_q_norm_g, mla_w_uq, mla_kv_norm_g, mla_w_uk, mla_w_uv, w_branch_gla, w_branch_mla,
              w_out, ln_mix_g, ln_mix_b, router_group_w, router_group_b, router_expert_w,
              router_expert_b, expert_w_gate, expert_w_up, expert_w_down, ln_ffn_g, ln_ffn_b):
    B = x.shape[0]
    meta = jnp.broadcast_to(meta_tokens[None].astype(x.dtype), (B, N_META, x.shape[-1]))
    s = layer_norm(jnp.concatenate([meta, x], axis=1), ln_emb_g, ln_emb_b)
    L = s.shape[1]
    pos = jnp.arange(L, dtype=jnp.float32)
    inv_freq = ROPE_BASE ** (-jnp.arange(0, MLA_ROPE, 2, dtype=jnp.float32) / MLA_ROPE)
    ang = pos[:, None] * inv_freq[None, :]
    cos, sin = jnp.cos(ang), jnp.sin(ang)
    for l in range(DEPTH):
        proj = s @ w_in[l]
        (q_g, k_g, v_g, r_g, a_lr, c_q, c_kv, k_r, gate_a, gate_b) = jnp.split(proj, IN_OFFSETS, axis=-1)
        log_a = jax.nn.log_sigmoid((a_lr @ gla_gate_w2[l] + gla_gate_b[l]).astype(jnp.float32)) / GLA_GATE_TAU
        o_gla = gla_mixer(q_g, k_g, v_g, log_a, r_g, gla_norm_g[l])
        o_mla = mla_mixer(c_q, c_kv, k_r, mla_q_norm_g[l], mla_w_uq[l], mla_kv_norm_g[l],
                          mla_w_uk[l], mla_w_uv[l], cos, sin)
        merged = (jax.nn.sigmoid(gate_a) * (o_gla @ w_branch_gla[l])
                  + jax.nn.sigmoid(gate_b) * (o_mla @ w_branch_mla[l]))
        s = layer_norm(ALPHA * s + merged @ w_out[l], ln_mix_g[l], ln_mix_b[l])
        ffn = moe_ffn(s, router_group_w[l], router_group_b[l], router_expert_w[l], router_expert_b[l],
                      expert_w_gate[l], expert_w_up[l], expert_w_down[l])
        s = layer_norm(ALPHA * s + ffn, ln_ffn_g[l], ln_ffn_b[l])
    return s[:, N_META:]
```

```python
import contextlib
import numpy as np
import concourse.bass as bass
import concourse.mybir as mybir
from concourse.bass_utils import run_bass_kernel_spmd

F32 = mybir.dt.float32
BF16 = mybir.dt.bfloat16
AF = mybir.ActivationFunctionType
ALU = mybir.AluOpType
AX = mybir.AxisListType

NDSEM = 8
D = 1024
NPREV = 2176
NOWN = 2048
NPOS = NPREV + NOWN
TP = NPREV // 128
TO = NOWN // 128
TT = TP + TO
W_RES = 3760
ALPHA = 2.0 ** 0.25
LN_EPS = 1e-5
RMS_EPS = 1e-6
NEG = -30000.0
CAP = 256


class Op:
    __slots__ = ("eng", "fn", "waits", "signal", "idx", "sig_no", "dsem", "dtarget", "is_dma", "prewait")

    def __init__(self, eng, fn, is_dma):
        self.eng = eng
        self.fn = fn
        self.waits = []
        self.signal = False
        self.sig_no = None
        self.is_dma = is_dma
        self.dsem = None
        self.dtarget = None
        self.prewait = None
        self.idx = None


class Sched:
    ENGS = ("pe", "act", "dve", "pool", "sp")

    def __init__(self, nc):
        self.nc = nc
        self.ops = []
        self.last_w = {}
        self.readers = {}
        self.bar = None
        self.bar_passed = set()
        self.last_op = {}
        self.dma_recent = {}
        self.mute = False

    def barrier(self):
        b = [o for o in self.last_op.values()]
        for lst in self.dma_recent.values():
            b.extend(lst)
        self.bar = b
        self.bar_passed = set()
        self.last_w = {}
        self.readers = {}

    def op(self, eng, fn, reads=(), writes=(), dma=False):
        o = Op(eng, fn, dma)
        if self.mute:
            return o
        o.idx = len(self.ops)
        deps = set()
        for r in reads:
            w = self.last_w.get(r)
            if w is not None:
                deps.add(w)
            if isinstance(r, tuple) and r[0] == "pb":
                for rd in self.readers.get(r, ()):
                    if rd.eng != eng:
                        deps.add(rd)
        for r in writes:
            w = self.last_w.get(r)
            if w is not None:
                deps.add(w)
            for rd in self.readers.get(r, ()):
                deps.add(rd)
        for d in deps:
            if d.eng == eng and not d.is_dma and not dma:
                if eng == "pe":
                    continue
            o.waits.append(d)
            d.signal = True
        if self.bar is not None and eng not in self.bar_passed:
            self.bar_passed.add(eng)
            for d in self.bar:
                if d not in o.waits:
                    o.waits.append(d)
                    d.signal = True
        for r in reads:
            self.readers.setdefault(r, []).append(o)
        for r in writes:
            self.last_w[r] = o
            self.readers[r] = []
        self.ops.append(o)
        if dma:
            lst = self.dma_recent.setdefault(eng, [])
            lst.append(o)
            if len(lst) > NDSEM:
                lst.pop(0)
        else:
            self.last_op[eng] = o
        return o

    def emit(self, final_ops):
        nc = self.nc
        for fo in final_ops:
            fo.signal = True
        cnt = {e: 0 for e in self.ENGS}
        dcnt = {}
        dma_i = {e: 0 for e in self.ENGS}
        dma_hist = {}
        for o in self.ops:
            if o.is_dma:
                i = dma_i[o.eng]
                dma_i[o.eng] += 1
                slot = (o.eng, i % NDSEM)
                o.prewait = dma_hist.get(slot)
                dcnt[slot] = dcnt.get(slot, 0) + 16
                o.dsem = slot
                o.dtarget = dcnt[slot]
                dma_hist[slot] = o
            elif o.signal:
                cnt[o.eng] += 1
                o.sig_no = cnt[o.eng]
        with contextlib.ExitStack() as st:
            esem = {e: st.enter_context(nc.semaphore("s_" + e)) for e in ("pe", "act", "dve", "pool")}
            dsem = {}
            for e in ("sp", "pool", "act"):
                if dma_i[e] > 0:
                    for j in range(NDSEM):
                        dsem[(e, j)] = st.enter_context(nc.semaphore("d_%s%d" % (e, j)))
            block = st.enter_context(nc.Block())
            per = {e: [o for o in self.ops if o.eng == e] for e in self.ENGS}

            def run(ename, eng):
                waited = {}

                def wait_for(p):
                    if p.is_dma:
                        key, val, sem = p.dsem, p.dtarget, dsem[p.dsem]
                    else:
                        key, val, sem = p.eng, p.sig_no, esem[p.eng]
                    if waited.get(key, 0) >= val:
                        return
                    waited[key] = val
                    eng.wait_ge(sem, val)

                for o in per[ename]:
                    if o.is_dma and o.prewait is not None:
                        wait_for(o.prewait)
                    for p in o.waits:
                        wait_for(p)
                    ins = o.fn(eng)
                    if o.is_dma:
                        ins.then_inc(dsem[o.dsem], 16)
                    elif o.signal:
                        ins.then_inc(esem[o.eng], 1)
                if ename == "sp":
                    for fo in final_ops:
                        wait_for(fo)

            @block.tensor
            def _(eng):
                run("pe", eng)

            @block.scalar
            def _(eng):
                run("act", eng)

            @block.vector
            def _(eng):
                run("dve", eng)

            @block.gpsimd
            def _(eng):
                run("pool", eng)

            @block.sync
            def _(eng):
                run("sp", eng)
        return {e: len(per[e]) for e in self.ENGS}


class KB:
    def __init__(self, nc, st):
        self.nc = nc
        self.st = st
        self.S = Sched(nc)
        self.nps = 0
        self.banks = []
        self.uid = 0

    def sb(self, name, shape, dt):
        return self.st.enter_context(self.nc.sbuf_tensor(name, shape, dt))

    def init_psum(self):
        for i in range(8):
            self.banks.append(self.st.enter_context(self.nc.psum_tensor("pb%d" % i, [128, 512], F32)))
        self.open = [False] * 8

    def pb(self):
        for _ in range(8):
            i = self.nps % 8
            self.nps += 1
            if not self.open[i]:
                self.open[i] = True
                return self.banks[i], ("pb", i)
        raise RuntimeError("all PSUM banks open")

    def done(self, *keys):
        for k in keys:
            assert self.open[k[1]], k
            self.open[k[1]] = False

    def mm(self, out, lhsT, rhs, start, stop, r, w, skip=False):
        if skip:
            return self.S.op("pe", lambda e: e.matmul(out, lhsT=lhsT, rhs=rhs, start=start, stop=stop, skip_group_check=True), r, w)
        return self.S.op("pe", lambda e: e.matmul(out, lhsT=lhsT, rhs=rhs, start=start, stop=stop), r, w)

    def tr(self, out, in_, ident, r, w):
        return self.S.op("pe", lambda e: e.transpose(out, in_, ident), r, w)

    def act(self, out, in_, func, r, w, bias=None, scale=None, accum=None):
        kw = {}
        if bias is not None:
            kw["bias"] = bias
        if scale is not None:
            kw["scale"] = scale
        if accum is not None:
            kw["accum_out"] = accum
        return self.S.op("act", lambda e: e.activation(out=out, in_=in_, func=func, **kw), r, w)

    def tt(self, eng, out, in0, in1, op, r, w):
        return self.S.op(eng, lambda e: e.tensor_tensor(out=out, in0=in0, in1=in1, op=op), r, w)

    def ts(self, eng, out, in0, s1, s2, op0, op1, r, w, accum=None):
        if op1 is None:
            return self.S.op(eng, lambda e: e.tensor_scalar(out=out, in0=in0, scalar1=s1, scalar2=None, op0=op0), r, w)
        if accum is not None:
            return self.S.op(eng, lambda e: e.tensor_scalar(out=out, in0=in0, scalar1=s1, scalar2=s2, op0=op0, op1=op1, accum_out=accum), r, w)
        return self.S.op(eng, lambda e: e.tensor_scalar(out=out, in0=in0, scalar1=s1, scalar2=s2, op0=op0, op1=op1), r, w)

    def stt(self, eng, out, in0, scalar, in1, op0, op1, r, w):
        return self.S.op(eng, lambda e: e.scalar_tensor_tensor(out=out, in0=in0, scalar=scalar, in1=in1, op0=op0, op1=op1), r, w)

    def cp(self, eng, out, in_, r, w):
        if eng == "act":
            return self.S.op("act", lambda e: e.activation(out=out, in_=in_, func=AF.Copy), r, w)
        return self.S.op(eng, lambda e: e.tensor_copy(out=out, in_=in_), r, w)

    def recip(self, out, in_, r, w):
        return self.S.op("dve", lambda e: e.reciprocal(out=out, in_=in_), r, w)

    def red(self, out, in_, op, r, w):
        return self.S.op("dve", lambda e: e.tensor_reduce(out=out, in_=in_, axis=AX.X, op=op), r, w)

    def memset(self, eng, ap, val, w):
        return self.S.op(eng, lambda e: e.memset(ap, val), (), w)

    def dma(self, q, out, in_, r, w):
        return self.S.op(q, lambda e: e.dma_start(out=out, in_=in_), r, w, dma=True)


ARENA_BYTES = 176 * 1024


class Arena:
    def __init__(self, K):
        self.f = K.sb("arena", [128, ARENA_BYTES // 4], F32)
        self.fa = self.f[:]
        self.ba = self.f[:].bitcast(BF16)
        self.off = 0

    def set(self, off):
        self.off = off

    def F(self, n, parts=128):
        assert self.off % 4 == 0
        o = self.off // 4
        self.off += n * 4
        assert self.off <= ARENA_BYTES, self.off
        return self.fa[0:parts, o:o + n]

    def B(self, n, parts=128):
        assert self.off % 4 == 0
        o = self.off // 2
        self.off += ((n * 2 + 3) // 4) * 4
        assert self.off <= ARENA_BYTES, self.off
        return self.ba[0:parts, o:o + n]


def build(debug=False, stages=5, only=None, flags=()):
    nc = bass.Bass("TRN2", target_bir_lowering=False)

    def din(name, shape):
        return nc.dram_tensor(name, list(shape), F32, kind="ExternalInput").ap()

    xin = din("xin", [NPOS, D])
    vk = din("vk", [128, 2 * TT])
    cskv = din("cskv", [32, 2, NPOS])
    csq = din("csq", [96, 2, NOWN])
    cm = din("cm", [128, 776])
    cv = din("cv", [128, 16])
    lng = din("lng", [3, 2, D])
    w_in = din("w_in", [D, 5808])
    wkr2 = din("wkr2", [D, 2, 32])
    w2a = din("w2a", [33, 512])
    gcols = din("gcols", [128, 8])
    wuq2 = din("wuq2", [384, 16, 2, 96])
    wukp = din("wukp", [256, 16, 96])
    wuv = din("wuv", [256, 1024])
    wbg = din("wbg", [1024, 1024])
    wbm = din("wbm", [1024, 1024])
    wout = din("wout", [1024, 1024])
    wr = din("wr", [D, 36])
    rbias = din("rbias", [36])
    ewg = din("ewg", [32, D, 256])
    ewu = din("ewu", [32, D, 256])
    ewd = din("ewd", [32, 256, D])
    out = nc.dram_tensor("out", [NOWN, D], F32, kind="ExternalOutput").ap()
    okind = "ExternalOutput" if debug else "Internal"
    oglaT_d = nc.dram_tensor("oglaT_d", [128, 8, NOWN], BF16, kind=okind).ap()
    sT_d = nc.dram_tensor("sT_d", [128, 8, NOWN], BF16, kind=okind).ap()
    s_d = nc.dram_tensor("s_d", [NOWN, D], F32, kind=okind).ap()
    s2_d = nc.dram_tensor("s2_d", [NOWN, D], F32, kind=okind).ap()
    G_d = nc.dram_tensor("G_d", [32 * CAP, D], BF16, kind="Internal").ap()
    Y_d = nc.dram_tensor("Y_d", [32 * CAP, D], BF16, kind="Internal").ap()
    ewg_b = nc.dram_tensor("ewg_b", [32, D, 256], BF16, kind="Internal").ap()
    ewu_b = nc.dram_tensor("ewu_b", [32, D, 256], BF16, kind="Internal").ap()
    ewd_b = nc.dram_tensor("ewd_b", [32, 256, D], BF16, kind="Internal").ap()
    sparse = "dense" not in flags
    bc_cache = {}

    def bcreg(e):
        if "r" not in bc_cache:
            bc_cache["r"] = e.to_reg(32 * CAP - 1)
        return bc_cache["r"]
    dbg = {}
    if debug:
        dbg["omlaT"] = nc.dram_tensor("omlaT_d", [128, 8, NOWN], BF16, kind="ExternalOutput").ap()
        dbg["comb"] = nc.dram_tensor("comb_d", [128, TO, 32], F32, kind="ExternalOutput").ap()
        dbg["ckvnT"] = nc.dram_tensor("ckvnT_d", [128, 2, NPOS], BF16, kind="ExternalOutput").ap()
        dbg["kropeT"] = nc.dram_tensor("kropeT_d", [32, NPOS], BF16, kind="ExternalOutput").ap()
        dbg["cqnT"] = nc.dram_tensor("cqnT_d", [128, 3, NOWN], BF16, kind="ExternalOutput").ap()

    with contextlib.ExitStack() as st:
        K = KB(nc, st)
        S = K.S
        sb = K.sb
        K.init_psum()
        finals = []
        cmt = sb("cmt", [128, 776], F32)
        cvt = sb("cvt", [128, 16], F32)
        vkt = sb("vkt", [128, 2 * TT], F32)
        cmb = sb("cmb", [128, 640], BF16)
        gcol = sb("gcol", [128, 8], F32)
        st8 = [sb("st8_%d" % i, [128, 8], F32) for i in range(4)]
        comb = sb("comb", [128, TO, 32], F32)
        idxi = sb("idxi", [128, 32], mybir.dt.int32)
        wts = sb("wts", [128, 32], F32)
        gthb = [sb("gthb%d" % i, [128, D], F32) for i in range(3)]
        junk = sb("junk", [128, D], BF16)
        junk2 = [junk, sb("junkb", [128, D], BF16)]
        K.dma("sp", cmt[:], cm, (), ["cmt"])
        K.dma("sp", cvt[:], cv, (), ["cvt"])
        K.dma("sp", vkt[:], vk, (), ["vkt"])
        K.dma("sp", gcol[:], gcols, (), ["gcol"])
        K.cp("dve", cmb[:], cmt[:, 0:640], ["cmt"], ["cmb"])
        ident = cmt[:, 0:128]
        triT = cmt[:, 128:256]
        chsel = cmt[:, 384:386]
        ones_b = cmb[:, 482:610]
        ut_b = cmb[:, 256:384]
        tri_b = cmb[:, 128:256]
        selr_b = cmb[0:32, 386:482]
        c_one = cvt[:, 0:1]
        c_lneps = cvt[:, 1:2]
        c_rmseps = cvt[:, 2:3]
        C0 = ["cmt", "cvt", "vkt", "cmb", "gcol"]
        A = Arena(K)

        def v3(ap, n):
            return ap.rearrange("p (c n) -> p c n", n=n)

        def ln_tok(x_t, XT, stt_, ST, lnbc, LK, gi):
            K.act(junk[:], x_t, AF.Identity, [XT], ["junk", ST], accum=stt_[:, 0:1])
            K.act(junk[:], x_t, AF.Square, [XT], ["junk", ST], accum=stt_[:, 1:2])
            K.ts("dve", stt_[:, 2:3], stt_[:, 0:1], 1.0 / D, None, ALU.mult, None, [ST], [ST])
            K.tt("dve", stt_[:, 3:4], stt_[:, 2:3], stt_[:, 2:3], ALU.mult, [ST], [ST])
            K.stt("dve", stt_[:, 4:5], stt_[:, 1:2], 1.0 / D, stt_[:, 3:4], ALU.mult, ALU.subtract, [ST], [ST])
            K.act(stt_[:, 5:6], stt_[:, 4:5], AF.Sqrt, [ST] + C0, [ST], bias=c_lneps, scale=1.0)
            K.recip(stt_[:, 5:6], stt_[:, 5:6], [ST], [ST])
            K.stt("dve", stt_[:, 6:7], stt_[:, 2:3], -1.0, stt_[:, 5:6], ALU.mult, ALU.mult, [ST], [ST])
            K.act(x_t, x_t, AF.Identity, [XT, ST], [XT], bias=stt_[:, 6:7], scale=stt_[:, 5:6])
            K.tt("dve", x_t, x_t, lnbc[:, gi, :], ALU.mult, [XT, LK], [XT])
            K.tt("dve", x_t, x_t, lnbc[:, gi + 1, :], ALU.add, [XT, LK], [XT])

        A.set(0)
        ckvnT = v3(A.B(2 * NPOS), NPOS)
        cqnT = v3(A.B(3 * NOWN), NOWN)
        kropeT = A.B(NPOS, parts=32)
        P1_END = A.off
        OMLA_OFF = P1_END

        S.mute = only is not None and 1 not in only
        wres = v3(A.B(8 * W_RES), W_RES)
        wkr = v3(A.B(8 * 64), 64)
        sTw = [v3(A.B(1024), 128) for i in range(2)]
        ktb = [A.B(512) for i in range(2)]
        vb = [A.B(1024) for i in range(2)]
        q0T = [v3(A.B(512), 128) for i in range(2)]
        q1T = [v3(A.B(512), 128) for i in range(2)]
        ktT = v3(A.B(512), 128)
        attm = [v3(A.B(512), 128) for i in range(2)]
        Sbf = [[A.B(256) for i in range(3)] for h in range(4)]
        sqb = A.B(1024)
        oglaT = [v3(A.B(1024), 128) for i in range(2)]
        sqm = A.B(640)
        S32 = [A.F(256) for h in range(4)]
        loga = A.F(512)
        e2 = A.F(512)
        e1T = A.F(512)
        e2T = A.F(512)
        silur2 = [A.F(1024) for i in range(2)]
        rstdg = A.F(512)
        aaug = A.F(128, parts=33)
        w2t = A.F(512, parts=33)
        dec2 = [A.F(8) for i in range(2)]
        cs32 = [v3(A.F(256, parts=32), 128) for i in range(2)]
        rsm = A.F(128)
        tmpS = [A.F(256) for h in range(4)]
        xt = [A.F(D) for i in range(2)]
        lnbc = v3(A.F(2 * D), D)
        print("stage1 arena end", A.off)

        K.dma("sp", lnbc[:, 0, :], lng[0, 0].partition_broadcast(128), (), ["lnbc"])
        K.dma("sp", lnbc[:, 1, :], lng[0, 1].partition_broadcast(128), (), ["lnbc"])
        w_in_v = w_in.rearrange("(c p) n -> p c n", p=128)
        for c in range(8):
            K.dma("pool", wres[:, c, :], w_in_v[:, c, 0:W_RES], (), [("wres", c)])
        K.dma("pool", wkr, wkr2.rearrange("(c p) a n -> p c (a n)", p=128), (), ["wkr"])
        K.dma("sp", w2t, w2a, (), ["w2t"])
        K.memset("dve", aaug, 0.0, ["aaug"])
        K.memset("dve", aaug[32:33, :], 1.0, ["aaug"])
        for h in range(4):
            K.memset("pool", S32[h], 0.0, [("S32", h)])
            K.memset("pool", Sbf[h][0], 0.0, [("Sbf", h, 0)])
        for i in range(2):
            K.memset("pool", q0T[i], 0.0, [("q0T", i)])
            K.memset("pool", q1T[i], 0.0, [("q1T", i)])
        sver = [0, 0, 0, 0]

        def X(i):
            own = i >= TP
            j = i - TP
            p0 = i * 128
            b2 = i % 2
            x_t = xt[b2]
            XT = ("xt", b2)
            stt_ = st8[i % 4]
            ST = ("st8", i % 4)
            valid = vkt[:, i:i + 1]
            K.dma("sp", x_t, xin[p0:p0 + 128, :], (), [XT])
            ln_tok(x_t, XT, stt_, ST, lnbc, "lnbc", 0)
            if own:
                K.dma("sp", s_d[j * 128:(j + 1) * 128, :], x_t, [XT], ["s_d"])
            yield
            sT_t = sTw[b2]
            STK = ("sTw", b2)
            for half in range(2):
                pt, PK = K.pb()
                for cc in range(4):
                    c = half * 4 + cc
                    K.tr(pt[:, cc * 128:(cc + 1) * 128], x_t[:, c * 128:(c + 1) * 128], ident, [XT] + C0, [PK])
                K.cp("dve" if half == 0 else "act", sT_t[:, half * 4:half * 4 + 4, :], v3(pt[:, :], 128), [PK], [STK])
                K.done(PK)
            if own:
                K.dma("sp", sT_d[:, :, j * 128:(j + 1) * 128], sT_t, [STK], ["sT_d"])
            yield

            def proj_tok(col0, ncols):
                pt, PK = K.pb()
                for c in range(8):
                    K.mm(pt[:, 0:ncols], sT_t[:, c, :], wres[:, c, col0:col0 + ncols], c == 0, c == 7, [STK, ("wres", c)], [PK])
                return pt, PK

            def proj_feat(pt, PK, off, wsrc, col0, m, wkey):
                for c in range(8):
                    K.mm(pt[0:m, off:off + 128], wsrc[:, c, col0:col0 + m], sT_t[:, c, :], c == 0, c == 7, [STK, wkey if wkey else ("wres", c)], [PK])

            pa, PKa = K.pb()
            proj_feat(pa, PKa, 0, wres, 3072, 16, None)
            K.cp("dve", aaug[0:16, :], pa[0:16, 0:128], [PKa], ["aaug"])
            K.done(PKa)
            pz, PKz = K.pb()
            K.mm(pz[:, :], aaug, w2t, True, True, ["aaug", "w2t"], [PKz])
            lg = loga
            LG = "loga"
            K.act(lg, pz[:, :], AF.Exp, [PKz], [LG], scale=-1.0)
            K.done(PKz)
            K.act(lg, lg, AF.Ln, [LG] + C0, [LG], bias=c_one, scale=1.0)
            K.ts("dve", lg, lg, valid, -1.0 / 16.0, ALU.mult, ALU.mult, [LG] + C0, [LG])
            yield
            v_b = vb[b2]
            VB = ("vb", b2)
            for hv in range(2):
                pv, PKv = proj_tok(1024 + hv * 512, 512)
                K.act(v_b[:, hv * 512:(hv + 1) * 512], pv[:, :], AF.Copy, [PKv] + C0, [VB], scale=valid)
                K.done(PKv)
                yield
            pbc, PKbc = K.pb()
            K.mm(pbc[:, :], triT, lg, True, True, [LG] + C0, [PKbc])
            K.act(e2, pbc[:, :], AF.Exp, [PKbc], ["e2"], scale=-1.0)
            K.done(PKbc)
            pk, PKk = proj_tok(512, 512)
            k_t = ktb[b2]
            KT = ("ktb", b2)
            K.tt("dve", k_t, pk[:, :], e2, ALU.mult, [PKk, "e2"], [KT])
            K.done(PKk)
            pd, PKd = K.pb()
            for h in range(4):
                K.mm(pd[:, h * 2:h * 2 + 2], lg[:, h * 128:(h + 1) * 128], chsel, True, True, [LG] + C0, [PKd])
            K.act(dec2[b2], pd[:, 0:8], AF.Exp, [PKd], [("dec", b2)])
            K.done(PKd)
            yield
            if own:
                pbT, PKbT = K.pb()
                for h in range(4):
                    K.mm(pbT[:, h * 128:(h + 1) * 128], lg[:, h * 128:(h + 1) * 128], triT, True, True, [LG] + C0, [PKbT])
                K.act(e1T, pbT[:, :], AF.Exp, [PKbT], ["e1T"])
                K.act(e2T, pbT[:, :], AF.Exp, [PKbT], ["e2T"], scale=-1.0)
                K.done(PKbT)
                pq, PKq = K.pb()
                for h in range(4):
                    proj_feat(pq, PKq, h * 128, wres, h * 128, 128, None)
                q0 = q0T[b2]
                q1 = q1T[b2]
                Q0 = ("q0T", b2)
                Q1 = ("q1T", b2)
                K.stt("dve", q0[:, :, 0:64], v3(pq[:, :], 128)[:, :, 0:64], 128.0 ** -0.5, v3(e1T, 128)[:, :, 0:64], ALU.mult, ALU.mult, [PKq, "e1T"], [Q0])
                K.stt("dve", q1[:, :, 64:128], v3(pq[:, :], 128)[:, :, 64:128], 128.0 ** -0.5, v3(e1T, 128)[:, :, 64:128], ALU.mult, ALU.mult, [PKq, "e1T"], [Q1])
                K.done(PKq)
                yield
                pkT, PKkT = K.pb()
                for h in range(4):
                    proj_feat(pkT, PKkT, h * 128, wres, 512 + h * 128, 128, None)
                k_T = ktT
                KTT = "ktT"
                K.tt("dve", k_T.rearrange("p h n -> p (h n)"), pkT[:, :], e2T, ALU.mult, [PKkT, "e2T"], [KTT])
                K.done(PKkT)
                yield
                pat, PKat = K.pb()
                for h in range(4):
                    K.mm(pat[:, h * 128:(h + 1) * 128], k_T[:, h, :], q0[:, h, :], True, False, [KTT, Q0], [PKat])
                    K.mm(pat[:, h * 128:(h + 1) * 128], k_T[:, h, :], q1[:, h, :], False, True, [KTT, Q1], [PKat])
                a_m = attm[b2]
                AM = ("attm", b2)
                for h in range(4):
                    K.tt("dve", a_m[:, h, :], pat[:, h * 128:(h + 1) * 128], tri_b, ALU.mult, [PKat] + C0, [AM])
                K.done(PKat)
                yield
                for hr in range(2):
                    pr_, PKr_ = K.pb()
                    for vc in range(4):
                        proj_feat(pr_, PKr_, vc * 128, wres, 2048 + (hr * 4 + vc) * 128, 128, None)
                    K.act(silur2[b2][:, hr * 512:(hr + 1) * 512], pr_[:, :], AF.Silu, [PKr_], [("silur", b2)])
                    K.done(PKr_)
                    yield
            pc, PKc = K.pb()
            proj_feat(pc, PKc, 0, wres, 3472, 128, None)
            proj_feat(pc, PKc, 128, wres, 3600, 128, None)
            proj_feat(pc, PKc, 256, wkr, 0, 32, "wkr")
            proj_feat(pc, PKc, 384, wkr, 32, 32, "wkr")
            sm = sqm
            SM = "sqm"
            K.act(sm[:, 0:256], pc[:, 0:256], AF.Square, [PKc], [SM])
            pr, PKr = K.pb()
            K.mm(pr[:, 0:128], ones_b, sm[:, 0:128], True, False, [SM] + C0, [PKr])
            K.mm(pr[:, 0:128], ones_b, sm[:, 128:256], False, True, [SM] + C0, [PKr])
            K.act(rsm, pr[:, 0:128], AF.Sqrt, [PKr] + C0, ["rsm"], bias=c_rmseps, scale=1.0 / 256.0)
            K.done(PKr)
            K.recip(rsm, rsm, ["rsm"], ["rsm"])
            for c in range(2):
                K.stt("dve", ckvnT[:, c, p0:p0 + 128], pc[:, c * 128:(c + 1) * 128], gcol[:, 5 + c:6 + c], rsm, ALU.mult, ALU.mult,
                      [PKc, "rsm"] + C0, [("ckvnT", i)])
            cs_ = cs32[b2]
            CS = ("cs32", b2)
            K.dma("sp", cs_, cskv[:, :, p0:p0 + 128], (), [CS])
            K.tt("dve", cs_[:, 0, :], pc[0:32, 256:384], cs_[:, 0, :], ALU.mult, [PKc, CS], [CS])
            K.tt("dve", cs_[:, 1, :], pc[0:32, 384:512], cs_[:, 1, :], ALU.mult, [PKc, CS], [CS])
            K.done(PKc)
            K.tt("pool", kropeT[:, p0:p0 + 128], cs_[:, 0, :], cs_[:, 1, :], ALU.add, [CS], [("kropeT", i)])
            yield
            if own:
                pq3, PKq3 = K.pb()
                for c in range(3):
                    proj_feat(pq3, PKq3, c * 128, wres, 3088 + c * 128, 128, None)
                K.act(sm[:, 256:640], pq3[:, 0:384], AF.Square, [PKq3], [SM])
                pr, PKr = K.pb()
                for c in range(3):
                    K.mm(pr[:, 0:128], ones_b, sm[:, 256 + c * 128:256 + (c + 1) * 128], c == 0, c == 2, [SM] + C0, [PKr])
                K.act(rsm, pr[:, 0:128], AF.Sqrt, [PKr] + C0, ["rsm"], bias=c_rmseps, scale=1.0 / 384.0)
                K.done(PKr)
                K.recip(rsm, rsm, ["rsm"], ["rsm"])
                for c in range(3):
                    K.stt("dve", cqnT[:, c, j * 128:(j + 1) * 128], pq3[:, c * 128:(c + 1) * 128], gcol[:, 2 + c:3 + c], rsm, ALU.mult, ALU.mult,
                          [PKq3, "rsm"] + C0, [("cqnT", j)])
                K.done(PKq3)
                yield

        def Y(i):
            own = i >= TP
            j = i - TP
            b2 = i % 2
            v_b = vb[b2]
            VB = ("vb", b2)
            k_t = ktb[b2]
            KT = ("ktb", b2)
            dec = dec2[b2]
            DK = ("dec", b2)

            def state_update(ch):
                for h in range(4):
                    pu, PKu = K.pb()
                    K.mm(pu[:, 0:256], k_t[ch * 64:(ch + 1) * 64, h * 128:(h + 1) * 128], v_b[ch * 64:(ch + 1) * 64, h * 256:(h + 1) * 256],
                         True, True, [KT, VB], [PKu])
                    K.tt("dve", tmpS[h], pu[:, 0:256], S32[h], ALU.add, [PKu, ("S32", h)], [("tmpS", h)])
                    K.done(PKu)
                    nv = (sver[h] + 1) % 3
                    K.act(Sbf[h][nv], tmpS[h], AF.Copy, [("tmpS", h), DK], [("Sbf", h, nv)], scale=dec[:, h * 2 + ch:h * 2 + ch + 1])
                    K.ts("dve", S32[h], tmpS[h], dec[:, h * 2 + ch:h * 2 + ch + 1], None, ALU.mult, None, [("tmpS", h), DK], [("S32", h)])
                    sver[h] = nv
                    if h % 2 == 1:
                        yield

            if not own:
                yield from state_update(0)
                yield from state_update(1)
                return
            q0 = q0T[b2]
            q1 = q1T[b2]
            Q0 = ("q0T", b2)
            Q1 = ("q1T", b2)
            a_m = attm[b2]
            AM = ("attm", b2)
            silur = silur2[b2]
            SR = ("silur", b2)
            po0, PKo0 = K.pb()
            po1, PKo1 = K.pb()
            sa = list(sver)
            for h in range(4):
                for vc in range(2):
                    idx = h * 2 + vc
                    po, PKo = (po0, PKo0) if idx < 4 else (po1, PKo1)
                    oc = (idx % 4) * 128
                    K.mm(po[:, oc:oc + 128], Sbf[h][sa[h]][:, vc * 128:(vc + 1) * 128], q0[:, h, :], idx % 4 == 0, False, [("Sbf", h, sa[h]), Q0], [PKo], skip=True)
                    K.mm(po[:, oc:oc + 128], v_b[:, h * 256 + vc * 128:h * 256 + (vc + 1) * 128], a_m[:, h, :], False, False, [VB, AM], [PKo], skip=True)
                if h % 2 == 1:
                    yield
            yield from state_update(0)
            for h in range(4):
                for vc in range(2):
                    idx = h * 2 + vc
                    po, PKo = (po0, PKo0) if idx < 4 else (po1, PKo1)
                    oc = (idx % 4) * 128
                    K.mm(po[:, oc:oc + 128], Sbf[h][sver[h]][:, vc * 128:(vc + 1) * 128], q1[:, h, :], False, idx % 4 == 3, [("Sbf", h, sver[h]), Q1], [PKo], skip=True)
            yield
            yield from state_update(1)
            sq_ = sqb
            SQ = "sqb"
            K.act(sq_[:, 0:512], po0[:, :], AF.Square, [PKo0], [SQ])
            K.act(sq_[:, 512:1024], po1[:, :], AF.Square, [PKo1], [SQ])
            pss, PKss = K.pb()
            for h in range(4):
                for vc in range(2):
                    K.mm(pss[:, h * 128:(h + 1) * 128], ones_b, sq_[:, (h * 2 + vc) * 128:(h * 2 + vc + 1) * 128], vc == 0, vc == 1, [SQ] + C0, [PKss])
            K.act(rstdg, pss[:, :], AF.Sqrt, [PKss] + C0, ["rstdg"], bias=c_rmseps, scale=1.0 / 256.0)
            K.done(PKss)
            K.recip(rstdg, rstdg, ["rstdg"], ["rstdg"])
            yield
            og = oglaT[b2]
            OG = ("oglaT", b2)
            for h in range(4):
                for vc in range(2):
                    idx = h * 2 + vc
                    po, PKo = (po0, PKo0) if idx < 4 else (po1, PKo1)
                    oc = (idx % 4) * 128
                    K.stt("dve", silur[:, idx * 128:(idx + 1) * 128], po[:, oc:oc + 128], gcol[:, vc:vc + 1], silur[:, idx * 128:(idx + 1) * 128],
                          ALU.mult, ALU.mult, [PKo, SR] + C0, [SR])
                    K.tt("pool", og[:, idx, :], silur[:, idx * 128:(idx + 1) * 128], rstdg[:, h * 128:(h + 1) * 128], ALU.mult, [SR, "rstdg"], [OG])
                if h % 2 == 1:
                    yield
            K.done(PKo0, PKo1)
            K.dma("sp", oglaT_d[:, :, j * 128:(j + 1) * 128], og, [OG], ["oglaT_d"])

        def drain(g):
            for _ in g:
                pass

        def interleave(ga, gb):
            ga_done = ga is None
            gb_done = gb is None
            while not (ga_done and gb_done):
                if not ga_done:
                    try:
                        next(ga)
                    except StopIteration:
                        ga_done = True
                if not gb_done:
                    try:
                        next(gb)
                    except StopIteration:
                        gb_done = True

        if "nopipe" in flags:
            for i in range(TT):
                drain(X(i))
                drain(Y(i))
        else:
            drain(X(0))
            for i in range(TT):
                interleave(X(i + 1) if i + 1 < TT else None, Y(i))

        CKV = [("ckvnT", i) for i in range(TT)]
        KRP = [("kropeT", i) for i in range(TT)]
        CQN = [("cqnT", j) for j in range(TO)]
        if debug:
            finals.append(K.dma("sp", dbg["ckvnT"], ckvnT, CKV, []))
            finals.append(K.dma("sp", dbg["kropeT"], kropeT, KRP, []))
            finals.append(K.dma("sp", dbg["cqnT"], cqnT, CQN, []))
        if stages >= 3:
            S.barrier()
            S.mute = only is not None and 3 not in only
            A.set(OMLA_OFF)
            omlaT = v3(A.B(8 * NOWN), NOWN)
            OMLA_END = A.off
            wuqt = A.B(3 * 16 * 192).rearrange("p (c h n) -> p c h n", c=3, h=16)
            wukt = A.B(2 * 16 * 96).rearrange("p (c h n) -> p c h n", c=2, h=16)
            wuvt = v3(A.B(2 * 1024), 1024)
            KTh = [A.B(NPOS, parts=96) for i in range(2)]
            Vh = [v3(A.B(TT * 128), 128) for i in range(2)]
            QTh = [A.B(NOWN, parts=96) for i in range(2)]
            PT = [A.B(512) for i in range(6)]
            csqt = v3(A.F(2 * NOWN, parts=96), NOWN)
            qa = [A.F(512, parts=96) for i in range(2)]
            rden = A.F(512)
            print("stage3 arena end", A.off)
            K.dma("pool", wuqt.rearrange("p c h n -> p c (h n)"), wuq2.rearrange("(c p) h a n -> p c (h a n)", p=128), (), ["wuqt"])
            K.dma("pool", wukt.rearrange("p c h n -> p c (h n)"), wukp.rearrange("(c p) h n -> p c (h n)", p=128), (), ["wukt"])
            K.dma("pool", wuvt, wuv.rearrange("(c p) n -> p c n", p=128), (), ["wuvt"])
            K.dma("sp", csqt, csq, (), ["csqt"])
            for p_ in range(2):
                K.cp("act", KTh[p_][64:96, :], kropeT, KRP, [("KTh", p_)])
            K.memset("pool", Vh[0][:, :, 64:128], 1.0, [("Vh", 0)])
            K.memset("pool", Vh[1][:, :, 0:64], 1.0, [("Vh", 1)])
            sm_scale = 96.0 ** -0.5

            def prep_chunks(h):
                par = h % 2
                KT_, KTK = KTh[par], ("KTh", par)
                V_, VK = Vh[par], ("Vh", par)
                Q_, QK = QTh[par], ("QTh", par)
                chunks = []
                nb = (NPOS + 511) // 512

                def m1(blk):
                    c0 = blk * 512
                    n = min(512, NPOS - c0)
                    pt, PK = K.pb()
                    K.mm(pt[0:64, 0:n], wukt[:, 0, h, 0:64], ckvnT[:, 0, c0:c0 + n], True, False, ["wukt"] + CKV, [PK])
                    K.mm(pt[0:64, 0:n], wukt[:, 1, h, 0:64], ckvnT[:, 1, c0:c0 + n], False, True, ["wukt"] + CKV, [PK])
                    K.cp("dve", KT_[0:64, c0:c0 + n], pt[0:64, 0:n], [PK], [KTK])
                    K.done(PK)

                def m2(t0):
                    vo = 0 if par == 0 else 64
                    nt = min(8, TT - t0)
                    pt, PK = K.pb()
                    for t in range(nt):
                        for c in range(2):
                            K.mm(pt[:, t * 64:(t + 1) * 64], ckvnT[:, c, (t0 + t) * 128:(t0 + t + 1) * 128], wuvt[:, c, h * 64:(h + 1) * 64], c == 0, c == 1,
                                 ["wuvt"] + CKV, [PK])
                    K.cp("dve", V_[:, t0:t0 + nt, vo:vo + 64], v3(pt[:, 0:nt * 64], 64), [PK], [VK])
                    K.done(PK)

                def m3(qb):
                    c0 = qb * 512
                    pA, PKA = K.pb()
                    pB, PKB = K.pb()
                    for c in range(3):
                        K.mm(pA[0:96, :], wuqt[:, c, h, 0:96], cqnT[:, c, c0:c0 + 512], c == 0, c == 2, ["wuqt"] + CQN, [PKA])
                    for c in range(3):
                        K.mm(pB[0:96, :], wuqt[:, c, h, 96:192], cqnT[:, c, c0:c0 + 512], c == 0, c == 2, ["wuqt"] + CQN, [PKB])
                    K.tt("dve", qa[0], pA[0:96, :], csqt[:, 0, c0:c0 + 512], ALU.mult, [PKA, "csqt"], ["qa0"])
                    K.tt("dve", qa[1], pB[0:96, :], csqt[:, 1, c0:c0 + 512], ALU.mult, [PKB, "csqt"], ["qa1"])
                    K.done(PKA, PKB)
                    K.tt("pool", Q_[:, c0:c0 + 512], qa[0], qa[1], ALU.add, ["qa0", "qa1"], [QK])

                for blk in range(nb):
                    chunks.append((m1, blk))
                for t0 in range(0, TT, 8):
                    chunks.append((m2, t0))
                for qb in range(4):
                    chunks.append((m3, qb))
                return chunks

            items = []
            for h in range(16):
                for qb in range(4):
                    full = [(t, 0) for t in range(TP)] + [(TP + 4 * qb2 + d, 0) for qb2 in range(qb) for d in range(4)]
                    diag = [(TP + 4 * qb + d, d * 128) for d in range(4)]
                    ktiles = full[0:1] + diag + full[1:]
                    for n_, (t, qo) in enumerate(ktiles):
                        items.append((h, qb, t, qo, n_ == 0, n_ == len(ktiles) - 1, t >= TP + 4 * qb))
            LA = 3
            precast = []
            for e_ in range(32):
                precast += [(ewg[e_], ewg_b[e_]), (ewu[e_], ewu_b[e_]), (ewd[e_], ewd_b[e_])]
            NPT = len(PT)
            pobank = {}
            pending = []
            for f_, a_ in prep_chunks(0):
                f_(a_)
            hcur = -1
            since = 0
            for idx in range(len(items) + LA):
                if idx < len(items):
                    h, qb, t, qo, first, last, isdiag = items[idx]
                    par = h % 2
                    if h != hcur:
                        for f_, a_ in pending:
                            f_(a_)
                        pending = prep_chunks(h + 1) if h + 1 < 16 else []
                        hcur = h
                        since = 0
                    since += 1
                    if sparse and idx % 18 == 0 and precast:
                        src_, dst_ = precast.pop(0)
                        K.dma("pool", dst_, src_, (), [])
                    if pending and since % 5 == 0:
                        f_, a_ = pending.pop(0)
                        f_(a_)
                    ps_, PKs = K.pb()
                    nq = 512 - qo
                    c0 = qb * 512
                    K.mm(ps_[:, 0:nq], KTh[par][:, t * 128:(t + 1) * 128], QTh[par][:, c0 + qo:c0 + 512], True, True, [("KTh", par), ("QTh", par)], [PKs])
                    pT = PT[idx % NPT]
                    PTK = ("PT", idx % NPT)
                    K.act(pT[:, 0:nq], ps_[:, 0:nq], AF.Exp, [PKs] + C0, [PTK], bias=vkt[:, TT + t:TT + t + 1], scale=sm_scale)
                    K.done(PKs)
                    if isdiag:
                        K.tt("pool", pT[:, 0:128], pT[:, 0:128], ut_b, ALU.mult, [PTK] + C0, [PTK])
                j_ = idx - LA
                if j_ >= 0:
                    h, qb, t, qo, first, last, isdiag = items[j_]
                    par = h % 2
                    c0 = qb * 512
                    nq = 512 - qo
                    if first:
                        pobank[(h, qb)] = K.pb()
                    po, PKo = pobank[(h, qb)]
                    pT = PT[j_ % NPT]
                    PTK = ("PT", j_ % NPT)
                    K.mm(po[:, qo:512], Vh[par][:, t, :], pT[:, 0:nq], first, last, [("Vh", par), PTK], [PKo])
                    if last:
                        hp = h // 2
                        if par == 0:
                            K.recip(rden[0:64, :], po[64:128, :], [PKo], ["rden"])
                            K.tt("dve", omlaT[0:64, hp, c0:c0 + 512], po[0:64, :], rden[0:64, :], ALU.mult, [PKo, "rden"], [("omlaT", qb)])
                        else:
                            K.recip(rden[64:128, :], po[0:64, :], [PKo], ["rden"])
                            K.tt("dve", omlaT[64:128, hp, c0:c0 + 512], po[64:128, :], rden[64:128, :], ALU.mult, [PKo, "rden"], [("omlaT", qb)])
                        K.done(PKo)
                        del pobank[(h, qb)]
            if sparse:
                for src_, dst_ in precast:
                    K.dma("pool", dst_, src_, (), [])
            OMK = [("omlaT", q) for q in range(4)]
            if debug:
                finals.append(K.dma("sp", dbg["omlaT"], omlaT, OMK, []))
        def ln_gen(x_t, XTL, stt_, ST, lnbc, LK, gi):
            JK = ("junk2", ST[1] % 2)
            jk = junk2[ST[1] % 2]
            K.act(jk[:], x_t, AF.Identity, XTL, [JK, ST], accum=stt_[:, 0:1]); yield
            K.act(jk[:], x_t, AF.Square, XTL, [JK, ST], accum=stt_[:, 1:2]); yield
            K.ts("dve", stt_[:, 2:3], stt_[:, 0:1], 1.0 / D, None, ALU.mult, None, [ST], [ST]); yield
            K.tt("dve", stt_[:, 3:4], stt_[:, 2:3], stt_[:, 2:3], ALU.mult, [ST], [ST]); yield
            K.stt("dve", stt_[:, 4:5], stt_[:, 1:2], 1.0 / D, stt_[:, 3:4], ALU.mult, ALU.subtract, [ST], [ST]); yield
            K.act(stt_[:, 5:6], stt_[:, 4:5], AF.Sqrt, [ST] + C0, [ST], bias=c_lneps, scale=1.0); yield
            K.recip(stt_[:, 5:6], stt_[:, 5:6], [ST], [ST]); yield
            K.stt("dve", stt_[:, 6:7], stt_[:, 2:3], -1.0, stt_[:, 5:6], ALU.mult, ALU.mult, [ST], [ST]); yield
            K.act(x_t, x_t, AF.Identity, XTL + [ST], XTL, bias=stt_[:, 6:7], scale=stt_[:, 5:6]); yield
            K.tt("dve", x_t, x_t, lnbc[:, gi, :], ALU.mult, XTL + [LK], XTL); yield
            K.tt("dve", x_t, x_t, lnbc[:, gi + 1, :], ALU.add, XTL + [LK], XTL); yield

        if stages >= 4:
            S.barrier()
            S.mute = only is not None and 4 not in only
            assert OMLA_OFF == 37632 and OMLA_END == 70400
            A.set(0)
            oglaTs = v3(A.B(8 * 512), 512)
            sTs = v3(A.B(8 * 512), 512)
            woutt = v3(A.B(8 * 1024), 1024)
            A.set(OMLA_END)
            wgat = v3(A.B(8 * 2048), 2048)
            wbgt = v3(A.B(8 * 1024), 1024)
            wbmt = v3(A.B(8 * 1024), 1024)
            W4_END = A.off
            mergedT = v3(A.B(8 * NOWN), NOWN)
            M_END = A.off
            sga = [A.B(512) for i in range(4)]
            print("stage4a arena end", A.off)
            for c in range(8):
                K.dma("pool", wgat[:, c, :], w_in_v[:, c, W_RES:5808], (), [("wgat", c)])
                K.dma("pool", wbgt[:, c, :], wbg.rearrange("(c p) n -> p c n", p=128)[:, c, :], (), [("wbgt", c)])
                K.dma("pool", wbmt[:, c, :], wbm.rearrange("(c p) n -> p c n", p=128)[:, c, :], (), [("wbmt", c)])
            K.dma("pool", woutt, wout.rearrange("(c p) n -> p c n", p=128), (), ["woutt"])
            WG8 = [("wgat", c) for c in range(8)]
            WB8 = [("wbgt", c) for c in range(8)]
            WM8 = [("wbmt", c) for c in range(8)]
            nsg = 0
            for blk in range(4):
                c0 = blk * 512
                K.dma("sp", oglaTs, oglaT_d[:, :, c0:c0 + 512], ["oglaT_d"], ["oglaTs"])
                K.dma("sp", sTs, sT_d[:, :, c0:c0 + 512], ["sT_d"], ["sTs"])
                for dc in range(8):
                    sa_ = sga[nsg % 4]; SA = ("sga", nsg % 4); nsg += 1
                    sb_ = sga[nsg % 4]; SB = ("sga", nsg % 4); nsg += 1
                    pga, PKga = K.pb()
                    pgb, PKgb = K.pb()
                    for c in range(8):
                        K.mm(pga[:, :], wgat[:, c, dc * 128:(dc + 1) * 128], sTs[:, c, :], c == 0, c == 7, [("wgat", c), "sTs"], [PKga])
                    for c in range(8):
                        K.mm(pgb[:, :], wgat[:, c, 1024 + dc * 128:1024 + (dc + 1) * 128], sTs[:, c, :], c == 0, c == 7, [("wgat", c), "sTs"], [PKgb])
                    K.act(sa_, pga[:, :], AF.Sigmoid, [PKga], [SA])
                    K.act(sb_, pgb[:, :], AF.Sigmoid, [PKgb], [SB])
                    K.done(PKga, PKgb)
                    pbg, PKbg = K.pb()
                    pbm, PKbm = K.pb()
                    for c in range(8):
                        K.mm(pbg[:, :], wbgt[:, c, dc * 128:(dc + 1) * 128], oglaTs[:, c, :], c == 0, c == 7, [("wbgt", c), "oglaTs"], [PKbg])
                    for c in range(8):
                        K.mm(pbm[:, :], wbmt[:, c, dc * 128:(dc + 1) * 128], omlaT[:, c, c0:c0 + 512], c == 0, c == 7, [("wbmt", c), ("omlaT", blk)], [PKbm])
                    K.tt("dve", sa_, sa_, pbg[:, :], ALU.mult, [SA, PKbg], [SA])
                    K.tt("dve", sb_, sb_, pbm[:, :], ALU.mult, [SB, PKbm], [SB])
                    K.done(PKbg, PKbm)
                    K.tt("pool", mergedT[:, dc, c0:c0 + 512], sa_, sb_, ALU.add, [SA, SB], [("mergedT", blk)])
            MT = [("mergedT", q) for q in range(4)]
            S.barrier()
            A.set(OMLA_OFF)
            s2T = v3(A.B(8 * NOWN), NOWN)
            lnbc = v3(A.F(2 * D), D)
            s2Tf = [A.F(1024)] * 2
            xt = [A.F(D) for i in range(4)]
            rlog = v3(A.F(TO * 36), 36)
            rb_bc = A.F(36)
            ixb = A.F(3616)
            wrt = v3(A.F(8 * 36), 36)
            s2b = [None] * TO
            for j in range(8, TO):
                s2b[j] = A.B(D)
            assert A.off <= W4_END, A.off
            end4b = A.off
            A.set(0)
            for j in range(8):
                s2b[j] = A.B(D)
            A.set(M_END)
            rtb = A.F(2560)
            A.set(end4b)
            print("stage4b arena end", A.off)
            K.dma("sp", lnbc[:, 0, :], lng[1, 0].partition_broadcast(128), (), ["lnbc"])
            K.dma("sp", lnbc[:, 1, :], lng[1, 1].partition_broadcast(128), (), ["lnbc"])
            K.dma("sp", rb_bc, rbias.partition_broadcast(128), (), ["rb_bc"])
            K.dma("sp", wrt, wr.rearrange("(c p) n -> p c n", p=128), (), ["wrt"])
            def P4(j):
                blk = j // 4
                x_t = xt[j % 4]
                XT = ("xt", j % 4)
                K.dma("sp", x_t, s_d[j * 128:(j + 1) * 128, :], ["s_d"], [XT])
                for hh in range(2):
                    ph, PKh = K.pb()
                    for c in range(8):
                        K.mm(ph[:, :], mergedT[:, c, j * 128:(j + 1) * 128], woutt[:, c, hh * 512:(hh + 1) * 512], c == 0, c == 7, [("mergedT", blk), "woutt"], [PKh])
                    K.stt("dve", x_t[:, hh * 512:(hh + 1) * 512], x_t[:, hh * 512:(hh + 1) * 512], ALPHA, ph[:, :], ALU.mult, ALU.add, [XT, PKh], [XT])
                    K.done(PKh)

            def L4(j):
                return ln_gen(xt[j % 4], [("xt", j % 4)], st8[j % 4], ("st8", j % 4), lnbc, "lnbc", 0)

            def Q4(j):
                x_t = xt[j % 4]
                XT = ("xt", j % 4)
                K.dma("sp", s2_d[j * 128:(j + 1) * 128, :], x_t, [XT], ["s2_d"])
                if sparse:
                    K.cp("act", s2b[j], x_t, [XT], [("s2b", j)])
                for half in range(2):
                    pt, PK = K.pb()
                    for cc in range(4):
                        c = half * 4 + cc
                        K.tr(pt[:, cc * 128:(cc + 1) * 128], x_t[:, c * 128:(c + 1) * 128], ident, [XT] + C0, [PK])
                    sf = s2Tf[j % 2]
                    K.cp("dve", sf[:, half * 512:(half + 1) * 512], pt[:, :], [PK], [("s2Tf", 0, half)])
                    K.done(PK)
                    if not sparse:
                        K.cp("act", s2T[:, half * 4:half * 4 + 4, j * 128:(j + 1) * 128], v3(sf[:, half * 512:(half + 1) * 512], 128), [("s2Tf", 0, half)], [("s2T", j)])
                prt, PKrt = K.pb()
                for c in range(8):
                    K.mm(prt[:, 0:36], s2Tf[j % 2][:, c * 128:(c + 1) * 128], wrt[:, c, :], c == 0, c == 7, [("s2Tf", 0, 0), ("s2Tf", 0, 1), "wrt"], [PKrt])
                K.tt("dve", rlog[:, j, :], prt[:, 0:36], rb_bc, ALU.add, [PKrt, "rb_bc"], [("rlog", j)])
                K.done(PKrt)

            P4(0)
            P4(1)
            for j0 in range(0, TO, 2):
                if j0 + 2 < TO:
                    P4(j0 + 2)
                    P4(j0 + 3)
                interleave(L4(j0), L4(j0 + 1))
                Q4(j0)
                Q4(j0 + 1)
            RL = [("rlog", j) for j in range(TO)]
            R = ["rtb"]
            l4 = rlog[:, :, 0:4]
            le = rtb[:, 0:512].rearrange("p (a e) -> p a e", e=8)
            K.cp("dve", rtb[:, 0:512].rearrange("p (t n) -> p t n", n=32), rlog[:, :, 4:36], RL, R)
            m4 = rtb[:, 512:528]
            K.red(m4, l4, ALU.max, RL, R)
            d4 = rtb[:, 528:592].rearrange("p (t g) -> p t g", g=4)
            K.tt("dve", d4, l4, m4.unsqueeze(2).broadcast_to([128, 16, 4]), ALU.subtract, RL + R, R)
            e4 = rtb[:, 592:656].rearrange("p (t g) -> p t g", g=4)
            K.act(e4, d4, AF.Exp, R, R)
            s4 = rtb[:, 656:672]
            K.red(s4, e4, ALU.add, R, R)
            K.recip(s4, s4, R, R)
            gw = rtb[:, 672:736].rearrange("p (t g) -> p t g", g=4)
            K.ts("dve", gw, d4, 0.0, None, ALU.is_equal, None, R, R)
            K.tt("dve", gw, gw, s4.unsqueeze(2).broadcast_to([128, 16, 4]), ALU.mult, R, R)
            m1 = rtb[:, 736:800]
            K.red(m1, le, ALU.max, R, R)
            eq1 = rtb[:, 800:1312].rearrange("p (a e) -> p a e", e=8)
            K.tt("dve", eq1, le, m1.unsqueeze(2).broadcast_to([128, 64, 8]), ALU.is_equal, R, R)
            l2 = rtb[:, 1312:1824].rearrange("p (a e) -> p a e", e=8)
            K.stt("dve", l2, eq1, -1e30, le, ALU.mult, ALU.add, R, R)
            m2 = rtb[:, 1824:1888]
            K.red(m2, l2, ALU.max, R, R)
            eq2 = rtb[:, 1888:2400].rearrange("p (a e) -> p a e", e=8)
            K.tt("dve", eq2, l2, m2.unsqueeze(2).broadcast_to([128, 64, 8]), ALU.is_equal, R, R)
            w1 = rtb[:, 2400:2464]
            K.tt("dve", w1, m1, m2, ALU.subtract, R, R)
            K.act(w1, w1, AF.Sigmoid, R, R)
            w2 = rtb[:, 2464:2528]
            K.ts("dve", w2, w1, -1.0, 1.0, ALU.mult, ALU.add, R, R)
            K.tt("dve", eq1, eq1, w1.unsqueeze(2).broadcast_to([128, 64, 8]), ALU.mult, R, R)
            K.tt("dve", eq2, eq2, w2.unsqueeze(2).broadcast_to([128, 64, 8]), ALU.mult, R, R)
            K.tt("dve", eq1, eq1, eq2, ALU.add, R, R)
            K.tt("dve", comb[:].rearrange("p t (g e) -> p (t g) e", e=8), eq1, gw.rearrange("p t g -> p (t g)").unsqueeze(2).broadcast_to([128, 64, 8]),
                 ALU.mult, R, [("comb", j) for j in range(TO)])
            if sparse:
                CM_ = [("comb", j) for j in range(TO)]
                IX = ["ixb"]
                comb3 = comb[:].rearrange("p t e -> p (t e)")
                Mk = ixb[:, 0:512]
                K.ts("dve", Mk, comb3, 0.0, None, ALU.is_gt, None, CM_, IX)
                prk, PKrk = K.pb()
                pcn, PKcn = K.pb()
                K.mm(prk[:, :], cmt[:, 610:738], Mk, True, True, IX + C0, [PKrk])
                K.mm(pcn[:, :], cmt[:, 482:610], Mk, True, True, IX + C0, [PKcn])
                cnts = v3(ixb[:, 512:1024], 32)
                K.cp("dve", ixb[:, 512:1024], pcn[:, :], [PKcn], IX)
                K.done(PKcn)
                offs = v3(ixb[:, 1024:1536], 32)
                K.memset("dve", offs[:, 0, :], 0.0, IX)
                for j in range(1, TO):
                    K.tt("dve", offs[:, j, :], offs[:, j - 1, :], cnts[:, j - 1, :], ALU.add, IX, IX)
                slot = ixb[:, 1536:2048]
                K.tt("dve", slot, prk[:, :], ixb[:, 1024:1536], ALU.add, [PKrk] + IX, IX)
                K.done(PKrk)
                valid_ = ixb[:, 2048:2560]
                K.ts("dve", valid_, slot, float(CAP), None, ALU.is_lt, None, IX, IX)
                K.tt("dve", valid_, valid_, Mk, ALU.mult, IX, IX)
                K.tt("dve", v3(slot, 32), v3(slot, 32), cmt[:, 738:770].unsqueeze(1).broadcast_to([128, TO, 32]), ALU.add, IX + C0, IX)
                rowv = ixb[:, 2560:3072]
                K.stt("dve", rowv, slot, 1.0, valid_, ALU.add, ALU.mult, IX, IX)
                K.ts("dve", rowv, rowv, -1.0, None, ALU.add, None, IX, IX)
                idxf = ixb[:, 3072:3104]
                K.red(idxf[:, 0:16], v3(rowv, 32), ALU.max, IX, IX)
                eqh = ixb[:, 3104:3616]
                K.tt("dve", v3(eqh, 32), v3(rowv, 32), idxf[:, 0:16].unsqueeze(2).broadcast_to([128, TO, 32]), ALU.is_equal, IX, IX)
                K.tt("dve", eqh, eqh, valid_, ALU.mult, IX, IX)
                K.stt("dve", rowv, eqh, -1e9, rowv, ALU.mult, ALU.add, IX, IX)
                K.red(idxf[:, 16:32], v3(rowv, 32), ALU.max, IX, IX)
                K.tt("dve", eqh, eqh, comb3, ALU.mult, IX + CM_, IX)
                K.red(wts[:, 0:16], v3(eqh, 32), ALU.add, IX, ["wts"])
                eql = ixb[:, 3104:3616]
                K.tt("dve", v3(eql, 32), v3(rowv, 32), idxf[:, 16:32].unsqueeze(2).broadcast_to([128, TO, 32]), ALU.is_equal, IX, IX)
                K.tt("dve", eql, eql, valid_, ALU.mult, IX, IX)
                K.tt("dve", eql, eql, comb3, ALU.mult, IX + CM_, IX)
                K.red(wts[:, 16:32], v3(eql, 32), ALU.add, IX, ["wts"])
                K.ts("dve", idxf, idxf, -1.0, None, ALU.max, None, IX, IX)
                K.cp("dve", idxi[:], idxf, IX, ["idxi"])
                for j in range(TO):
                    for k_ in range(2):
                        col = k_ * 16 + j
                        S.op("pool", (lambda j=j, col=col: (lambda e: e.indirect_dma_start(
                            out=G_d, out_offset=bass.IndirectOffsetOnAxis(ap=idxi[:, col:col + 1], axis=0), in_=s2b[j], in_offset=None,
                            bounds_check=bcreg(e), oob_is_err=False)))(), [("s2b", j), "idxi"], ["G_d"], dma=True)
            CMB = [("comb", j) for j in range(TO)]
            if debug:
                finals.append(K.dma("sp", dbg["comb"], comb[:], CMB, []))
        if stages >= 5 and not sparse:
            S.barrier()
            S.mute = only is not None and 5 not in only
            A.set(0)
            ewb = []
            for i in range(2):
                ewb.append((v3(A.B(2048), 256), v3(A.B(2048), 256), v3(A.B(2048), 1024)))
            lnbc = v3(A.F(2 * D), D)
            assert A.off <= OMLA_OFF
            A.set(OMLA_END)
            yacc = v3(A.F(TO * D), D)
            hidT = [v3(A.B(2 * NOWN), NOWN) for i in range(2)]
            silb = [A.B(512) for i in range(2)]
            print("stage5 arena end", A.off)
            S2K = [("s2T", j) for j in range(TO)]
            K.dma("sp", lnbc[:, 0, :], lng[2, 0].partition_broadcast(128), (), ["lnbc"])
            K.dma("sp", lnbc[:, 1, :], lng[2, 1].partition_broadcast(128), (), ["lnbc"])
            for j in range(TO):
                K.dma("sp", yacc[:, j, :], s2_d[j * 128:(j + 1) * 128, :], ["s2_d"], [("yacc", j, 0), ("yacc", j, 1)])
                K.act(yacc[:, j, :], yacc[:, j, :], AF.Copy, [("yacc", j, 0), ("yacc", j, 1)], [("yacc", j, 0), ("yacc", j, 1)], scale=ALPHA)
            for e_ in range(32):
                g_, u_, d_ = ewb[e_ % 2]
                EW = ("ew", e_ % 2)
                K.dma("pool", g_, ewg[e_].rearrange("(c p) n -> p c n", p=128), (), [EW])
                K.dma("pool", u_, ewu[e_].rearrange("(c p) n -> p c n", p=128), (), [EW])
                K.dma("pool", d_, ewd[e_].rearrange("(c p) n -> p c n", p=128), (), [EW])
                hT = hidT[e_ % 2]
                HT = ("hidT", e_ % 2)
                for fc in range(2):
                    for blk in range(4):
                        c0 = blk * 512
                        pg, PKg = K.pb()
                        pu, PKu = K.pb()
                        for c in range(8):
                            K.mm(pg[:, :], g_[:, c, fc * 128:(fc + 1) * 128], s2T[:, c, c0:c0 + 512], c == 0, c == 7, [EW] + S2K, [PKg])
                        for c in range(8):
                            K.mm(pu[:, :], u_[:, c, fc * 128:(fc + 1) * 128], s2T[:, c, c0:c0 + 512], c == 0, c == 7, [EW] + S2K, [PKu])
                        sl = silb[(fc * 4 + blk) % 2]
                        SL = ("silb", (fc * 4 + blk) % 2)
                        K.act(sl, pg[:, :], AF.Silu, [PKg], [SL])
                        K.tt("dve", hT[:, fc, c0:c0 + 512], sl, pu[:, :], ALU.mult, [SL, PKu], [HT])
                        K.done(PKg, PKu)
                for j in range(TO):
                    for half in range(2):
                        py, PKy = K.pb()
                        for fc in range(2):
                            K.mm(py[:, :], hT[:, fc, j * 128:(j + 1) * 128], d_[:, fc, half * 512:(half + 1) * 512], fc == 0, fc == 1, [HT, EW], [PKy])
                        ya = yacc[:, j, half * 512:(half + 1) * 512]
                        YK = ("yacc", j, half)
                        K.stt("dve", ya, py[:, :], comb[:, j, e_:e_ + 1], ya, ALU.mult, ALU.add, [PKy, ("comb", j), YK], [YK])
                        K.done(PKy)
            for j0 in range(0, TO, 2):
                gens = [ln_gen(yacc[:, j, :], [("yacc", j, 0), ("yacc", j, 1)], st8[j % 4], ("st8", j % 4), lnbc, "lnbc", 0) for j in (j0, j0 + 1)]
                alive = [True, True]
                while any(alive):
                    for k_, g_ in enumerate(gens):
                        if alive[k_]:
                            try:
                                next(g_)
                            except StopIteration:
                                alive[k_] = False
                for j in (j0, j0 + 1):
                    finals.append(K.dma("sp", out[j * 128:(j + 1) * 128, :], yacc[:, j, :], [("yacc", j, 0), ("yacc", j, 1)], []))
        if stages >= 5 and sparse:
            S.barrier()
            S.mute = only is not None and 5 not in only
            A.set(0)
            NW = 4
            ewb = []
            for i in range(NW):
                ewb.append((v3(A.B(2048), 256), v3(A.B(2048), 256), v3(A.B(2048), 1024)))
            lnbc = v3(A.F(2 * D), D)
            NST = CAP // 128
            gt = [v3(A.B(NST * D), D) for i in range(2)]
            gT = [v3(A.B(8 * CAP), CAP) for i in range(2)]
            hidT = [v3(A.B(2 * CAP), CAP) for i in range(2)]
            silb = [A.B(CAP) for i in range(2)]
            ysb = [v3(A.B(NST * D), D) for i in range(2)]
            NG = 8
            gth = [A.B(D) for i in range(NG)]
            xs2 = [A.F(D) for i in range(4)]
            print("stage5 sparse arena end", A.off)
            K.dma("sp", lnbc[:, 0, :], lng[2, 0].partition_broadcast(128), (), ["lnbc"])
            K.dma("sp", lnbc[:, 1, :], lng[2, 1].partition_broadcast(128), (), ["lnbc"])
            for i in range(NG):
                K.memset("pool", gth[i], 0.0, [("gth", i)])

            def wload(e_):
                g_, u_, d_ = ewb[e_ % NW]
                EW = ("ew", e_ % NW)
                K.dma("sp", g_, ewg_b[e_].rearrange("(c p) n -> p c n", p=128), (), [EW])
                K.dma("sp", u_, ewu_b[e_].rearrange("(c p) n -> p c n", p=128), (), [EW])
                K.dma("sp", d_, ewd_b[e_].rearrange("(c p) n -> p c n", p=128), (), [EW])

            for e_ in range(NW - 1):
                wload(e_)
            ncp = [0]

            def gload(e_):
                K.dma("sp", gt[e_ % 2], G_d[e_ * CAP:(e_ + 1) * CAP, :].rearrange("(s p) n -> p s n", p=128), ["G_d"], [("gt", e_ % 2)])

            def tpose(e_):
                g_t = gt[e_ % 2]
                GTK = ("gt", e_ % 2)
                gTe = gT[e_ % 2]
                GTT = ("gT", e_ % 2)
                for s_ in range(NST):
                    pt, PK = K.pb()
                    ptb = pt[:, :].bitcast(BF16)
                    for c in range(8):
                        K.tr(ptb[:, c * 128:(c + 1) * 128], g_t[:, s_, c * 128:(c + 1) * 128], cmb[:, 0:128], [GTK] + C0, [PK])
                    K.cp("dve" if ncp[0] % 2 == 0 else "act", gTe[:, :, s_ * 128:(s_ + 1) * 128], v3(ptb, 128), [PK], [GTT])
                    ncp[0] += 1
                    K.done(PK)

            def gate_up(e_):
                g_, u_, d_ = ewb[e_ % NW]
                EW = ("ew", e_ % NW)
                gTe = gT[e_ % 2]
                GTT = ("gT", e_ % 2)
                hT = hidT[e_ % 2]
                HT = ("hidT", e_ % 2)
                for fc in range(2):
                    pg, PKg = K.pb()
                    pu, PKu = K.pb()
                    for c in range(8):
                        K.mm(pg[:, 0:CAP], g_[:, c, fc * 128:(fc + 1) * 128], gTe[:, c, :], c == 0, c == 7, [EW, GTT], [PKg])
                    for c in range(8):
                        K.mm(pu[:, 0:CAP], u_[:, c, fc * 128:(fc + 1) * 128], gTe[:, c, :], c == 0, c == 7, [EW, GTT], [PKu])
                    sl = silb[fc]
                    SL = ("silb", fc)
                    K.act(sl, pg[:, 0:CAP], AF.Silu, [PKg], [SL])
                    K.tt("dve", hT[:, fc, :], sl, pu[:, 0:CAP], ALU.mult, [SL, PKu], [HT])
                    K.done(PKg, PKu)

            def down(e_):
                g_, u_, d_ = ewb[e_ % NW]
                EW = ("ew", e_ % NW)
                hT = hidT[e_ % 2]
                HT = ("hidT", e_ % 2)
                y_s = ysb[e_ % 2]
                YS = ("ysb", e_ % 2)
                for s_ in range(NST):
                    for half in range(2):
                        py, PKy = K.pb()
                        for fc in range(2):
                            K.mm(py[:, :], hT[:, fc, s_ * 128:(s_ + 1) * 128], d_[:, fc, half * 512:(half + 1) * 512], fc == 0, fc == 1, [HT, EW], [PKy])
                        K.cp("act" if ncp[0] % 2 == 0 else "dve", y_s[:, s_, half * 512:(half + 1) * 512], py[:, :], [PKy], [YS])
                        ncp[0] += 1
                        K.done(PKy)
                K.dma("sp", Y_d[e_ * CAP:(e_ + 1) * CAP, :].rearrange("(s p) n -> p s n", p=128), y_s, [YS], ["Y_d"])

            gload(0)
            gload(1)
            tpose(0)
            for e_ in range(32):
                gate_up(e_)
                if e_ + 1 < 32:
                    tpose(e_ + 1)
                if e_ + 2 < 32:
                    gload(e_ + 2)
                if e_ + NW - 1 < 32:
                    wload(e_ + NW - 1)
                down(e_)

            def fetch(j):
                x_t = xs2[j % 4]
                XT = ("xs2", j % 4)
                K.dma("sp", x_t, s2_d[j * 128:(j + 1) * 128, :], ["s2_d"], [XT])
                for k_ in range(2):
                    col = k_ * 16 + j
                    gi_ = (j * 2 + k_) % NG
                    gb = gth[gi_]
                    GK = ("gth", gi_)
                    S.op("pool", (lambda gb=gb, col=col: (lambda e: e.indirect_dma_start(
                        out=gb, out_offset=None, in_=Y_d, in_offset=bass.IndirectOffsetOnAxis(ap=idxi[:, col:col + 1], axis=0),
                        bounds_check=bcreg(e), oob_is_err=False)))(), ["Y_d", "idxi", GK], [GK], dma=True)

            def combine(j):
                x_t = xs2[j % 4]
                XT = ("xs2", j % 4)
                K.act(x_t, x_t, AF.Copy, [XT], [XT], scale=ALPHA)
                yield
                for k_ in range(2):
                    col = k_ * 16 + j
                    gi_ = (j * 2 + k_) % NG
                    K.stt("dve", x_t, gth[gi_], wts[:, col:col + 1], x_t, ALU.mult, ALU.add, [("gth", gi_), XT, "wts"], [XT])
                    yield
                yield from ln_gen(x_t, [XT], st8[j % 4], ("st8", j % 4), lnbc, "lnbc", 0)
                finals.append(K.dma("sp", out[j * 128:(j + 1) * 128, :], x_t, [XT], []))

            for j in range(4):
                fetch(j)
            for j0 in range(0, TO, 2):
                interleave(combine(j0), combine(j0 + 1))
                for j in (j0 + 4, j0 + 5):
                    if j < TO:
                        fetch(j)
        finals = [f_ for f_ in finals if f_.idx is not None and f_.idx < len(S.ops) and S.ops[f_.idx] is f_] + [o for o in S.ops if o.is_dma and o.eng == "sp"][-NDSEM:]
        counts = S.emit(finals)
        print("op counts", counts)
    return nc, counts


def _const_mats():
    cm = np.zeros((128, 776), np.float32)
    cm[:, 0:128] = np.eye(128, dtype=np.float32)
    p = np.arange(128)
    tri = ((p[:, None] // 64 == p[None, :] // 64) & (p[:, None] <= p[None, :])).astype(np.float32)
    cm[:, 128:256] = tri
    cm[:, 256:384] = (p[:, None] <= p[None, :]).astype(np.float32)
    cm[0:64, 384] = 1.0
    cm[64:128, 385] = 1.0
    for i in range(32):
        cm[i, 386 + 64 + i] = 1.0
    cm[:, 482:610] = 1.0
    cm[:, 610:738] = (p[:, None] < p[None, :]).astype(np.float32)
    cm[:, 738:770] = (np.arange(32) * CAP)[None, :]
    cv = np.zeros((128, 16), np.float32)
    cv[:, 0] = 1.0
    cv[:, 1] = LN_EPS
    cv[:, 2] = RMS_EPS
    return cm, cv


def _core_inputs(x, meta_tokens, half):
    xin = np.zeros((NPOS, D), np.float32)
    valid = np.zeros((NPOS,), np.float32)
    pos = np.zeros((NPOS,), np.float64)
    if half == 0:
        xin[NPREV - 16:NPREV] = meta_tokens
        valid[NPREV - 16:] = 1.0
        pos[NPREV - 16:NPREV] = np.arange(16)
        xin[NPREV:] = x[0:2048]
        pos[NPREV:] = 16 + np.arange(2048)
    else:
        m0 = NPREV - 2048 - 16
        xin[m0:m0 + 16] = meta_tokens
        xin[m0 + 16:NPREV] = x[0:2048]
        valid[m0:] = 1.0
        pos[m0:m0 + 16] = np.arange(16)
        pos[m0 + 16:NPREV] = 16 + np.arange(2048)
        xin[NPREV:] = x[2048:4096]
        pos[NPREV:] = 16 + 2048 + np.arange(2048)
    vk = np.zeros((128, 2 * TT), np.float32)
    vk[:, 0:TT] = valid.reshape(TT, 128).T
    vk[:, TT:] = ((valid - 1.0) * (-NEG)).reshape(TT, 128).T
    inv_freq = (10000.0 ** (-np.arange(0, 32, 2, dtype=np.float32) / 32)).astype(np.float32)
    ang = (pos.astype(np.float32)[:, None] * inv_freq[None, :]).astype(np.float32)
    cos = np.cos(ang).T.astype(np.float32)
    sin = np.sin(ang).T.astype(np.float32)
    cskv = np.zeros((32, 2, NPOS), np.float32)
    cskv[0:16, 0] = cos
    cskv[16:32, 0] = cos
    cskv[0:16, 1] = -sin
    cskv[16:32, 1] = sin
    csq = np.zeros((96, 2, NOWN), np.float32)
    csq[0:64, 0] = 1.0
    csq[64:96, 0] = cskv[:, 0, NPREV:]
    csq[64:96, 1] = cskv[:, 1, NPREV:]
    return xin, vk, cskv, csq


def _shared_inputs(inp):
    f = lambda a: np.ascontiguousarray(np.asarray(a, dtype=np.float32))
    cm, cv = _const_mats()
    w_in = f(inp["w_in"][0])
    kr0 = 512 + 512 + 1024 + 1024 + 16 + 384 + 256
    wkr2 = np.zeros((D, 2, 32), np.float32)
    wkr2[:, 0] = w_in[:, kr0:kr0 + 32]
    wkr2[:, 1, 0:16] = w_in[:, kr0 + 16:kr0 + 32]
    wkr2[:, 1, 16:32] = w_in[:, kr0:kr0 + 16]
    w2a = np.zeros((33, 512), np.float32)
    w2a[0:16] = f(inp["gla_gate_w2"][0])
    w2a[32] = f(inp["gla_gate_b"][0])
    wuq = f(inp["mla_w_uq"][0]).reshape(384, 16, 96)
    wuq2 = np.zeros((384, 16, 2, 96), np.float32)
    wuq2[:, :, 0] = wuq
    wuq2[:, :, 1, 64:80] = wuq[:, :, 80:96]
    wuq2[:, :, 1, 80:96] = wuq[:, :, 64:80]
    wukp = np.zeros((256, 16, 96), np.float32)
    wukp[:, :, 0:64] = f(inp["mla_w_uk"][0]).reshape(256, 16, 64)
    lng = np.stack([np.stack([f(inp["ln_emb_g"]), f(inp["ln_emb_b"])]),
                    np.stack([f(inp["ln_mix_g"][0]), f(inp["ln_mix_b"][0])]),
                    np.stack([f(inp["ln_ffn_g"][0]), f(inp["ln_ffn_b"][0])])])
    gcols = np.zeros((128, 8), np.float32)
    gcols[:, 0:2] = f(inp["gla_norm_g"][0]).reshape(2, 128).T
    gcols[:, 2:5] = f(inp["mla_q_norm_g"][0]).reshape(3, 128).T
    gcols[:, 5:7] = f(inp["mla_kv_norm_g"][0]).reshape(2, 128).T
    sh = {
        "cm": cm, "cv": cv, "lng": f(lng), "w_in": w_in, "wkr2": wkr2, "w2a": w2a, "gcols": gcols,
        "wuq2": wuq2, "wukp": wukp, "wuv": f(inp["mla_w_uv"][0]),
        "wbg": f(inp["w_branch_gla"][0]), "wbm": f(inp["w_branch_mla"][0]), "wout": f(inp["w_out"][0]),
        "wr": f(np.concatenate([inp["router_group_w"][0], inp["router_expert_w"][0]], axis=1)),
        "rbias": f(np.concatenate([inp["router_group_b"][0], inp["router_expert_b"][0]], axis=0)),
        "ewg": f(np.asarray(inp["expert_w_gate"][0]).reshape(32, D, 256)),
        "ewu": f(np.asarray(inp["expert_w_up"][0]).reshape(32, D, 256)),
        "ewd": f(np.asarray(inp["expert_w_down"][0]).reshape(32, 256, D)),
    }
    return sh


_NC_CACHE = {}


def kernel(**inputs):
    inp = {k: np.asarray(v) for k, v in inputs.items()}
    x = inp["x"].astype(np.float32)
    meta = inp["meta_tokens"].astype(np.float32)
    sh = _shared_inputs(inp)
    in_maps = []
    for c in range(8):
        b, half = c // 2, c % 2
        xin, vk, cskv, csq = _core_inputs(x[b], meta, half)
        m = dict(sh)
        m.update({"xin": xin, "vk": vk, "cskv": cskv, "csq": csq})
        in_maps.append(m)
    if "nc" not in _NC_CACHE:
        _NC_CACHE["nc"] = build(False)[0]
    res = run_bass_kernel_spmd(_NC_CACHE["nc"], in_maps, core_ids=list(range(8)))
    out = np.zeros((4, 4096, D), np.float32)
    for c in range(8):
        b, half = c // 2, c % 2
        out[b, half * 2048:(half + 1) * 2048] = res.results[c]["out"]
    return out
```

```python
import contextlib
import numpy as np
import concourse.bass as bass
import concourse.mybir as mybir
from concourse.bass_utils import run_bass_kernel_spmd

F32 = mybir.dt.float32
BF16 = mybir.dt.bfloat16
AF = mybir.ActivationFunctionType
ALU = mybir.AluOpType
AX = mybir.AxisListType

NDSEM = 8
D = 1024
NPREV = 2176
NOWN = 2048
NPOS = NPREV + NOWN
TP = NPREV // 128
TO = NOWN // 128
TT = TP + TO
W_RES = 3760
ALPHA = 2.0 ** 0.25
LN_EPS = 1e-5
RMS_EPS = 1e-6
NEG = -30000.0
CAP = 256


class Op:
    __slots__ = ("eng", "fn", "waits", "signal", "idx", "sig_no", "dsem", "dtarget", "is_dma", "prewait")

    def __init__(self, eng, fn, is_dma):
        self.eng = eng
        self.fn = fn
        self.waits = []
        self.signal = False
        self.sig_no = None
        self.is_dma = is_dma
        self.dsem = None
        self.dtarget = None
        self.prewait = None
        self.idx = None


class Sched:
    ENGS = ("pe", "act", "dve", "pool", "sp")

    def __init__(self, nc):
        self.nc = nc
        self.ops = []
        self.last_w = {}
        self.readers = {}
        self.bar = None
        self.bar_passed = set()
        self.last_op = {}
        self.dma_recent = {}
        self.mute = False

    def barrier(self):
        b = [o for o in self.last_op.values()]
        for lst in self.dma_recent.values():
            b.extend(lst)
        self.bar = b
        self.bar_passed = set()
        self.last_w = {}
        self.readers = {}

    def op(self, eng, fn, reads=(), writes=(), dma=False):
        o = Op(eng, fn, dma)
        if self.mute:
            return o
        o.idx = len(self.ops)
        deps = set()
        for r in reads:
            w = self.last_w.get(r)
            if w is not None:
                deps.add(w)
            if isinstance(r, tuple) and r[0] == "pb":
                for rd in self.readers.get(r, ()):
                    if rd.eng != eng:
                        deps.add(rd)
        for r in writes:
            w = self.last_w.get(r)
            if w is not None:
                deps.add(w)
            for rd in self.readers.get(r, ()):
                deps.add(rd)
        for d in deps:
            if d.eng == eng and not d.is_dma and not dma:
                if eng == "pe":
                    continue
            o.waits.append(d)
            d.signal = True
        if self.bar is not None and eng not in self.bar_passed:
            self.bar_passed.add(eng)
            for d in self.bar:
                if d not in o.waits:
                    o.waits.append(d)
                    d.signal = True
        for r in reads:
            self.readers.setdefault(r, []).append(o)
        for r in writes:
            self.last_w[r] = o
            self.readers[r] = []
        self.ops.append(o)
        if dma:
            lst = self.dma_recent.setdefault(eng, [])
            lst.append(o)
            if len(lst) > NDSEM:
                lst.pop(0)
        else:
            self.last_op[eng] = o
        return o

    def emit(self, final_ops):
        nc = self.nc
        for fo in final_ops:
            fo.signal = True
        cnt = {e: 0 for e in self.ENGS}
        dcnt = {}
        dma_i = {e: 0 for e in self.ENGS}
        dma_hist = {}
        for o in self.ops:
            if o.is_dma:
                i = dma_i[o.eng]
                dma_i[o.eng] += 1
                slot = (o.eng, i % NDSEM)
                o.prewait = dma_hist.get(slot)
                dcnt[slot] = dcnt.get(slot, 0) + 16
                o.dsem = slot
                o.dtarget = dcnt[slot]
                dma_hist[slot] = o
            elif o.signal:
                cnt[o.eng] += 1
                o.sig_no = cnt[o.eng]
        with contextlib.ExitStack() as st:
            esem = {e: st.enter_context(nc.semaphore("s_" + e)) for e in ("pe", "act", "dve", "pool")}
            dsem = {}
            for e in ("sp", "pool", "act"):
                if dma_i[e] > 0:
                    for j in range(NDSEM):
                        dsem[(e, j)] = st.enter_context(nc.semaphore("d_%s%d" % (e, j)))
            block = st.enter_context(nc.Block())
            per = {e: [o for o in self.ops if o.eng == e] for e in self.ENGS}

            def run(ename, eng):
                waited = {}

                def wait_for(p):
                    if p.is_dma:
                        key, val, sem = p.dsem, p.dtarget, dsem[p.dsem]
                    else:
                        key, val, sem = p.eng, p.sig_no, esem[p.eng]
                    if waited.get(key, 0) >= val:
                        return
                    waited[key] = val
                    eng.wait_ge(sem, val)

                for o in per[ename]:
                    if o.is_dma and o.prewait is not None:
                        wait_for(o.prewait)
                    for p in o.waits:
                        wait_for(p)
                    ins = o.fn(eng)
                    if o.is_dma:
                        ins.then_inc(dsem[o.dsem], 16)
                    elif o.signal:
                        ins.then_inc(esem[o.eng], 1)
                if ename == "sp":
                    for fo in final_ops:
                        wait_for(fo)

            @block.tensor
            def _(eng):
                run("pe", eng)

            @block.scalar
            def _(eng):
                run("act", eng)

            @block.vector
            def _(eng):
                run("dve", eng)

            @block.gpsimd
            def _(eng):
                run("pool", eng)

            @block.sync
            def _(eng):
                run("sp", eng)
        return {e: len(per[e]) for e in self.ENGS}


class KB:
    def __init__(self, nc, st):
        self.nc = nc
        self.st = st
        self.S = Sched(nc)
        self.nps = 0
        self.banks = []
        self.uid = 0

    def sb(self, name, shape, dt):
        return self.st.enter_context(self.nc.sbuf_tensor(name, shape, dt))

    def init_psum(self):
        for i in range(8):
            self.banks.append(self.st.enter_context(self.nc.psum_tensor("pb%d" % i, [128, 512], F32)))
        self.open = [False] * 8

    def pb(self):
        for _ in range(8):
            i = self.nps % 8
            self.nps += 1
            if not self.open[i]:
                self.open[i] = True
                return self.banks[i], ("pb", i)
        raise RuntimeError("all PSUM banks open")

    def done(self, *keys):
        for k in keys:
            assert self.open[k[1]], k
            self.open[k[1]] = False

    def mm(self, out, lhsT, rhs, start, stop, r, w, skip=False):
        if skip:
            return self.S.op("pe", lambda e: e.matmul(out, lhsT=lhsT, rhs=rhs, start=start, stop=stop, skip_group_check=True), r, w)
        return self.S.op("pe", lambda e: e.matmul(out, lhsT=lhsT, rhs=rhs, start=start, stop=stop), r, w)

    def tr(self, out, in_, ident, r, w):
        return self.S.op("pe", lambda e: e.transpose(out, in_, ident), r, w)

    def act(self, out, in_, func, r, w, bias=None, scale=None, accum=None):
        kw = {}
        if bias is not None:
            kw["bias"] = bias
        if scale is not None:
            kw["scale"] = scale
        if accum is not None:
            kw["accum_out"] = accum
        return self.S.op("act", lambda e: e.activation(out=out, in_=in_, func=func, **kw), r, w)

    def tt(self, eng, out, in0, in1, op, r, w):
        return self.S.op(eng, lambda e: e.tensor_tensor(out=out, in0=in0, in1=in1, op=op), r, w)

    def ts(self, eng, out, in0, s1, s2, op0, op1, r, w, accum=None):
        if op1 is None:
            return self.S.op(eng, lambda e: e.tensor_scalar(out=out, in0=in0, scalar1=s1, scalar2=None, op0=op0), r, w)
        if accum is not None:
            return self.S.op(eng, lambda e: e.tensor_scalar(out=out, in0=in0, scalar1=s1, scalar2=s2, op0=op0, op1=op1, accum_out=accum), r, w)
        return self.S.op(eng, lambda e: e.tensor_scalar(out=out, in0=in0, scalar1=s1, scalar2=s2, op0=op0, op1=op1), r, w)

    def stt(self, eng, out, in0, scalar, in1, op0, op1, r, w):
        return self.S.op(eng, lambda e: e.scalar_tensor_tensor(out=out, in0=in0, scalar=scalar, in1=in1, op0=op0, op1=op1), r, w)

    def cp(self, eng, out, in_, r, w):
        if eng == "act":
            return self.S.op("act", lambda e: e.activation(out=out, in_=in_, func=AF.Copy), r, w)
        return self.S.op(eng, lambda e: e.tensor_copy(out=out, in_=in_), r, w)

    def recip(self, out, in_, r, w):
        return self.S.op("dve", lambda e: e.reciprocal(out=out, in_=in_), r, w)

    def red(self, out, in_, op, r, w):
        return self.S.op("dve", lambda e: e.tensor_reduce(out=out, in_=in_, axis=AX.X, op=op), r, w)

    def memset(self, eng, ap, val, w):
        return self.S.op(eng, lambda e: e.memset(ap, val), (), w)

    def dma(self, q, out, in_, r, w):
        return self.S.op(q, lambda e: e.dma_start(out=out, in_=in_), r, w, dma=True)


ARENA_BYTES = 176 * 1024


class Arena:
    def __init__(self, K):
        self.f = K.sb("arena", [128, ARENA_BYTES // 4], F32)
        self.fa = self.f[:]
        self.ba = self.f[:].bitcast(BF16)
        self.off = 0

    def set(self, off):
        self.off = off

    def F(self, n, parts=128):
        assert self.off % 4 == 0
        o = self.off // 4
        self.off += n * 4
        assert self.off <= ARENA_BYTES, self.off
        return self.fa[0:parts, o:o + n]

    def B(self, n, parts=128):
        assert self.off % 4 == 0
        o = self.off // 2
        self.off += ((n * 2 + 3) // 4) * 4
        assert self.off <= ARENA_BYTES, self.off
        return self.ba[0:parts, o:o + n]


def build(debug=False, stages=5, only=None, flags=()):
    nc = bass.Bass("TRN2", target_bir_lowering=False)

    def din(name, shape):
        return nc.dram_tensor(name, list(shape), F32, kind="ExternalInput").ap()

    xin = din("xin", [NPOS, D])
    vk = din("vk", [128, 2 * TT])
    cskv = din("cskv", [32, 2, NPOS])
    csq = din("csq", [96, 2, NOWN])
    cm = din("cm", [128, 776])
    cv = din("cv", [128, 16])
    lng = din("lng", [3, 2, D])
    w_in = din("w_in", [D, 5808])
    wkr2 = din("wkr2", [D, 2, 32])
    w2a = din("w2a", [33, 512])
    gcols = din("gcols", [128, 8])
    wuq2 = din("wuq2", [384, 16, 2, 96])
    wukp = din("wukp", [256, 16, 96])
    wuv = din("wuv", [256, 1024])
    wbg = din("wbg", [1024, 1024])
    wbm = din("wbm", [1024, 1024])
    wout = din("wout", [1024, 1024])
    wr = din("wr", [D, 36])
    rbias = din("rbias", [36])
    ewg = din("ewg", [32, D, 256])
    ewu = din("ewu", [32, D, 256])
    ewd = din("ewd", [32, 256, D])
    out = nc.dram_tensor("out", [NOWN, D], F32, kind="ExternalOutput").ap()
    okind = "ExternalOutput" if debug else "Internal"
    oglaT_d = nc.dram_tensor("oglaT_d", [128, 8, NOWN], BF16, kind=okind).ap()
    sT_d = nc.dram_tensor("sT_d", [128, 8, NOWN], BF16, kind=okind).ap()
    s_d = nc.dram_tensor("s_d", [NOWN, D], F32, kind=okind).ap()
    s2_d = nc.dram_tensor("s2_d", [NOWN, D], F32, kind=okind).ap()
    G_d = nc.dram_tensor("G_d", [32 * CAP, D], BF16, kind="Internal").ap()
    Y_d = nc.dram_tensor("Y_d", [32 * CAP, D], BF16, kind="Internal").ap()
    ewg_b = nc.dram_tensor("ewg_b", [32, D, 256], BF16, kind="Internal").ap()
    ewu_b = nc.dram_tensor("ewu_b", [32, D, 256], BF16, kind="Internal").ap()
    ewd_b = nc.dram_tensor("ewd_b", [32, 256, D], BF16, kind="Internal").ap()
    sparse = "dense" not in flags
    bc_cache = {}

    def bcreg(e):
        if "r" not in bc_cache:
            bc_cache["r"] = e.to_reg(32 * CAP - 1)
        return bc_cache["r"]
    dbg = {}
    if debug:
        dbg["omlaT"] = nc.dram_tensor("omlaT_d", [128, 8, NOWN], BF16, kind="ExternalOutput").ap()
        dbg["comb"] = nc.dram_tensor("comb_d", [128, TO, 32], F32, kind="ExternalOutput").ap()
        dbg["ckvnT"] = nc.dram_tensor("ckvnT_d", [128, 2, NPOS], BF16, kind="ExternalOutput").ap()
        dbg["kropeT"] = nc.dram_tensor("kropeT_d", [32, NPOS], BF16, kind="ExternalOutput").ap()
        dbg["cqnT"] = nc.dram_tensor("cqnT_d", [128, 3, NOWN], BF16, kind="ExternalOutput").ap()

    with contextlib.ExitStack() as st:
        K = KB(nc, st)
        S = K.S
        sb = K.sb
        K.init_psum()
        finals = []
        cmt = sb("cmt", [128, 776], F32)
        cvt = sb("cvt", [128, 16], F32)
        vkt = sb("vkt", [128, 2 * TT], F32)
        cmb = sb("cmb", [128, 640], BF16)
        gcol = sb("gcol", [128, 8], F32)
        st8 = [sb("st8_%d" % i, [128, 8], F32) for i in range(4)]
        comb = sb("comb", [128, TO, 32], F32)
        idxi = sb("idxi", [128, 32], mybir.dt.int32)
        wts = sb("wts", [128, 32], F32)
        zt = sb("zt", [128, 2048], BF16)
        junk = sb("junk", [128, D], BF16)
        junk2 = [junk, sb("junkb", [128, D], BF16)]
        K.dma("sp", cmt[:], cm, (), ["cmt"])
        K.dma("sp", cvt[:], cv, (), ["cvt"])
        K.dma("sp", vkt[:], vk, (), ["vkt"])
        K.dma("sp", gcol[:], gcols, (), ["gcol"])
        K.cp("dve", cmb[:], cmt[:, 0:640], ["cmt"], ["cmb"])
        K.memset("pool", zt[:], 0.0, ["zt"])
        ident = cmt[:, 0:128]
        triT = cmt[:, 128:256]
        chsel = cmt[:, 384:386]
        ones_b = cmb[:, 482:610]
        ut_b = cmb[:, 256:384]
        tri_b = cmb[:, 128:256]
        selr_b = cmb[0:32, 386:482]
        c_one = cvt[:, 0:1]
        c_lneps = cvt[:, 1:2]
        c_rmseps = cvt[:, 2:3]
        C0 = ["cmt", "cvt", "vkt", "cmb", "gcol"]
        A = Arena(K)

        def v3(ap, n):
            return ap.rearrange("p (c n) -> p c n", n=n)

        def ln_tok(x_t, XT, stt_, ST, lnbc, LK, gi):
            K.act(junk[:], x_t, AF.Identity, [XT], ["junk", ST], accum=stt_[:, 0:1])
            K.act(junk[:], x_t, AF.Square, [XT], ["junk", ST], accum=stt_[:, 1:2])
            K.ts("dve", stt_[:, 2:3], stt_[:, 0:1], 1.0 / D, None, ALU.mult, None, [ST], [ST])
            K.tt("dve", stt_[:, 3:4], stt_[:, 2:3], stt_[:, 2:3], ALU.mult, [ST], [ST])
            K.stt("dve", stt_[:, 4:5], stt_[:, 1:2], 1.0 / D, stt_[:, 3:4], ALU.mult, ALU.subtract, [ST], [ST])
            K.act(stt_[:, 5:6], stt_[:, 4:5], AF.Sqrt, [ST] + C0, [ST], bias=c_lneps, scale=1.0)
            K.recip(stt_[:, 5:6], stt_[:, 5:6], [ST], [ST])
            K.stt("dve", stt_[:, 6:7], stt_[:, 2:3], -1.0, stt_[:, 5:6], ALU.mult, ALU.mult, [ST], [ST])
            K.act(x_t, x_t, AF.Identity, [XT, ST], [XT], bias=stt_[:, 6:7], scale=stt_[:, 5:6])
            K.tt("dve", x_t, x_t, lnbc[:, gi, :], ALU.mult, [XT, LK], [XT])
            K.tt("dve", x_t, x_t, lnbc[:, gi + 1, :], ALU.add, [XT, LK], [XT])

        A.set(0)
        ckvnT = v3(A.B(2 * NPOS), NPOS)
        cqnT = v3(A.B(3 * NOWN), NOWN)
        kropeT = A.B(NPOS, parts=32)
        P1_END = A.off
        OMLA_OFF = P1_END

        S.mute = only is not None and 1 not in only
        wres = v3(A.B(8 * W_RES), W_RES)
        wkr = v3(A.B(8 * 64), 64)
        sTw = [v3(A.B(1024), 128) for i in range(2)]
        ktb = [A.B(512) for i in range(2)]
        vb = [A.B(1024) for i in range(2)]
        q0T = [v3(A.B(512), 128) for i in range(2)]
        q1T = [v3(A.B(512), 128) for i in range(2)]
        ktT = v3(A.B(512), 128)
        attm = [v3(A.B(512), 128) for i in range(2)]
        Sbf = [[A.B(256) for i in range(3)] for h in range(4)]
        sqb = A.B(1024)
        oglaT = [v3(A.B(1024), 128) for i in range(2)]
        sqm = A.B(640)
        S32 = [A.F(256) for h in range(4)]
        loga = A.F(512)
        e2 = A.F(512)
        e1T = A.F(512)
        e2T = A.F(512)
        silur2 = [A.F(1024) for i in range(2)]
        rstdg = A.F(512)
        aaug = A.F(128, parts=33)
        w2t = A.F(512, parts=33)
        dec2 = [A.F(8) for i in range(2)]
        cs32 = [v3(A.F(256, parts=32), 128) for i in range(2)]
        rsm = A.F(128)
        tmpS = [A.F(256) for h in range(4)]
        xt = [A.F(D) for i in range(2)]
        lnbc = v3(A.F(2 * D), D)
        print("stage1 arena end", A.off)

        K.dma("sp", lnbc[:, 0, :], lng[0, 0].partition_broadcast(128), (), ["lnbc"])
        K.dma("sp", lnbc[:, 1, :], lng[0, 1].partition_broadcast(128), (), ["lnbc"])
        w_in_v = w_in.rearrange("(c p) n -> p c n", p=128)
        for c in range(8):
            K.dma("pool", wres[:, c, :], w_in_v[:, c, 0:W_RES], (), [("wres", c)])
        K.dma("pool", wkr, wkr2.rearrange("(c p) a n -> p c (a n)", p=128), (), ["wkr"])
        K.dma("sp", w2t, w2a, (), ["w2t"])
        K.memset("dve", aaug, 0.0, ["aaug"])
        K.memset("dve", aaug[32:33, :], 1.0, ["aaug"])
        for h in range(4):
            K.memset("pool", S32[h], 0.0, [("S32", h)])
            K.memset("pool", Sbf[h][0], 0.0, [("Sbf", h, 0)])
        for i in range(2):
            K.memset("pool", q0T[i], 0.0, [("q0T", i)])
            K.memset("pool", q1T[i], 0.0, [("q1T", i)])
        sver = [0, 0, 0, 0]

        def X(i):
            own = i >= TP
            j = i - TP
            p0 = i * 128
            b2 = i % 2
            x_t = xt[b2]
            XT = ("xt", b2)
            stt_ = st8[i % 4]
            ST = ("st8", i % 4)
            valid = vkt[:, i:i + 1]
            K.dma("sp", x_t, xin[p0:p0 + 128, :], (), [XT])
            ln_tok(x_t, XT, stt_, ST, lnbc, "lnbc", 0)
            if own:
                K.dma("sp", s_d[j * 128:(j + 1) * 128, :], x_t, [XT], ["s_d"])
            yield
            sT_t = sTw[b2]
            STK = ("sTw", b2)
            for half in range(2):
                pt, PK = K.pb()
                for cc in range(4):
                    c = half * 4 + cc
                    K.tr(pt[:, cc * 128:(cc + 1) * 128], x_t[:, c * 128:(c + 1) * 128], ident, [XT] + C0, [PK])
                K.cp("dve" if half == 0 else "act", sT_t[:, half * 4:half * 4 + 4, :], v3(pt[:, :], 128), [PK], [STK])
                K.done(PK)
            if own:
                K.dma("sp", sT_d[:, :, j * 128:(j + 1) * 128], sT_t, [STK], ["sT_d"])
            yield

            def proj_tok(col0, ncols):
                pt, PK = K.pb()
                for c in range(8):
                    K.mm(pt[:, 0:ncols], sT_t[:, c, :], wres[:, c, col0:col0 + ncols], c == 0, c == 7, [STK, ("wres", c)], [PK])
                return pt, PK

            def proj_feat(pt, PK, off, wsrc, col0, m, wkey):
                for c in range(8):
                    K.mm(pt[0:m, off:off + 128], wsrc[:, c, col0:col0 + m], sT_t[:, c, :], c == 0, c == 7, [STK, wkey if wkey else ("wres", c)], [PK])

            pa, PKa = K.pb()
            proj_feat(pa, PKa, 0, wres, 3072, 16, None)
            K.cp("dve", aaug[0:16, :], pa[0:16, 0:128], [PKa], ["aaug"])
            K.done(PKa)
            pz, PKz = K.pb()
            K.mm(pz[:, :], aaug, w2t, True, True, ["aaug", "w2t"], [PKz])
            lg = loga
            LG = "loga"
            K.act(lg, pz[:, :], AF.Exp, [PKz], [LG], scale=-1.0)
            K.done(PKz)
            K.act(lg, lg, AF.Ln, [LG] + C0, [LG], bias=c_one, scale=1.0)
            K.ts("dve", lg, lg, valid, -1.0 / 16.0, ALU.mult, ALU.mult, [LG] + C0, [LG])
            yield
            v_b = vb[b2]
            VB = ("vb", b2)
            for hv in range(2):
                pv, PKv = proj_tok(1024 + hv * 512, 512)
                K.act(v_b[:, hv * 512:(hv + 1) * 512], pv[:, :], AF.Copy, [PKv] + C0, [VB], scale=valid)
                K.done(PKv)
                yield
            pbc, PKbc = K.pb()
            K.mm(pbc[:, :], triT, lg, True, True, [LG] + C0, [PKbc])
            K.act(e2, pbc[:, :], AF.Exp, [PKbc], ["e2"], scale=-1.0)
            K.done(PKbc)
            pk, PKk = proj_tok(512, 512)
            k_t = ktb[b2]
            KT = ("ktb", b2)
            K.tt("dve", k_t, pk[:, :], e2, ALU.mult, [PKk, "e2"], [KT])
            K.done(PKk)
            pd, PKd = K.pb()
            for h in range(4):
                K.mm(pd[:, h * 2:h * 2 + 2], lg[:, h * 128:(h + 1) * 128], chsel, True, True, [LG] + C0, [PKd])
            K.act(dec2[b2], pd[:, 0:8], AF.Exp, [PKd], [("dec", b2)])
            K.done(PKd)
            yield
            if own:
                pbT, PKbT = K.pb()
                for h in range(4):
                    K.mm(pbT[:, h * 128:(h + 1) * 128], lg[:, h * 128:(h + 1) * 128], triT, True, True, [LG] + C0, [PKbT])
                K.act(e1T, pbT[:, :], AF.Exp, [PKbT], ["e1T"])
                K.act(e2T, pbT[:, :], AF.Exp, [PKbT], ["e2T"], scale=-1.0)
                K.done(PKbT)
                pq, PKq = K.pb()
                for h in range(4):
                    proj_feat(pq, PKq, h * 128, wres, h * 128, 128, None)
                q0 = q0T[b2]
                q1 = q1T[b2]
                Q0 = ("q0T", b2)
                Q1 = ("q1T", b2)
                K.stt("dve", q0[:, :, 0:64], v3(pq[:, :], 128)[:, :, 0:64], 128.0 ** -0.5, v3(e1T, 128)[:, :, 0:64], ALU.mult, ALU.mult, [PKq, "e1T"], [Q0])
                K.stt("dve", q1[:, :, 64:128], v3(pq[:, :], 128)[:, :, 64:128], 128.0 ** -0.5, v3(e1T, 128)[:, :, 64:128], ALU.mult, ALU.mult, [PKq, "e1T"], [Q1])
                K.done(PKq)
                yield
                pkT, PKkT = K.pb()
                for h in range(4):
                    proj_feat(pkT, PKkT, h * 128, wres, 512 + h * 128, 128, None)
                k_T = ktT
                KTT = "ktT"
                K.tt("dve", k_T.rearrange("p h n -> p (h n)"), pkT[:, :], e2T, ALU.mult, [PKkT, "e2T"], [KTT])
                K.done(PKkT)
                yield
                pat, PKat = K.pb()
                for h in range(4):
                    K.mm(pat[:, h * 128:(h + 1) * 128], k_T[:, h, :], q0[:, h, :], True, False, [KTT, Q0], [PKat])
                    K.mm(pat[:, h * 128:(h + 1) * 128], k_T[:, h, :], q1[:, h, :], False, True, [KTT, Q1], [PKat])
                a_m = attm[b2]
                AM = ("attm", b2)
                for h in range(4):
                    K.tt("dve", a_m[:, h, :], pat[:, h * 128:(h + 1) * 128], tri_b, ALU.mult, [PKat] + C0, [AM])
                K.done(PKat)
                yield
                for hr in range(2):
                    pr_, PKr_ = K.pb()
                    for vc in range(4):
                        proj_feat(pr_, PKr_, vc * 128, wres, 2048 + (hr * 4 + vc) * 128, 128, None)
                    K.act(silur2[b2][:, hr * 512:(hr + 1) * 512], pr_[:, :], AF.Silu, [PKr_], [("silur", b2)])
                    K.done(PKr_)
                    yield
            pc, PKc = K.pb()
            proj_feat(pc, PKc, 0, wres, 3472, 128, None)
            proj_feat(pc, PKc, 128, wres, 3600, 128, None)
            proj_feat(pc, PKc, 256, wkr, 0, 32, "wkr")
            proj_feat(pc, PKc, 384, wkr, 32, 32, "wkr")
            sm = sqm
            SM = "sqm"
            K.act(sm[:, 0:256], pc[:, 0:256], AF.Square, [PKc], [SM])
            pr, PKr = K.pb()
            K.mm(pr[:, 0:128], ones_b, sm[:, 0:128], True, False, [SM] + C0, [PKr])
            K.mm(pr[:, 0:128], ones_b, sm[:, 128:256], False, True, [SM] + C0, [PKr])
            K.act(rsm, pr[:, 0:128], AF.Sqrt, [PKr] + C0, ["rsm"], bias=c_rmseps, scale=1.0 / 256.0)
            K.done(PKr)
            K.recip(rsm, rsm, ["rsm"], ["rsm"])
            for c in range(2):
                K.stt("dve", ckvnT[:, c, p0:p0 + 128], pc[:, c * 128:(c + 1) * 128], gcol[:, 5 + c:6 + c], rsm, ALU.mult, ALU.mult,
                      [PKc, "rsm"] + C0, [("ckvnT", i)])
            cs_ = cs32[b2]
            CS = ("cs32", b2)
            K.dma("sp", cs_, cskv[:, :, p0:p0 + 128], (), [CS])
            K.tt("dve", cs_[:, 0, :], pc[0:32, 256:384], cs_[:, 0, :], ALU.mult, [PKc, CS], [CS])
            K.tt("dve", cs_[:, 1, :], pc[0:32, 384:512], cs_[:, 1, :], ALU.mult, [PKc, CS], [CS])
            K.done(PKc)
            K.tt("pool", kropeT[:, p0:p0 + 128], cs_[:, 0, :], cs_[:, 1, :], ALU.add, [CS], [("kropeT", i)])
            yield
            if own:
                pq3, PKq3 = K.pb()
                for c in range(3):
                    proj_feat(pq3, PKq3, c * 128, wres, 3088 + c * 128, 128, None)
                K.act(sm[:, 256:640], pq3[:, 0:384], AF.Square, [PKq3], [SM])
                pr, PKr = K.pb()
                for c in range(3):
                    K.mm(pr[:, 0:128], ones_b, sm[:, 256 + c * 128:256 + (c + 1) * 128], c == 0, c == 2, [SM] + C0, [PKr])
                K.act(rsm, pr[:, 0:128], AF.Sqrt, [PKr] + C0, ["rsm"], bias=c_rmseps, scale=1.0 / 384.0)
                K.done(PKr)
                K.recip(rsm, rsm, ["rsm"], ["rsm"])
                for c in range(3):
                    K.stt("dve", cqnT[:, c, j * 128:(j + 1) * 128], pq3[:, c * 128:(c + 1) * 128], gcol[:, 2 + c:3 + c], rsm, ALU.mult, ALU.mult,
                          [PKq3, "rsm"] + C0, [("cqnT", j)])
                K.done(PKq3)
                yield

        def Y(i):
            own = i >= TP
            j = i - TP
            b2 = i % 2
            v_b = vb[b2]
            VB = ("vb", b2)
            k_t = ktb[b2]
            KT = ("ktb", b2)
            dec = dec2[b2]
            DK = ("dec", b2)

            def state_update(ch):
                for h in range(4):
                    pu, PKu = K.pb()
                    K.mm(pu[:, 0:256], k_t[ch * 64:(ch + 1) * 64, h * 128:(h + 1) * 128], v_b[ch * 64:(ch + 1) * 64, h * 256:(h + 1) * 256],
                         True, True, [KT, VB], [PKu])
                    K.tt("dve", tmpS[h], pu[:, 0:256], S32[h], ALU.add, [PKu, ("S32", h)], [("tmpS", h)])
                    K.done(PKu)
                    nv = (sver[h] + 1) % 3
                    K.act(Sbf[h][nv], tmpS[h], AF.Copy, [("tmpS", h), DK], [("Sbf", h, nv)], scale=dec[:, h * 2 + ch:h * 2 + ch + 1])
                    K.ts("dve", S32[h], tmpS[h], dec[:, h * 2 + ch:h * 2 + ch + 1], None, ALU.mult, None, [("tmpS", h), DK], [("S32", h)])
                    sver[h] = nv
                    if h % 2 == 1:
                        yield

            if not own:
                yield from state_update(0)
                yield from state_update(1)
                return
            q0 = q0T[b2]
            q1 = q1T[b2]
            Q0 = ("q0T", b2)
            Q1 = ("q1T", b2)
            a_m = attm[b2]
            AM = ("attm", b2)
            silur = silur2[b2]
            SR = ("silur", b2)
            po0, PKo0 = K.pb()
            po1, PKo1 = K.pb()
            sa = list(sver)
            for h in range(4):
                for vc in range(2):
                    idx = h * 2 + vc
                    po, PKo = (po0, PKo0) if idx < 4 else (po1, PKo1)
                    oc = (idx % 4) * 128
                    K.mm(po[:, oc:oc + 128], Sbf[h][sa[h]][:, vc * 128:(vc + 1) * 128], q0[:, h, :], idx % 4 == 0, False, [("Sbf", h, sa[h]), Q0], [PKo], skip=True)
                    K.mm(po[:, oc:oc + 128], v_b[:, h * 256 + vc * 128:h * 256 + (vc + 1) * 128], a_m[:, h, :], False, False, [VB, AM], [PKo], skip=True)
                if h % 2 == 1:
                    yield
            yield from state_update(0)
            for h in range(4):
                for vc in range(2):
                    idx = h * 2 + vc
                    po, PKo = (po0, PKo0) if idx < 4 else (po1, PKo1)
                    oc = (idx % 4) * 128
                    K.mm(po[:, oc:oc + 128], Sbf[h][sver[h]][:, vc * 128:(vc + 1) * 128], q1[:, h, :], False, idx % 4 == 3, [("Sbf", h, sver[h]), Q1], [PKo], skip=True)
            yield
            yield from state_update(1)
            sq_ = sqb
            SQ = "sqb"
            K.act(sq_[:, 0:512], po0[:, :], AF.Square, [PKo0], [SQ])
            K.act(sq_[:, 512:1024], po1[:, :], AF.Square, [PKo1], [SQ])
            pss, PKss = K.pb()
            for h in range(4):
                for vc in range(2):
                    K.mm(pss[:, h * 128:(h + 1) * 128], ones_b, sq_[:, (h * 2 + vc) * 128:(h * 2 + vc + 1) * 128], vc == 0, vc == 1, [SQ] + C0, [PKss])
            K.act(rstdg, pss[:, :], AF.Sqrt, [PKss] + C0, ["rstdg"], bias=c_rmseps, scale=1.0 / 256.0)
            K.done(PKss)
            K.recip(rstdg, rstdg, ["rstdg"], ["rstdg"])
            yield
            og = oglaT[b2]
            OG = ("oglaT", b2)
            for h in range(4):
                for vc in range(2):
                    idx = h * 2 + vc
                    po, PKo = (po0, PKo0) if idx < 4 else (po1, PKo1)
                    oc = (idx % 4) * 128
                    K.stt("dve", silur[:, idx * 128:(idx + 1) * 128], po[:, oc:oc + 128], gcol[:, vc:vc + 1], silur[:, idx * 128:(idx + 1) * 128],
                          ALU.mult, ALU.mult, [PKo, SR] + C0, [SR])
                    K.tt("pool", og[:, idx, :], silur[:, idx * 128:(idx + 1) * 128], rstdg[:, h * 128:(h + 1) * 128], ALU.mult, [SR, "rstdg"], [OG])
                if h % 2 == 1:
                    yield
            K.done(PKo0, PKo1)
            K.dma("sp", oglaT_d[:, :, j * 128:(j + 1) * 128], og, [OG], ["oglaT_d"])

        def drain(g):
            for _ in g:
                pass

        def interleave(ga, gb):
            ga_done = ga is None
            gb_done = gb is None
            while not (ga_done and gb_done):
                if not ga_done:
                    try:
                        next(ga)
                    except StopIteration:
                        ga_done = True
                if not gb_done:
                    try:
                        next(gb)
                    except StopIteration:
                        gb_done = True

        if "nopipe" in flags:
            for i in range(TT):
                drain(X(i))
                drain(Y(i))
        else:
            drain(X(0))
            for i in range(TT):
                interleave(X(i + 1) if i + 1 < TT else None, Y(i))

        CKV = [("ckvnT", i) for i in range(TT)]
        KRP = [("kropeT", i) for i in range(TT)]
        CQN = [("cqnT", j) for j in range(TO)]
        if debug:
            finals.append(K.dma("sp", dbg["ckvnT"], ckvnT, CKV, []))
            finals.append(K.dma("sp", dbg["kropeT"], kropeT, KRP, []))
            finals.append(K.dma("sp", dbg["cqnT"], cqnT, CQN, []))
        if stages >= 3:
            S.barrier()
            S.mute = only is not None and 3 not in only
            A.set(OMLA_OFF)
            omlaT = v3(A.B(8 * NOWN), NOWN)
            OMLA_END = A.off
            wuqt = A.B(3 * 16 * 192).rearrange("p (c h n) -> p c h n", c=3, h=16)
            wukt = A.B(2 * 16 * 96).rearrange("p (c h n) -> p c h n", c=2, h=16)
            wuvt = v3(A.B(2 * 1024), 1024)
            KTh = [A.B(NPOS, parts=96) for i in range(2)]
            Vh = [v3(A.B(TT * 128), 128) for i in range(2)]
            QTh = [A.B(NOWN, parts=96) for i in range(2)]
            PT = [A.B(512) for i in range(6)]
            csqt = v3(A.F(2 * NOWN, parts=96), NOWN)
            qa = [A.F(512, parts=96) for i in range(2)]
            rden = A.F(512)
            print("stage3 arena end", A.off)
            K.dma("pool", wuqt.rearrange("p c h n -> p c (h n)"), wuq2.rearrange("(c p) h a n -> p c (h a n)", p=128), (), ["wuqt"])
            K.dma("pool", wukt.rearrange("p c h n -> p c (h n)"), wukp.rearrange("(c p) h n -> p c (h n)", p=128), (), ["wukt"])
            K.dma("pool", wuvt, wuv.rearrange("(c p) n -> p c n", p=128), (), ["wuvt"])
            K.dma("sp", csqt, csq, (), ["csqt"])
            for p_ in range(2):
                K.cp("act", KTh[p_][64:96, :], kropeT, KRP, [("KTh", p_)])
            if sparse:
                Gz = G_d.rearrange("(p r) n -> p (r n)", p=128)
                for k_ in range(32):
                    K.dma("sp", Gz[:, k_ * 2048:(k_ + 1) * 2048], zt[:], ["zt"], ["G_d"])
            K.memset("pool", Vh[0][:, :, 64:128], 1.0, [("Vh", 0)])
            K.memset("pool", Vh[1][:, :, 0:64], 1.0, [("Vh", 1)])
            sm_scale = 96.0 ** -0.5

            def prep_chunks(h):
                par = h % 2
                KT_, KTK = KTh[par], ("KTh", par)
                V_, VK = Vh[par], ("Vh", par)
                Q_, QK = QTh[par], ("QTh", par)
                chunks = []
                nb = (NPOS + 511) // 512

                def m1(blk):
                    c0 = blk * 512
                    n = min(512, NPOS - c0)
                    pt, PK = K.pb()
                    K.mm(pt[0:64, 0:n], wukt[:, 0, h, 0:64], ckvnT[:, 0, c0:c0 + n], True, False, ["wukt"] + CKV, [PK])
                    K.mm(pt[0:64, 0:n], wukt[:, 1, h, 0:64], ckvnT[:, 1, c0:c0 + n], False, True, ["wukt"] + CKV, [PK])
                    K.cp("dve", KT_[0:64, c0:c0 + n], pt[0:64, 0:n], [PK], [KTK])
                    K.done(PK)

                def m2(t0):
                    vo = 0 if par == 0 else 64
                    nt = min(8, TT - t0)
                    pt, PK = K.pb()
                    for t in range(nt):
                        for c in range(2):
                            K.mm(pt[:, t * 64:(t + 1) * 64], ckvnT[:, c, (t0 + t) * 128:(t0 + t + 1) * 128], wuvt[:, c, h * 64:(h + 1) * 64], c == 0, c == 1,
                                 ["wuvt"] + CKV, [PK])
                    K.cp("dve", V_[:, t0:t0 + nt, vo:vo + 64], v3(pt[:, 0:nt * 64], 64), [PK], [VK])
                    K.done(PK)

                def m3(qb):
                    c0 = qb * 512
                    pA, PKA = K.pb()
                    pB, PKB = K.pb()
                    for c in range(3):
                        K.mm(pA[0:96, :], wuqt[:, c, h, 0:96], cqnT[:, c, c0:c0 + 512], c == 0, c == 2, ["wuqt"] + CQN, [PKA])
                    for c in range(3):
                        K.mm(pB[0:96, :], wuqt[:, c, h, 96:192], cqnT[:, c, c0:c0 + 512], c == 0, c == 2, ["wuqt"] + CQN, [PKB])
                    K.tt("dve", qa[0], pA[0:96, :], csqt[:, 0, c0:c0 + 512], ALU.mult, [PKA, "csqt"], ["qa0"])
                    K.tt("dve", qa[1], pB[0:96, :], csqt[:, 1, c0:c0 + 512], ALU.mult, [PKB, "csqt"], ["qa1"])
                    K.done(PKA, PKB)
                    K.tt("pool", Q_[:, c0:c0 + 512], qa[0], qa[1], ALU.add, ["qa0", "qa1"], [QK])

                for blk in range(nb):
                    chunks.append((m1, blk))
                for t0 in range(0, TT, 8):
                    chunks.append((m2, t0))
                for qb in range(4):
                    chunks.append((m3, qb))
                return chunks

            items = []
            for h in range(16):
                for qb in range(4):
                    full = [(t, 0) for t in range(TP)] + [(TP + 4 * qb2 + d, 0) for qb2 in range(qb) for d in range(4)]
                    diag = [(TP + 4 * qb + d, d * 128) for d in range(4)]
                    ktiles = full[0:1] + diag + full[1:]
                    for n_, (t, qo) in enumerate(ktiles):
                        items.append((h, qb, t, qo, n_ == 0, n_ == len(ktiles) - 1, t >= TP + 4 * qb))
            LA = 3
            precast = []
            for e_ in range(32):
                precast += [(ewg[e_], ewg_b[e_]), (ewu[e_], ewu_b[e_]), (ewd[e_], ewd_b[e_])]
            NPT = len(PT)
            pobank = {}
            pending = []
            for f_, a_ in prep_chunks(0):
                f_(a_)
            hcur = -1
            since = 0
            for idx in range(len(items) + LA):
                if idx < len(items):
                    h, qb, t, qo, first, last, isdiag = items[idx]
                    par = h % 2
                    if h != hcur:
                        for f_, a_ in pending:
                            f_(a_)
                        pending = prep_chunks(h + 1) if h + 1 < 16 else []
                        hcur = h
                        since = 0
                    since += 1
                    if sparse and idx % 18 == 0 and precast:
                        src_, dst_ = precast.pop(0)
                        K.dma("pool", dst_, src_, (), [])
                    if pending and since % 5 == 0:
                        f_, a_ = pending.pop(0)
                        f_(a_)
                    ps_, PKs = K.pb()
                    nq = 512 - qo
                    c0 = qb * 512
                    K.mm(ps_[:, 0:nq], KTh[par][:, t * 128:(t + 1) * 128], QTh[par][:, c0 + qo:c0 + 512], True, True, [("KTh", par), ("QTh", par)], [PKs])
                    pT = PT[idx % NPT]
                    PTK = ("PT", idx % NPT)
                    K.act(pT[:, 0:nq], ps_[:, 0:nq], AF.Exp, [PKs] + C0, [PTK], bias=vkt[:, TT + t:TT + t + 1], scale=sm_scale)
                    K.done(PKs)
                    if isdiag:
                        K.tt("pool", pT[:, 0:128], pT[:, 0:128], ut_b, ALU.mult, [PTK] + C0, [PTK])
                j_ = idx - LA
                if j_ >= 0:
                    h, qb, t, qo, first, last, isdiag = items[j_]
                    par = h % 2
                    c0 = qb * 512
                    nq = 512 - qo
                    if first:
                        pobank[(h, qb)] = K.pb()
                    po, PKo = pobank[(h, qb)]
                    pT = PT[j_ % NPT]
                    PTK = ("PT", j_ % NPT)
                    K.mm(po[:, qo:512], Vh[par][:, t, :], pT[:, 0:nq], first, last, [("Vh", par), PTK], [PKo])
                    if last:
                        hp = h // 2
                        if par == 0:
                            K.recip(rden[0:64, :], po[64:128, :], [PKo], ["rden"])
                            K.tt("dve", omlaT[0:64, hp, c0:c0 + 512], po[0:64, :], rden[0:64, :], ALU.mult, [PKo, "rden"], [("omlaT", qb)])
                        else:
                            K.recip(rden[64:128, :], po[0:64, :], [PKo], ["rden"])
                            K.tt("dve", omlaT[64:128, hp, c0:c0 + 512], po[64:128, :], rden[64:128, :], ALU.mult, [PKo, "rden"], [("omlaT", qb)])
                        K.done(PKo)
                        del pobank[(h, qb)]
            if sparse:
                for src_, dst_ in precast:
                    K.dma("pool", dst_, src_, (), [])
            OMK = [("omlaT", q) for q in range(4)]
            if debug:
                finals.append(K.dma("sp", dbg["omlaT"], omlaT, OMK, []))
        def ln_gen(x_t, XTL, stt_, ST, lnbc, LK, gi):
            JK = ("junk2", ST[1] % 2)
            jk = junk2[ST[1] % 2]
            K.act(jk[:], x_t, AF.Identity, XTL, [JK, ST], accum=stt_[:, 0:1]); yield
            K.act(jk[:], x_t, AF.Square, XTL, [JK, ST], accum=stt_[:, 1:2]); yield
            K.ts("dve", stt_[:, 2:3], stt_[:, 0:1], 1.0 / D, None, ALU.mult, None, [ST], [ST]); yield
            K.tt("dve", stt_[:, 3:4], stt_[:, 2:3], stt_[:, 2:3], ALU.mult, [ST], [ST]); yield
            K.stt("dve", stt_[:, 4:5], stt_[:, 1:2], 1.0 / D, stt_[:, 3:4], ALU.mult, ALU.subtract, [ST], [ST]); yield
            K.act(stt_[:, 5:6], stt_[:, 4:5], AF.Sqrt, [ST] + C0, [ST], bias=c_lneps, scale=1.0); yield
            K.recip(stt_[:, 5:6], stt_[:, 5:6], [ST], [ST]); yield
            K.stt("dve", stt_[:, 6:7], stt_[:, 2:3], -1.0, stt_[:, 5:6], ALU.mult, ALU.mult, [ST], [ST]); yield
            K.act(x_t, x_t, AF.Identity, XTL + [ST], XTL, bias=stt_[:, 6:7], scale=stt_[:, 5:6]); yield
            K.tt("dve", x_t, x_t, lnbc[:, gi, :], ALU.mult, XTL + [LK], XTL); yield
            K.tt("dve", x_t, x_t, lnbc[:, gi + 1, :], ALU.add, XTL + [LK], XTL); yield

        if stages >= 4:
            S.barrier()
            S.mute = only is not None and 4 not in only
            assert OMLA_OFF == 37632 and OMLA_END == 70400
            A.set(0)
            oglaTs = v3(A.B(8 * 512), 512)
            sTs = v3(A.B(8 * 512), 512)
            woutt = v3(A.B(8 * 1024), 1024)
            A.set(OMLA_END)
            wgat = v3(A.B(8 * 2048), 2048)
            wbgt = v3(A.B(8 * 1024), 1024)
            wbmt = v3(A.B(8 * 1024), 1024)
            W4_END = A.off
            mergedT = v3(A.B(8 * NOWN), NOWN)
            M_END = A.off
            sga = [A.B(512) for i in range(4)]
            print("stage4a arena end", A.off)
            for c in range(8):
                K.dma("pool", wgat[:, c, :], w_in_v[:, c, W_RES:5808], (), [("wgat", c)])
                K.dma("pool", wbgt[:, c, :], wbg.rearrange("(c p) n -> p c n", p=128)[:, c, :], (), [("wbgt", c)])
                K.dma("pool", wbmt[:, c, :], wbm.rearrange("(c p) n -> p c n", p=128)[:, c, :], (), [("wbmt", c)])
            K.dma("pool", woutt, wout.rearrange("(c p) n -> p c n", p=128), (), ["woutt"])
            WG8 = [("wgat", c) for c in range(8)]
            WB8 = [("wbgt", c) for c in range(8)]
            WM8 = [("wbmt", c) for c in range(8)]
            nsg = 0
            for blk in range(4):
                c0 = blk * 512
                K.dma("sp", oglaTs, oglaT_d[:, :, c0:c0 + 512], ["oglaT_d"], ["oglaTs"])
                K.dma("sp", sTs, sT_d[:, :, c0:c0 + 512], ["sT_d"], ["sTs"])
                for dc in range(8):
                    sa_ = sga[nsg % 4]; SA = ("sga", nsg % 4); nsg += 1
                    sb_ = sga[nsg % 4]; SB = ("sga", nsg % 4); nsg += 1
                    pga, PKga = K.pb()
                    pgb, PKgb = K.pb()
                    for c in range(8):
                        K.mm(pga[:, :], wgat[:, c, dc * 128:(dc + 1) * 128], sTs[:, c, :], c == 0, c == 7, [("wgat", c), "sTs"], [PKga])
                    for c in range(8):
                        K.mm(pgb[:, :], wgat[:, c, 1024 + dc * 128:1024 + (dc + 1) * 128], sTs[:, c, :], c == 0, c == 7, [("wgat", c), "sTs"], [PKgb])
                    K.act(sa_, pga[:, :], AF.Sigmoid, [PKga], [SA])
                    K.act(sb_, pgb[:, :], AF.Sigmoid, [PKgb], [SB])
                    K.done(PKga, PKgb)
                    pbg, PKbg = K.pb()
                    pbm, PKbm = K.pb()
                    for c in range(8):
                        K.mm(pbg[:, :], wbgt[:, c, dc * 128:(dc + 1) * 128], oglaTs[:, c, :], c == 0, c == 7, [("wbgt", c), "oglaTs"], [PKbg])
                    for c in range(8):
                        K.mm(pbm[:, :], wbmt[:, c, dc * 128:(dc + 1) * 128], omlaT[:, c, c0:c0 + 512], c == 0, c == 7, [("wbmt", c), ("omlaT", blk)], [PKbm])
                    K.tt("dve", sa_, sa_, pbg[:, :], ALU.mult, [SA, PKbg], [SA])
                    K.tt("dve", sb_, sb_, pbm[:, :], ALU.mult, [SB, PKbm], [SB])
                    K.done(PKbg, PKbm)
                    K.tt("pool", mergedT[:, dc, c0:c0 + 512], sa_, sb_, ALU.add, [SA, SB], [("mergedT", blk)])
            MT = [("mergedT", q) for q in range(4)]
            S.barrier()
            A.set(OMLA_OFF)
            s2T = v3(A.B(8 * NOWN), NOWN)
            lnbc = v3(A.F(2 * D), D)
            s2Tf = [A.F(1024)] * 2
            xt = [A.F(D) for i in range(4)]
            rlog = v3(A.F(TO * 36), 36)
            rb_bc = A.F(36)
            ixb = A.F(3616)
            wrt = v3(A.F(8 * 36), 36)
            s2b = [None] * TO
            for j in range(8, TO):
                s2b[j] = A.B(D)
            assert A.off <= W4_END, A.off
            end4b = A.off
            A.set(0)
            for j in range(8):
                s2b[j] = A.B(D)
            A.set(M_END)
            rtb = A.F(2560)
            A.set(end4b)
            print("stage4b arena end", A.off)
            K.dma("sp", lnbc[:, 0, :], lng[1, 0].partition_broadcast(128), (), ["lnbc"])
            K.dma("sp", lnbc[:, 1, :], lng[1, 1].partition_broadcast(128), (), ["lnbc"])
            K.dma("sp", rb_bc, rbias.partition_broadcast(128), (), ["rb_bc"])
            K.dma("sp", wrt, wr.rearrange("(c p) n -> p c n", p=128), (), ["wrt"])
            def P4(j):
                blk = j // 4
                x_t = xt[j % 4]
                XT = ("xt", j % 4)
                K.dma("sp", x_t, s_d[j * 128:(j + 1) * 128, :], ["s_d"], [XT])
                for hh in range(2):
                    ph, PKh = K.pb()
                    for c in range(8):
                        K.mm(ph[:, :], mergedT[:, c, j * 128:(j + 1) * 128], woutt[:, c, hh * 512:(hh + 1) * 512], c == 0, c == 7, [("mergedT", blk), "woutt"], [PKh])
                    K.stt("dve", x_t[:, hh * 512:(hh + 1) * 512], x_t[:, hh * 512:(hh + 1) * 512], ALPHA, ph[:, :], ALU.mult, ALU.add, [XT, PKh], [XT])
                    K.done(PKh)

            def L4(j):
                return ln_gen(xt[j % 4], [("xt", j % 4)], st8[j % 4], ("st8", j % 4), lnbc, "lnbc", 0)

            def Q4(j):
                x_t = xt[j % 4]
                XT = ("xt", j % 4)
                K.dma("sp", s2_d[j * 128:(j + 1) * 128, :], x_t, [XT], ["s2_d"])
                if sparse:
                    K.cp("act", s2b[j], x_t, [XT], [("s2b", j)])
                for half in range(2):
                    pt, PK = K.pb()
                    for cc in range(4):
                        c = half * 4 + cc
                        K.tr(pt[:, cc * 128:(cc + 1) * 128], x_t[:, c * 128:(c + 1) * 128], ident, [XT] + C0, [PK])
                    sf = s2Tf[j % 2]
                    K.cp("dve", sf[:, half * 512:(half + 1) * 512], pt[:, :], [PK], [("s2Tf", 0, half)])
                    K.done(PK)
                    if not sparse:
                        K.cp("act", s2T[:, half * 4:half * 4 + 4, j * 128:(j + 1) * 128], v3(sf[:, half * 512:(half + 1) * 512], 128), [("s2Tf", 0, half)], [("s2T", j)])
                prt, PKrt = K.pb()
                for c in range(8):
                    K.mm(prt[:, 0:36], s2Tf[j % 2][:, c * 128:(c + 1) * 128], wrt[:, c, :], c == 0, c == 7, [("s2Tf", 0, 0), ("s2Tf", 0, 1), "wrt"], [PKrt])
                K.tt("dve", rlog[:, j, :], prt[:, 0:36], rb_bc, ALU.add, [PKrt, "rb_bc"], [("rlog", j)])
                K.done(PKrt)

            P4(0)
            P4(1)
            for j0 in range(0, TO, 2):
                if j0 + 2 < TO:
                    P4(j0 + 2)
                    P4(j0 + 3)
                interleave(L4(j0), L4(j0 + 1))
                Q4(j0)
                Q4(j0 + 1)
            RL = [("rlog", j) for j in range(TO)]
            R = ["rtb"]
            l4 = rlog[:, :, 0:4]
            le = rtb[:, 0:512].rearrange("p (a e) -> p a e", e=8)
            K.cp("dve", rtb[:, 0:512].rearrange("p (t n) -> p t n", n=32), rlog[:, :, 4:36], RL, R)
            m4 = rtb[:, 512:528]
            K.red(m4, l4, ALU.max, RL, R)
            d4 = rtb[:, 528:592].rearrange("p (t g) -> p t g", g=4)
            K.tt("dve", d4, l4, m4.unsqueeze(2).broadcast_to([128, 16, 4]), ALU.subtract, RL + R, R)
            e4 = rtb[:, 592:656].rearrange("p (t g) -> p t g", g=4)
            K.act(e4, d4, AF.Exp, R, R)
            s4 = rtb[:, 656:672]
            K.red(s4, e4, ALU.add, R, R)
            K.recip(s4, s4, R, R)
            gw = rtb[:, 672:736].rearrange("p (t g) -> p t g", g=4)
            K.ts("dve", gw, d4, 0.0, None, ALU.is_equal, None, R, R)
            K.tt("dve", gw, gw, s4.unsqueeze(2).broadcast_to([128, 16, 4]), ALU.mult, R, R)
            m1 = rtb[:, 736:800]
            K.red(m1, le, ALU.max, R, R)
            eq1 = rtb[:, 800:1312].rearrange("p (a e) -> p a e", e=8)
            K.tt("dve", eq1, le, m1.unsqueeze(2).broadcast_to([128, 64, 8]), ALU.is_equal, R, R)
            l2 = rtb[:, 1312:1824].rearrange("p (a e) -> p a e", e=8)
            K.stt("dve", l2, eq1, -1e30, le, ALU.mult, ALU.add, R, R)
            m2 = rtb[:, 1824:1888]
            K.red(m2, l2, ALU.max, R, R)
            eq2 = rtb[:, 1888:2400].rearrange("p (a e) -> p a e", e=8)
            K.tt("dve", eq2, l2, m2.unsqueeze(2).broadcast_to([128, 64, 8]), ALU.is_equal, R, R)
            w1 = rtb[:, 2400:2464]
            K.tt("dve", w1, m1, m2, ALU.subtract, R, R)
            K.act(w1, w1, AF.Sigmoid, R, R)
            w2 = rtb[:, 2464:2528]
            K.ts("dve", w2, w1, -1.0, 1.0, ALU.mult, ALU.add, R, R)
            K.tt("dve", eq1, eq1, w1.unsqueeze(2).broadcast_to([128, 64, 8]), ALU.mult, R, R)
            K.tt("dve", eq2, eq2, w2.unsqueeze(2).broadcast_to([128, 64, 8]), ALU.mult, R, R)
            K.tt("dve", eq1, eq1, eq2, ALU.add, R, R)
            K.tt("dve", comb[:].rearrange("p t (g e) -> p (t g) e", e=8), eq1, gw.rearrange("p t g -> p (t g)").unsqueeze(2).broadcast_to([128, 64, 8]),
                 ALU.mult, R, [("comb", j) for j in range(TO)])
            if sparse:
                CM_ = [("comb", j) for j in range(TO)]
                IX = ["ixb"]
                comb3 = comb[:].rearrange("p t e -> p (t e)")
                Mk = ixb[:, 0:512]
                K.ts("dve", Mk, comb3, 0.0, None, ALU.is_gt, None, CM_, IX)
                prk, PKrk = K.pb()
                pcn, PKcn = K.pb()
                K.mm(prk[:, :], cmt[:, 610:738], Mk, True, True, IX + C0, [PKrk])
                K.mm(pcn[:, :], cmt[:, 482:610], Mk, True, True, IX + C0, [PKcn])
                cnts = v3(ixb[:, 512:1024], 32)
                K.cp("dve", ixb[:, 512:1024], pcn[:, :], [PKcn], IX)
                K.done(PKcn)
                offs = v3(ixb[:, 1024:1536], 32)
                K.memset("dve", offs[:, 0, :], 0.0, IX)
                for j in range(1, TO):
                    K.tt("dve", offs[:, j, :], offs[:, j - 1, :], cnts[:, j - 1, :], ALU.add, IX, IX)
                slot = ixb[:, 1536:2048]
                K.tt("dve", slot, prk[:, :], ixb[:, 1024:1536], ALU.add, [PKrk] + IX, IX)
                K.done(PKrk)
                valid_ = ixb[:, 2048:2560]
                K.ts("dve", valid_, slot, float(CAP), None, ALU.is_lt, None, IX, IX)
                K.tt("dve", valid_, valid_, Mk, ALU.mult, IX, IX)
                K.tt("dve", v3(slot, 32), v3(slot, 32), cmt[:, 738:770].unsqueeze(1).broadcast_to([128, TO, 32]), ALU.add, IX + C0, IX)
                rowv = ixb[:, 2560:3072]
                K.stt("dve", rowv, slot, 1.0, valid_, ALU.add, ALU.mult, IX, IX)
                K.ts("dve", rowv, rowv, -1.0, None, ALU.add, None, IX, IX)
                idxf = ixb[:, 3072:3104]
                K.red(idxf[:, 0:16], v3(rowv, 32), ALU.max, IX, IX)
                eqh = ixb[:, 3104:3616]
                K.tt("dve", v3(eqh, 32), v3(rowv, 32), idxf[:, 0:16].unsqueeze(2).broadcast_to([128, TO, 32]), ALU.is_equal, IX, IX)
                K.tt("dve", eqh, eqh, valid_, ALU.mult, IX, IX)
                K.stt("dve", rowv, eqh, -1e9, rowv, ALU.mult, ALU.add, IX, IX)
                K.red(idxf[:, 16:32], v3(rowv, 32), ALU.max, IX, IX)
                K.tt("dve", eqh, eqh, comb3, ALU.mult, IX + CM_, IX)
                K.red(wts[:, 0:16], v3(eqh, 32), ALU.add, IX, ["wts"])
                eql = ixb[:, 3104:3616]
                K.tt("dve", v3(eql, 32), v3(rowv, 32), idxf[:, 16:32].unsqueeze(2).broadcast_to([128, TO, 32]), ALU.is_equal, IX, IX)
                K.tt("dve", eql, eql, valid_, ALU.mult, IX, IX)
                K.tt("dve", eql, eql, comb3, ALU.mult, IX + CM_, IX)
                K.red(wts[:, 16:32], v3(eql, 32), ALU.add, IX, ["wts"])
                K.ts("dve", idxf, idxf, -1.0, None, ALU.max, None, IX, IX)
                K.cp("dve", idxi[:], idxf, IX, ["idxi"])
                for j in range(TO):
                    for k_ in range(2):
                        col = k_ * 16 + j
                        S.op("pool", (lambda j=j, col=col: (lambda e: e.indirect_dma_start(
                            out=G_d, out_offset=bass.IndirectOffsetOnAxis(ap=idxi[:, col:col + 1], axis=0), in_=s2b[j], in_offset=None,
                            bounds_check=bcreg(e), oob_is_err=False)))(), [("s2b", j), "idxi"], ["G_d"], dma=True)
            CMB = [("comb", j) for j in range(TO)]
            if debug:
                finals.append(K.dma("sp", dbg["comb"], comb[:], CMB, []))
        if stages >= 5 and not sparse:
            S.barrier()
            S.mute = only is not None and 5 not in only
            A.set(0)
            ewb = []
            for i in range(2):
                ewb.append((v3(A.B(2048), 256), v3(A.B(2048), 256), v3(A.B(2048), 1024)))
            lnbc = v3(A.F(2 * D), D)
            assert A.off <= OMLA_OFF
            A.set(OMLA_END)
            yacc = v3(A.F(TO * D), D)
            hidT = [v3(A.B(2 * NOWN), NOWN) for i in range(2)]
            silb = [A.B(512) for i in range(2)]
            print("stage5 arena end", A.off)
            S2K = [("s2T", j) for j in range(TO)]
            K.dma("sp", lnbc[:, 0, :], lng[2, 0].partition_broadcast(128), (), ["lnbc"])
            K.dma("sp", lnbc[:, 1, :], lng[2, 1].partition_broadcast(128), (), ["lnbc"])
            for j in range(TO):
                K.dma("sp", yacc[:, j, :], s2_d[j * 128:(j + 1) * 128, :], ["s2_d"], [("yacc", j, 0), ("yacc", j, 1)])
                K.act(yacc[:, j, :], yacc[:, j, :], AF.Copy, [("yacc", j, 0), ("yacc", j, 1)], [("yacc", j, 0), ("yacc", j, 1)], scale=ALPHA)
            for e_ in range(32):
                g_, u_, d_ = ewb[e_ % 2]
                EW = ("ew", e_ % 2)
                K.dma("pool", g_, ewg[e_].rearrange("(c p) n -> p c n", p=128), (), [EW])
                K.dma("pool", u_, ewu[e_].rearrange("(c p) n -> p c n", p=128), (), [EW])
                K.dma("pool", d_, ewd[e_].rearrange("(c p) n -> p c n", p=128), (), [EW])
                hT = hidT[e_ % 2]
                HT = ("hidT", e_ % 2)
                for fc in range(2):
                    for blk in range(4):
                        c0 = blk * 512
                        pg, PKg = K.pb()
                        pu, PKu = K.pb()
                        for c in range(8):
                            K.mm(pg[:, :], g_[:, c, fc * 128:(fc + 1) * 128], s2T[:, c, c0:c0 + 512], c == 0, c == 7, [EW] + S2K, [PKg])
                        for c in range(8):
                            K.mm(pu[:, :], u_[:, c, fc * 128:(fc + 1) * 128], s2T[:, c, c0:c0 + 512], c == 0, c == 7, [EW] + S2K, [PKu])
                        sl = silb[(fc * 4 + blk) % 2]
                        SL = ("silb", (fc * 4 + blk) % 2)
                        K.act(sl, pg[:, :], AF.Silu, [PKg], [SL])
                        K.tt("dve", hT[:, fc, c0:c0 + 512], sl, pu[:, :], ALU.mult, [SL, PKu], [HT])
                        K.done(PKg, PKu)
                for j in range(TO):
                    for half in range(2):
                        py, PKy = K.pb()
                        for fc in range(2):
                            K.mm(py[:, :], hT[:, fc, j * 128:(j + 1) * 128], d_[:, fc, half * 512:(half + 1) * 512], fc == 0, fc == 1, [HT, EW], [PKy])
                        ya = yacc[:, j, half * 512:(half + 1) * 512]
                        YK = ("yacc", j, half)
                        K.stt("dve", ya, py[:, :], comb[:, j, e_:e_ + 1], ya, ALU.mult, ALU.add, [PKy, ("comb", j), YK], [YK])
                        K.done(PKy)
            for j0 in range(0, TO, 2):
                gens = [ln_gen(yacc[:, j, :], [("yacc", j, 0), ("yacc", j, 1)], st8[j % 4], ("st8", j % 4), lnbc, "lnbc", 0) for j in (j0, j0 + 1)]
                alive = [True, True]
                while any(alive):
                    for k_, g_ in enumerate(gens):
                        if alive[k_]:
                            try:
                                next(g_)
                            except StopIteration:
                                alive[k_] = False
                for j in (j0, j0 + 1):
                    finals.append(K.dma("sp", out[j * 128:(j + 1) * 128, :], yacc[:, j, :], [("yacc", j, 0), ("yacc", j, 1)], []))
        if stages >= 5 and sparse:
            S.barrier()
            S.mute = only is not None and 5 not in only
            A.set(0)
            NW = 4
            ewb = []
            for i in range(NW):
                ewb.append((v3(A.B(2048), 256), v3(A.B(2048), 256), v3(A.B(2048), 1024)))
            lnbc = v3(A.F(2 * D), D)
            NST = CAP // 128
            gt = [v3(A.B(NST * D), D) for i in range(2)]
            gT = [v3(A.B(8 * CAP), CAP) for i in range(2)]
            hidT = [v3(A.B(2 * CAP), CAP) for i in range(2)]
            silb = [A.B(CAP) for i in range(2)]
            ysb = [v3(A.B(NST * D), D) for i in range(2)]
            NG = 8
            gth = [A.B(D) for i in range(NG)]
            xs2 = [A.F(D) for i in range(4)]
            print("stage5 sparse arena end", A.off)
            K.dma("sp", lnbc[:, 0, :], lng[2, 0].partition_broadcast(128), (), ["lnbc"])
            K.dma("sp", lnbc[:, 1, :], lng[2, 1].partition_broadcast(128), (), ["lnbc"])
            for i in range(NG):
                K.memset("pool", gth[i], 0.0, [("gth", i)])

            def wload(e_):
                g_, u_, d_ = ewb[e_ % NW]
                EW = ("ew", e_ % NW)
                K.dma("sp", g_, ewg_b[e_].rearrange("(c p) n -> p c n", p=128), (), [EW])
                K.dma("sp", u_, ewu_b[e_].rearrange("(c p) n -> p c n", p=128), (), [EW])
                K.dma("sp", d_, ewd_b[e_].rearrange("(c p) n -> p c n", p=128), (), [EW])

            for e_ in range(NW - 1):
                wload(e_)
            ncp = [0]

            def gload(e_):
                K.dma("sp", gt[e_ % 2], G_d[e_ * CAP:(e_ + 1) * CAP, :].rearrange("(s p) n -> p s n", p=128), ["G_d"], [("gt", e_ % 2)])

            def tpose(e_):
                g_t = gt[e_ % 2]
                GTK = ("gt", e_ % 2)
                gTe = gT[e_ % 2]
                GTT = ("gT", e_ % 2)
                for s_ in range(NST):
                    pt, PK = K.pb()
                    ptb = pt[:, :].bitcast(BF16)
                    for c in range(8):
                        K.tr(ptb[:, c * 128:(c + 1) * 128], g_t[:, s_, c * 128:(c + 1) * 128], cmb[:, 0:128], [GTK] + C0, [PK])
                    K.cp("dve" if ncp[0] % 2 == 0 else "act", gTe[:, :, s_ * 128:(s_ + 1) * 128], v3(ptb, 128), [PK], [GTT])
                    ncp[0] += 1
                    K.done(PK)

            def gate_up(e_):
                g_, u_, d_ = ewb[e_ % NW]
                EW = ("ew", e_ % NW)
                gTe = gT[e_ % 2]
                GTT = ("gT", e_ % 2)
                hT = hidT[e_ % 2]
                HT = ("hidT", e_ % 2)
                for fc in range(2):
                    pg, PKg = K.pb()
                    pu, PKu = K.pb()
                    for c in range(8):
                        K.mm(pg[:, 0:CAP], g_[:, c, fc * 128:(fc + 1) * 128], gTe[:, c, :], c == 0, c == 7, [EW, GTT], [PKg])
                    for c in range(8):
                        K.mm(pu[:, 0:CAP], u_[:, c, fc * 128:(fc + 1) * 128], gTe[:, c, :], c == 0, c == 7, [EW, GTT], [PKu])
                    sl = silb[fc]
                    SL = ("silb", fc)
                    K.act(sl, pg[:, 0:CAP], AF.Silu, [PKg], [SL])
                    K.tt("dve", hT[:, fc, :], sl, pu[:, 0:CAP], ALU.mult, [SL, PKu], [HT])
                    K.done(PKg, PKu)

            def down(e_):
                g_, u_, d_ = ewb[e_ % NW]
                EW = ("ew", e_ % NW)
                hT = hidT[e_ % 2]
                HT = ("hidT", e_ % 2)
                y_s = ysb[e_ % 2]
                YS = ("ysb", e_ % 2)
                for s_ in range(NST):
                    for half in range(2):
                        py, PKy = K.pb()
                        for fc in range(2):
                            K.mm(py[:, :], hT[:, fc, s_ * 128:(s_ + 1) * 128], d_[:, fc, half * 512:(half + 1) * 512], fc == 0, fc == 1, [HT, EW], [PKy])
                        K.cp("act" if ncp[0] % 2 == 0 else "dve", y_s[:, s_, half * 512:(half + 1) * 512], py[:, :], [PKy], [YS])
                        ncp[0] += 1
                        K.done(PKy)
                K.dma("sp", Y_d[e_ * CAP:(e_ + 1) * CAP, :].rearrange("(s p) n -> p s n", p=128), y_s, [YS], ["Y_d"])

            gload(0)
            gload(1)
            tpose(0)
            for e_ in range(32):
                gate_up(e_)
                if e_ + 1 < 32:
                    tpose(e_ + 1)
                if e_ + 2 < 32:
                    gload(e_ + 2)
                if e_ + NW - 1 < 32:
                    wload(e_ + NW - 1)
                down(e_)

            def fetch(j):
                x_t = xs2[j % 4]
                XT = ("xs2", j % 4)
                K.dma("sp", x_t, s2_d[j * 128:(j + 1) * 128, :], ["s2_d"], [XT])
                for k_ in range(2):
                    col = k_ * 16 + j
                    gi_ = (j * 2 + k_) % NG
                    gb = gth[gi_]
                    GK = ("gth", gi_)
                    S.op("pool", (lambda gb=gb, col=col: (lambda e: e.indirect_dma_start(
                        out=gb, out_offset=None, in_=Y_d, in_offset=bass.IndirectOffsetOnAxis(ap=idxi[:, col:col + 1], axis=0),
                        bounds_check=bcreg(e), oob_is_err=False)))(), ["Y_d", "idxi", GK], [GK], dma=True)

            def combine(j):
                x_t = xs2[j % 4]
                XT = ("xs2", j % 4)
                K.act(x_t, x_t, AF.Copy, [XT], [XT], scale=ALPHA)
                yield
                for k_ in range(2):
                    col = k_ * 16 + j
                    gi_ = (j * 2 + k_) % NG
                    K.stt("dve", x_t, gth[gi_], wts[:, col:col + 1], x_t, ALU.mult, ALU.add, [("gth", gi_), XT, "wts"], [XT])
                    yield
                yield from ln_gen(x_t, [XT], st8[j % 4], ("st8", j % 4), lnbc, "lnbc", 0)
                finals.append(K.dma("sp", out[j * 128:(j + 1) * 128, :], x_t, [XT], []))

            for j in range(4):
                fetch(j)
            for j0 in range(0, TO, 2):
                interleave(combine(j0), combine(j0 + 1))
                for j in (j0 + 4, j0 + 5):
                    if j < TO:
                        fetch(j)
        finals = [f_ for f_ in finals if f_.idx is not None and f_.idx < len(S.ops) and S.ops[f_.idx] is f_] + [o for o in S.ops if o.is_dma and o.eng == "sp"][-NDSEM:]
        counts = S.emit(finals)
        print("op counts", counts)
    return nc, counts


def _const_mats():
    cm = np.zeros((128, 776), np.float32)
    cm[:, 0:128] = np.eye(128, dtype=np.float32)
    p = np.arange(128)
    tri = ((p[:, None] // 64 == p[None, :] // 64) & (p[:, None] <= p[None, :])).astype(np.float32)
    cm[:, 128:256] = tri
    cm[:, 256:384] = (p[:, None] <= p[None, :]).astype(np.float32)
    cm[0:64, 384] = 1.0
    cm[64:128, 385] = 1.0
    for i in range(32):
        cm[i, 386 + 64 + i] = 1.0
    cm[:, 482:610] = 1.0
    cm[:, 610:738] = (p[:, None] < p[None, :]).astype(np.float32)
    cm[:, 738:770] = (np.arange(32) * CAP)[None, :]
    cv = np.zeros((128, 16), np.float32)
    cv[:, 0] = 1.0
    cv[:, 1] = LN_EPS
    cv[:, 2] = RMS_EPS
    return cm, cv


def _core_inputs(x, meta_tokens, half):
    xin = np.zeros((NPOS, D), np.float32)
    valid = np.zeros((NPOS,), np.float32)
    pos = np.zeros((NPOS,), np.float64)
    if half == 0:
        xin[NPREV - 16:NPREV] = meta_tokens
        valid[NPREV - 16:] = 1.0
        pos[NPREV - 16:NPREV] = np.arange(16)
        xin[NPREV:] = x[0:2048]
        pos[NPREV:] = 16 + np.arange(2048)
    else:
        m0 = NPREV - 2048 - 16
        xin[m0:m0 + 16] = meta_tokens
        xin[m0 + 16:NPREV] = x[0:2048]
        valid[m0:] = 1.0
        pos[m0:m0 + 16] = np.arange(16)
        pos[m0 + 16:NPREV] = 16 + np.arange(2048)
        xin[NPREV:] = x[2048:4096]
        pos[NPREV:] = 16 + 2048 + np.arange(2048)
    vk = np.zeros((128, 2 * TT), np.float32)
    vk[:, 0:TT] = valid.reshape(TT, 128).T
    vk[:, TT:] = ((valid - 1.0) * (-NEG)).reshape(TT, 128).T
    inv_freq = (10000.0 ** (-np.arange(0, 32, 2, dtype=np.float32) / 32)).astype(np.float32)
    ang = (pos.astype(np.float32)[:, None] * inv_freq[None, :]).astype(np.float32)
    cos = np.cos(ang).T.astype(np.float32)
    sin = np.sin(ang).T.astype(np.float32)
    cskv = np.zeros((32, 2, NPOS), np.float32)
    cskv[0:16, 0] = cos
    cskv[16:32, 0] = cos
    cskv[0:16, 1] = -sin
    cskv[16:32, 1] = sin
    csq = np.zeros((96, 2, NOWN), np.float32)
    csq[0:64, 0] = 1.0
    csq[64:96, 0] = cskv[:, 0, NPREV:]
    csq[64:96, 1] = cskv[:, 1, NPREV:]
    return xin, vk, cskv, csq


def _shared_inputs(inp):
    f = lambda a: np.ascontiguousarray(np.asarray(a, dtype=np.float32))
    cm, cv = _const_mats()
    w_in = f(inp["w_in"][0])
    kr0 = 512 + 512 + 1024 + 1024 + 16 + 384 + 256
    wkr2 = np.zeros((D, 2, 32), np.float32)
    wkr2[:, 0] = w_in[:, kr0:kr0 + 32]
    wkr2[:, 1, 0:16] = w_in[:, kr0 + 16:kr0 + 32]
    wkr2[:, 1, 16:32] = w_in[:, kr0:kr0 + 16]
    w2a = np.zeros((33, 512), np.float32)
    w2a[0:16] = f(inp["gla_gate_w2"][0])
    w2a[32] = f(inp["gla_gate_b"][0])
    wuq = f(inp["mla_w_uq"][0]).reshape(384, 16, 96)
    wuq2 = np.zeros((384, 16, 2, 96), np.float32)
    wuq2[:, :, 0] = wuq
    wuq2[:, :, 1, 64:80] = wuq[:, :, 80:96]
    wuq2[:, :, 1, 80:96] = wuq[:, :, 64:80]
    wukp = np.zeros((256, 16, 96), np.float32)
    wukp[:, :, 0:64] = f(inp["mla_w_uk"][0]).reshape(256, 16, 64)
    lng = np.stack([np.stack([f(inp["ln_emb_g"]), f(inp["ln_emb_b"])]),
                    np.stack([f(inp["ln_mix_g"][0]), f(inp["ln_mix_b"][0])]),
                    np.stack([f(inp["ln_ffn_g"][0]), f(inp["ln_ffn_b"][0])])])
    gcols = np.zeros((128, 8), np.float32)
    gcols[:, 0:2] = f(inp["gla_norm_g"][0]).reshape(2, 128).T
    gcols[:, 2:5] = f(inp["mla_q_norm_g"][0]).reshape(3, 128).T
    gcols[:, 5:7] = f(inp["mla_kv_norm_g"][0]).reshape(2, 128).T
    sh = {
        "cm": cm, "cv": cv, "lng": f(lng), "w_in": w_in, "wkr2": wkr2, "w2a": w2a, "gcols": gcols,
        "wuq2": wuq2, "wukp": wukp, "wuv": f(inp["mla_w_uv"][0]),
        "wbg": f(inp["w_branch_gla"][0]), "wbm": f(inp["w_branch_mla"][0]), "wout": f(inp["w_out"][0]),
        "wr": f(np.concatenate([inp["router_group_w"][0], inp["router_expert_w"][0]], axis=1)),
        "rbias": f(np.concatenate([inp["router_group_b"][0], inp["router_expert_b"][0]], axis=0)),
        "ewg": f(np.asarray(inp["expert_w_gate"][0]).reshape(32, D, 256)),
        "ewu": f(np.asarray(inp["expert_w_up"][0]).reshape(32, D, 256)),
        "ewd": f(np.asarray(inp["expert_w_down"][0]).reshape(32, 256, D)),
    }
    return sh


_NC_CACHE = {}


def kernel(**inputs):
    inp = {k: np.asarray(v) for k, v in inputs.items()}
    x = inp["x"].astype(np.float32)
    meta = inp["meta_tokens"].astype(np.float32)
    sh = _shared_inputs(inp)
    in_maps = []
    for c in range(8):
        b, half = c // 2, c % 2
        xin, vk, cskv, csq = _core_inputs(x[b], meta, half)
        m = dict(sh)
        m.update({"xin": xin, "vk": vk, "cskv": cskv, "csq": csq})
        in_maps.append(m)
    if "nc" not in _NC_CACHE:
        _NC_CACHE["nc"] = build(False)[0]
    res = run_bass_kernel_spmd(_NC_CACHE["nc"], in_maps, core_ids=list(range(8)))
    out = np.zeros((4, 4096, D), np.float32)
    for c in range(8):
        b, half = c // 2, c % 2
        out[b, half * 2048:(half + 1) * 2048] = res.results[c]["out"]
    return out
```

```python
import contextlib
import numpy as np
import concourse.bass as bass
import concourse.mybir as mybir
from concourse.bass_utils import run_bass_kernel_spmd

F32 = mybir.dt.float32
BF16 = mybir.dt.bfloat16
AF = mybir.ActivationFunctionType
ALU = mybir.AluOpType
AX = mybir.AxisListType

NDSEM = 8
D = 1024
NPREV = 2176
NOWN = 2048
NPOS = NPREV + NOWN
TP = NPREV // 128
TO = NOWN // 128
TT = TP + TO
W_RES = 3760
ALPHA = 2.0 ** 0.25
LN_EPS = 1e-5
RMS_EPS = 1e-6
NEG = -30000.0
CAP = 256


class Op:
    __slots__ = ("eng", "fn", "waits", "signal", "idx", "sig_no", "dsem", "dtarget", "is_dma", "prewait")

    def __init__(self, eng, fn, is_dma):
        self.eng = eng
        self.fn = fn
        self.waits = []
        self.signal = False
        self.sig_no = None
        self.is_dma = is_dma
        self.dsem = None
        self.dtarget = None
        self.prewait = None
        self.idx = None


class Sched:
    ENGS = ("pe", "act", "dve", "pool", "sp")

    def __init__(self, nc):
        self.nc = nc
        self.ops = []
        self.last_w = {}
        self.readers = {}
        self.bar = None
        self.bar_passed = set()
        self.last_op = {}
        self.dma_recent = {}
        self.mute = False

    def barrier(self):
        b = [o for o in self.last_op.values()]
        for lst in self.dma_recent.values():
            b.extend(lst)
        self.bar = b
        self.bar_passed = set()
        self.last_w = {}
        self.readers = {}

    def op(self, eng, fn, reads=(), writes=(), dma=False):
        o = Op(eng, fn, dma)
        if self.mute:
            return o
        o.idx = len(self.ops)
        deps = set()
        for r in reads:
            w = self.last_w.get(r)
            if w is not None:
                deps.add(w)
            if isinstance(r, tuple) and r[0] == "pb":
                for rd in self.readers.get(r, ()):
                    if rd.eng != eng:
                        deps.add(rd)
        for r in writes:
            w = self.last_w.get(r)
            if w is not None:
                deps.add(w)
            for rd in self.readers.get(r, ()):
                deps.add(rd)
        for d in deps:
            if d.eng == eng and not d.is_dma and not dma:
                if eng == "pe":
                    continue
            o.waits.append(d)
            d.signal = True
        if self.bar is not None and eng not in self.bar_passed:
            self.bar_passed.add(eng)
            for d in self.bar:
                if d not in o.waits:
                    o.waits.append(d)
                    d.signal = True
        for r in reads:
            self.readers.setdefault(r, []).append(o)
        for r in writes:
            self.last_w[r] = o
            self.readers[r] = []
        self.ops.append(o)
        if dma:
            lst = self.dma_recent.setdefault(eng, [])
            lst.append(o)
            if len(lst) > NDSEM:
                lst.pop(0)
        else:
            self.last_op[eng] = o
        return o

    def emit(self, final_ops):
        nc = self.nc
        for fo in final_ops:
            fo.signal = True
        cnt = {e: 0 for e in self.ENGS}
        dcnt = {}
        dma_i = {e: 0 for e in self.ENGS}
        dma_hist = {}
        for o in self.ops:
            if o.is_dma:
                i = dma_i[o.eng]
                dma_i[o.eng] += 1
                slot = (o.eng, i % NDSEM)
                o.prewait = dma_hist.get(slot)
                dcnt[slot] = dcnt.get(slot, 0) + 16
                o.dsem = slot
                o.dtarget = dcnt[slot]
                dma_hist[slot] = o
            elif o.signal:
                cnt[o.eng] += 1
                o.sig_no = cnt[o.eng]
        with contextlib.ExitStack() as st:
            esem = {e: st.enter_context(nc.semaphore("s_" + e)) for e in ("pe", "act", "dve", "pool")}
            dsem = {}
            for e in ("sp", "pool", "act"):
                if dma_i[e] > 0:
                    for j in range(NDSEM):
                        dsem[(e, j)] = st.enter_context(nc.semaphore("d_%s%d" % (e, j)))
            block = st.enter_context(nc.Block())
            per = {e: [o for o in self.ops if o.eng == e] for e in self.ENGS}

            def run(ename, eng):
                waited = {}

                def wait_for(p):
                    if p.is_dma:
                        key, val, sem = p.dsem, p.dtarget, dsem[p.dsem]
                    else:
                        key, val, sem = p.eng, p.sig_no, esem[p.eng]
                    if waited.get(key, 0) >= val:
                        return
                    waited[key] = val
                    eng.wait_ge(sem, val)

                for o in per[ename]:
                    if o.is_dma and o.prewait is not None:
                        wait_for(o.prewait)
                    for p in o.waits:
                        wait_for(p)
                    ins = o.fn(eng)
                    if o.is_dma:
                        ins.then_inc(dsem[o.dsem], 16)
                    elif o.signal:
                        ins.then_inc(esem[o.eng], 1)
                if ename == "sp":
                    for fo in final_ops:
                        wait_for(fo)

            @block.tensor
            def _(eng):
                run("pe", eng)

            @block.scalar
            def _(eng):
                run("act", eng)

            @block.vector
            def _(eng):
                run("dve", eng)

            @block.gpsimd
            def _(eng):
                run("pool", eng)

            @block.sync
            def _(eng):
                run("sp", eng)
        return {e: len(per[e]) for e in self.ENGS}


class KB:
    def __init__(self, nc, st):
        self.nc = nc
        self.st = st
        self.S = Sched(nc)
        self.nps = 0
        self.banks = []
        self.uid = 0

    def sb(self, name, shape, dt):
        return self.st.enter_context(self.nc.sbuf_tensor(name, shape, dt))

    def init_psum(self):
        for i in range(8):
            self.banks.append(self.st.enter_context(self.nc.psum_tensor("pb%d" % i, [128, 512], F32)))
        self.open = [False] * 8

    def pb(self):
        for _ in range(8):
            i = self.nps % 8
            self.nps += 1
            if not self.open[i]:
                self.open[i] = True
                return self.banks[i], ("pb", i)
        raise RuntimeError("all PSUM banks open")

    def done(self, *keys):
        for k in keys:
            assert self.open[k[1]], k
            self.open[k[1]] = False

    def mm(self, out, lhsT, rhs, start, stop, r, w, skip=False):
        if skip:
            return self.S.op("pe", lambda e: e.matmul(out, lhsT=lhsT, rhs=rhs, start=start, stop=stop, skip_group_check=True), r, w)
        return self.S.op("pe", lambda e: e.matmul(out, lhsT=lhsT, rhs=rhs, start=start, stop=stop), r, w)

    def tr(self, out, in_, ident, r, w):
        return self.S.op("pe", lambda e: e.transpose(out, in_, ident), r, w)

    def act(self, out, in_, func, r, w, bias=None, scale=None, accum=None):
        kw = {}
        if bias is not None:
            kw["bias"] = bias
        if scale is not None:
            kw["scale"] = scale
        if accum is not None:
            kw["accum_out"] = accum
        return self.S.op("act", lambda e: e.activation(out=out, in_=in_, func=func, **kw), r, w)

    def tt(self, eng, out, in0, in1, op, r, w):
        return self.S.op(eng, lambda e: e.tensor_tensor(out=out, in0=in0, in1=in1, op=op), r, w)

    def ts(self, eng, out, in0, s1, s2, op0, op1, r, w, accum=None):
        if op1 is None:
            return self.S.op(eng, lambda e: e.tensor_scalar(out=out, in0=in0, scalar1=s1, scalar2=None, op0=op0), r, w)
        if accum is not None:
            return self.S.op(eng, lambda e: e.tensor_scalar(out=out, in0=in0, scalar1=s1, scalar2=s2, op0=op0, op1=op1, accum_out=accum), r, w)
        return self.S.op(eng, lambda e: e.tensor_scalar(out=out, in0=in0, scalar1=s1, scalar2=s2, op0=op0, op1=op1), r, w)

    def stt(self, eng, out, in0, scalar, in1, op0, op1, r, w):
        return self.S.op(eng, lambda e: e.scalar_tensor_tensor(out=out, in0=in0, scalar=scalar, in1=in1, op0=op0, op1=op1), r, w)

    def cp(self, eng, out, in_, r, w):
        if eng == "act":
            return self.S.op("act", lambda e: e.activation(out=out, in_=in_, func=AF.Copy), r, w)
        return self.S.op(eng, lambda e: e.tensor_copy(out=out, in_=in_), r, w)

    def recip(self, out, in_, r, w):
        return self.S.op("dve", lambda e: e.reciprocal(out=out, in_=in_), r, w)

    def red(self, out, in_, op, r, w):
        return self.S.op("dve", lambda e: e.tensor_reduce(out=out, in_=in_, axis=AX.X, op=op), r, w)

    def memset(self, eng, ap, val, w):
        return self.S.op(eng, lambda e: e.memset(ap, val), (), w)

    def dma(self, q, out, in_, r, w):
        return self.S.op(q, lambda e: e.dma_start(out=out, in_=in_), r, w, dma=True)


ARENA_BYTES = 176 * 1024


class Arena:
    def __init__(self, K):
        self.f = K.sb("arena", [128, ARENA_BYTES // 4], F32)
        self.fa = self.f[:]
        self.ba = self.f[:].bitcast(BF16)
        self.off = 0

    def set(self, off):
        self.off = off

    def F(self, n, parts=128):
        assert self.off % 4 == 0
        o = self.off // 4
        self.off += n * 4
        assert self.off <= ARENA_BYTES, self.off
        return self.fa[0:parts, o:o + n]

    def B(self, n, parts=128):
        assert self.off % 4 == 0
        o = self.off // 2
        self.off += ((n * 2 + 3) // 4) * 4
        assert self.off <= ARENA_BYTES, self.off
        return self.ba[0:parts, o:o + n]


def build(debug=False, stages=5, only=None, flags=()):
    nc = bass.Bass("TRN2", target_bir_lowering=False)

    def din(name, shape):
        return nc.dram_tensor(name, list(shape), F32, kind="ExternalInput").ap()

    xin = din("xin", [NPOS, D])
    vk = din("vk", [128, 2 * TT])
    cskv = din("cskv", [32, 2, NPOS])
    csq = din("csq", [96, 2, NOWN])
    cm = din("cm", [128, 776])
    cv = din("cv", [128, 16])
    lng = din("lng", [3, 2, D])
    w_in = din("w_in", [D, 5808])
    wkr2 = din("wkr2", [D, 2, 32])
    w2a = din("w2a", [33, 512])
    gcols = din("gcols", [128, 8])
    wuq2 = din("wuq2", [384, 16, 2, 96])
    wukp = din("wukp", [256, 16, 96])
    wuv = din("wuv", [256, 1024])
    wbg = din("wbg", [1024, 1024])
    wbm = din("wbm", [1024, 1024])
    wout = din("wout", [1024, 1024])
    wr = din("wr", [D, 36])
    rbias = din("rbias", [36])
    ewg = din("ewg", [32, D, 256])
    ewu = din("ewu", [32, D, 256])
    ewd = din("ewd", [32, 256, D])
    out = nc.dram_tensor("out", [NOWN, D], F32, kind="ExternalOutput").ap()
    okind = "ExternalOutput" if debug else "Internal"
    oglaT_d = nc.dram_tensor("oglaT_d", [128, 8, NOWN], BF16, kind=okind).ap()
    sT_d = nc.dram_tensor("sT_d", [128, 8, NOWN], BF16, kind=okind).ap()
    s_d = nc.dram_tensor("s_d", [NOWN, D], F32, kind=okind).ap()
    s2_d = nc.dram_tensor("s2_d", [NOWN, D], F32, kind=okind).ap()
    G_d = nc.dram_tensor("G_d", [32 * CAP, D], BF16, kind="Internal").ap()
    Y_d = nc.dram_tensor("Y_d", [32 * CAP, D], BF16, kind="Internal").ap()
    ewg_b = nc.dram_tensor("ewg_b", [32, D, 256], BF16, kind="Internal").ap()
    ewu_b = nc.dram_tensor("ewu_b", [32, D, 256], BF16, kind="Internal").ap()
    ewd_b = nc.dram_tensor("ewd_b", [32, 256, D], BF16, kind="Internal").ap()
    sparse = "dense" not in flags
    bc_cache = {}

    def bcreg(e):
        if "r" not in bc_cache:
            bc_cache["r"] = e.to_reg(32 * CAP - 1)
        return bc_cache["r"]
    dbg = {}
    if debug:
        dbg["omlaT"] = nc.dram_tensor("omlaT_d", [128, 8, NOWN], BF16, kind="ExternalOutput").ap()
        dbg["comb"] = nc.dram_tensor("comb_d", [128, TO, 32], F32, kind="ExternalOutput").ap()
        dbg["ckvnT"] = nc.dram_tensor("ckvnT_d", [128, 2, NPOS], BF16, kind="ExternalOutput").ap()
        dbg["kropeT"] = nc.dram_tensor("kropeT_d", [32, NPOS], BF16, kind="ExternalOutput").ap()
        dbg["cqnT"] = nc.dram_tensor("cqnT_d", [128, 3, NOWN], BF16, kind="ExternalOutput").ap()

    with contextlib.ExitStack() as st:
        K = KB(nc, st)
        S = K.S
        sb = K.sb
        K.init_psum()
        finals = []
        cmt = sb("cmt", [128, 776], F32)
        cvt = sb("cvt", [128, 16], F32)
        vkt = sb("vkt", [128, 2 * TT], F32)
        cmb = sb("cmb", [128, 640], BF16)
        gcol = sb("gcol", [128, 8], F32)
        st8 = [sb("st8_%d" % i, [128, 8], F32) for i in range(4)]
        comb = sb("comb", [128, TO, 32], F32)
        idxi = sb("idxi", [128, 32], mybir.dt.int32)
        wts = sb("wts", [128, 32], F32)
        zt = sb("zt", [128, 2048], BF16)
        junk = sb("junk", [128, D], BF16)
        junk2 = [junk, sb("junkb", [128, D], BF16)]
        K.dma("sp", cmt[:], cm, (), ["cmt"])
        K.dma("sp", cvt[:], cv, (), ["cvt"])
        K.dma("sp", vkt[:], vk, (), ["vkt"])
        K.dma("sp", gcol[:], gcols, (), ["gcol"])
        K.cp("dve", cmb[:], cmt[:, 0:640], ["cmt"], ["cmb"])
        K.memset("pool", zt[:], 0.0, ["zt"])
        ident = cmt[:, 0:128]
        triT = cmt[:, 128:256]
        chsel = cmt[:, 384:386]
        ones_b = cmb[:, 482:610]
        ut_b = cmb[:, 256:384]
        tri_b = cmb[:, 128:256]
        selr_b = cmb[0:32, 386:482]
        c_one = cvt[:, 0:1]
        c_lneps = cvt[:, 1:2]
        c_rmseps = cvt[:, 2:3]
        C0 = ["cmt", "cvt", "vkt", "cmb", "gcol"]
        A = Arena(K)

        def v3(ap, n):
            return ap.rearrange("p (c n) -> p c n", n=n)

        def ln_tok(x_t, XT, stt_, ST, lnbc, LK, gi):
            K.act(junk[:], x_t, AF.Identity, [XT], ["junk", ST], accum=stt_[:, 0:1])
            K.act(junk[:], x_t, AF.Square, [XT], ["junk", ST], accum=stt_[:, 1:2])
            K.ts("dve", stt_[:, 2:3], stt_[:, 0:1], 1.0 / D, None, ALU.mult, None, [ST], [ST])
            K.tt("dve", stt_[:, 3:4], stt_[:, 2:3], stt_[:, 2:3], ALU.mult, [ST], [ST])
            K.stt("dve", stt_[:, 4:5], stt_[:, 1:2], 1.0 / D, stt_[:, 3:4], ALU.mult, ALU.subtract, [ST], [ST])
            K.act(stt_[:, 5:6], stt_[:, 4:5], AF.Sqrt, [ST] + C0, [ST], bias=c_lneps, scale=1.0)
            K.recip(stt_[:, 5:6], stt_[:, 5:6], [ST], [ST])
            K.stt("dve", stt_[:, 6:7], stt_[:, 2:3], -1.0, stt_[:, 5:6], ALU.mult, ALU.mult, [ST], [ST])
            K.act(x_t, x_t, AF.Identity, [XT, ST], [XT], bias=stt_[:, 6:7], scale=stt_[:, 5:6])
            K.tt("dve", x_t, x_t, lnbc[:, gi, :], ALU.mult, [XT, LK], [XT])
            K.tt("dve", x_t, x_t, lnbc[:, gi + 1, :], ALU.add, [XT, LK], [XT])

        A.set(0)
        ckvnT = v3(A.B(2 * NPOS), NPOS)
        cqnT = v3(A.B(3 * NOWN), NOWN)
        kropeT = A.B(NPOS, parts=32)
        P1_END = A.off
        OMLA_OFF = P1_END

        S.mute = only is not None and 1 not in only
        wres = v3(A.B(8 * W_RES), W_RES)
        wkr = v3(A.B(8 * 64), 64)
        sTw = [v3(A.B(1024), 128) for i in range(2)]
        ktb = [A.B(512) for i in range(2)]
        vb = [A.B(1024) for i in range(2)]
        q0T = [v3(A.B(512), 128) for i in range(2)]
        q1T = [v3(A.B(512), 128) for i in range(2)]
        ktT = v3(A.B(512), 128)
        attm = [v3(A.B(512), 128) for i in range(2)]
        Sbf = [[A.B(256) for i in range(3)] for h in range(4)]
        sqb = A.B(1024)
        oglaT = [v3(A.B(1024), 128) for i in range(2)]
        sqm = A.B(640)
        S32 = [A.F(256) for h in range(4)]
        loga = A.F(512)
        e2 = A.F(512)
        e1T = A.F(512)
        e2T = A.F(512)
        silur2 = [A.F(1024) for i in range(2)]
        rstdg = A.F(512)
        aaug = A.F(128, parts=33)
        w2t = A.F(512, parts=33)
        dec2 = [A.F(8) for i in range(2)]
        cs32 = [v3(A.F(256, parts=32), 128) for i in range(2)]
        rsm = A.F(128)
        tmpS = [A.F(256) for h in range(4)]
        xt = [A.F(D) for i in range(2)]
        lnbc = v3(A.F(2 * D), D)
        print("stage1 arena end", A.off)

        K.dma("sp", lnbc[:, 0, :], lng[0, 0].partition_broadcast(128), (), ["lnbc"])
        K.dma("sp", lnbc[:, 1, :], lng[0, 1].partition_broadcast(128), (), ["lnbc"])
        w_in_v = w_in.rearrange("(c p) n -> p c n", p=128)
        for c in range(8):
            K.dma("pool", wres[:, c, :], w_in_v[:, c, 0:W_RES], (), [("wres", c)])
        K.dma("pool", wkr, wkr2.rearrange("(c p) a n -> p c (a n)", p=128), (), ["wkr"])
        K.dma("sp", w2t, w2a, (), ["w2t"])
        K.memset("dve", aaug, 0.0, ["aaug"])
        K.memset("dve", aaug[32:33, :], 1.0, ["aaug"])
        for h in range(4):
            K.memset("pool", S32[h], 0.0, [("S32", h)])
            K.memset("pool", Sbf[h][0], 0.0, [("Sbf", h, 0)])
        for i in range(2):
            K.memset("pool", q0T[i], 0.0, [("q0T", i)])
            K.memset("pool", q1T[i], 0.0, [("q1T", i)])
        sver = [0, 0, 0, 0]

        def X(i):
            own = i >= TP
            j = i - TP
            p0 = i * 128
            b2 = i % 2
            x_t = xt[b2]
            XT = ("xt", b2)
            stt_ = st8[i % 4]
            ST = ("st8", i % 4)
            valid = vkt[:, i:i + 1]
            K.dma("sp", x_t, xin[p0:p0 + 128, :], (), [XT])
            ln_tok(x_t, XT, stt_, ST, lnbc, "lnbc", 0)
            if own:
                K.dma("sp", s_d[j * 128:(j + 1) * 128, :], x_t, [XT], ["s_d"])
            yield
            sT_t = sTw[b2]
            STK = ("sTw", b2)
            for half in range(2):
                pt, PK = K.pb()
                for cc in range(4):
                    c = half * 4 + cc
                    K.tr(pt[:, cc * 128:(cc + 1) * 128], x_t[:, c * 128:(c + 1) * 128], ident, [XT] + C0, [PK])
                K.cp("dve" if half == 0 else "act", sT_t[:, half * 4:half * 4 + 4, :], v3(pt[:, :], 128), [PK], [STK])
                K.done(PK)
            if own:
                K.dma("sp", sT_d[:, :, j * 128:(j + 1) * 128], sT_t, [STK], ["sT_d"])
            yield

            def proj_tok(col0, ncols):
                pt, PK = K.pb()
                for c in range(8):
                    K.mm(pt[:, 0:ncols], sT_t[:, c, :], wres[:, c, col0:col0 + ncols], c == 0, c == 7, [STK, ("wres", c)], [PK])
                return pt, PK

            def proj_feat(pt, PK, off, wsrc, col0, m, wkey):
                for c in range(8):
                    K.mm(pt[0:m, off:off + 128], wsrc[:, c, col0:col0 + m], sT_t[:, c, :], c == 0, c == 7, [STK, wkey if wkey else ("wres", c)], [PK])

            pa, PKa = K.pb()
            proj_feat(pa, PKa, 0, wres, 3072, 16, None)
            K.cp("dve", aaug[0:16, :], pa[0:16, 0:128], [PKa], ["aaug"])
            K.done(PKa)
            pz, PKz = K.pb()
            K.mm(pz[:, :], aaug, w2t, True, True, ["aaug", "w2t"], [PKz])
            lg = loga
            LG = "loga"
            K.act(lg, pz[:, :], AF.Exp, [PKz], [LG], scale=-1.0)
            K.done(PKz)
            K.act(lg, lg, AF.Ln, [LG] + C0, [LG], bias=c_one, scale=1.0)
            K.ts("dve", lg, lg, valid, -1.0 / 16.0, ALU.mult, ALU.mult, [LG] + C0, [LG])
            yield
            v_b = vb[b2]
            VB = ("vb", b2)
            for hv in range(2):
                pv, PKv = proj_tok(1024 + hv * 512, 512)
                K.act(v_b[:, hv * 512:(hv + 1) * 512], pv[:, :], AF.Copy, [PKv] + C0, [VB], scale=valid)
                K.done(PKv)
                yield
            pbc, PKbc = K.pb()
            K.mm(pbc[:, :], triT, lg, True, True, [LG] + C0, [PKbc])
            K.act(e2, pbc[:, :], AF.Exp, [PKbc], ["e2"], scale=-1.0)
            K.done(PKbc)
            pk, PKk = proj_tok(512, 512)
            k_t = ktb[b2]
            KT = ("ktb", b2)
            K.tt("dve", k_t, pk[:, :], e2, ALU.mult, [PKk, "e2"], [KT])
            K.done(PKk)
            pd, PKd = K.pb()
            for h in range(4):
                K.mm(pd[:, h * 2:h * 2 + 2], lg[:, h * 128:(h + 1) * 128], chsel, True, True, [LG] + C0, [PKd])
            K.act(dec2[b2], pd[:, 0:8], AF.Exp, [PKd], [("dec", b2)])
            K.done(PKd)
            yield
            if own:
                pbT, PKbT = K.pb()
                for h in range(4):
                    K.mm(pbT[:, h * 128:(h + 1) * 128], lg[:, h * 128:(h + 1) * 128], triT, True, True, [LG] + C0, [PKbT])
                K.act(e1T, pbT[:, :], AF.Exp, [PKbT], ["e1T"])
                K.act(e2T, pbT[:, :], AF.Exp, [PKbT], ["e2T"], scale=-1.0)
                K.done(PKbT)
                pq, PKq = K.pb()
                for h in range(4):
                    proj_feat(pq, PKq, h * 128, wres, h * 128, 128, None)
                q0 = q0T[b2]
                q1 = q1T[b2]
                Q0 = ("q0T", b2)
                Q1 = ("q1T", b2)
                K.stt("dve", q0[:, :, 0:64], v3(pq[:, :], 128)[:, :, 0:64], 128.0 ** -0.5, v3(e1T, 128)[:, :, 0:64], ALU.mult, ALU.mult, [PKq, "e1T"], [Q0])
                K.stt("dve", q1[:, :, 64:128], v3(pq[:, :], 128)[:, :, 64:128], 128.0 ** -0.5, v3(e1T, 128)[:, :, 64:128], ALU.mult, ALU.mult, [PKq, "e1T"], [Q1])
                K.done(PKq)
                yield
                pkT, PKkT = K.pb()
                for h in range(4):
                    proj_feat(pkT, PKkT, h * 128, wres, 512 + h * 128, 128, None)
                k_T = ktT
                KTT = "ktT"
                K.tt("dve", k_T.rearrange("p h n -> p (h n)"), pkT[:, :], e2T, ALU.mult, [PKkT, "e2T"], [KTT])
                K.done(PKkT)
                yield
                pat, PKat = K.pb()
                for h in range(4):
                    K.mm(pat[:, h * 128:(h + 1) * 128], k_T[:, h, :], q0[:, h, :], True, False, [KTT, Q0], [PKat])
                    K.mm(pat[:, h * 128:(h + 1) * 128], k_T[:, h, :], q1[:, h, :], False, True, [KTT, Q1], [PKat])
                a_m = attm[b2]
                AM = ("attm", b2)
                for h in range(4):
                    K.tt("dve", a_m[:, h, :], pat[:, h * 128:(h + 1) * 128], tri_b, ALU.mult, [PKat] + C0, [AM])
                K.done(PKat)
                yield
                for hr in range(2):
                    pr_, PKr_ = K.pb()
                    for vc in range(4):
                        proj_feat(pr_, PKr_, vc * 128, wres, 2048 + (hr * 4 + vc) * 128, 128, None)
                    K.act(silur2[b2][:, hr * 512:(hr + 1) * 512], pr_[:, :], AF.Silu, [PKr_], [("silur", b2)])
                    K.done(PKr_)
                    yield
            pc, PKc = K.pb()
            proj_feat(pc, PKc, 0, wres, 3472, 128, None)
            proj_feat(pc, PKc, 128, wres, 3600, 128, None)
            proj_feat(pc, PKc, 256, wkr, 0, 32, "wkr")
            proj_feat(pc, PKc, 384, wkr, 32, 32, "wkr")
            sm = sqm
            SM = "sqm"
            K.act(sm[:, 0:256], pc[:, 0:256], AF.Square, [PKc], [SM])
            pr, PKr = K.pb()
            K.mm(pr[:, 0:128], ones_b, sm[:, 0:128], True, False, [SM] + C0, [PKr])
            K.mm(pr[:, 0:128], ones_b, sm[:, 128:256], False, True, [SM] + C0, [PKr])
            K.act(rsm, pr[:, 0:128], AF.Sqrt, [PKr] + C0, ["rsm"], bias=c_rmseps, scale=1.0 / 256.0)
            K.done(PKr)
            K.recip(rsm, rsm, ["rsm"], ["rsm"])
            for c in range(2):
                K.stt("dve", ckvnT[:, c, p0:p0 + 128], pc[:, c * 128:(c + 1) * 128], gcol[:, 5 + c:6 + c], rsm, ALU.mult, ALU.mult,
                      [PKc, "rsm"] + C0, [("ckvnT", i)])
            cs_ = cs32[b2]
            CS = ("cs32", b2)
            K.dma("sp", cs_, cskv[:, :, p0:p0 + 128], (), [CS])
            K.tt("dve", cs_[:, 0, :], pc[0:32, 256:384], cs_[:, 0, :], ALU.mult, [PKc, CS], [CS])
            K.tt("dve", cs_[:, 1, :], pc[0:32, 384:512], cs_[:, 1, :], ALU.mult, [PKc, CS], [CS])
            K.done(PKc)
            K.tt("pool", kropeT[:, p0:p0 + 128], cs_[:, 0, :], cs_[:, 1, :], ALU.add, [CS], [("kropeT", i)])
            yield
            if own:
                pq3, PKq3 = K.pb()
                for c in range(3):
                    proj_feat(pq3, PKq3, c * 128, wres, 3088 + c * 128, 128, None)
                K.act(sm[:, 256:640], pq3[:, 0:384], AF.Square, [PKq3], [SM])
                pr, PKr = K.pb()
                for c in range(3):
                    K.mm(pr[:, 0:128], ones_b, sm[:, 256 + c * 128:256 + (c + 1) * 128], c == 0, c == 2, [SM] + C0, [PKr])
                K.act(rsm, pr[:, 0:128], AF.Sqrt, [PKr] + C0, ["rsm"], bias=c_rmseps, scale=1.0 / 384.0)
                K.done(PKr)
                K.recip(rsm, rsm, ["rsm"], ["rsm"])
                for c in range(3):
                    K.stt("dve", cqnT[:, c, j * 128:(j + 1) * 128], pq3[:, c * 128:(c + 1) * 128], gcol[:, 2 + c:3 + c], rsm, ALU.mult, ALU.mult,
                          [PKq3, "rsm"] + C0, [("cqnT", j)])
                K.done(PKq3)
                yield

        def Y(i):
            own = i >= TP
            j = i - TP
            b2 = i % 2
            v_b = vb[b2]
            VB = ("vb", b2)
            k_t = ktb[b2]
            KT = ("ktb", b2)
            dec = dec2[b2]
            DK = ("dec", b2)

            def state_update(ch):
                for h in range(4):
                    pu, PKu = K.pb()
                    K.mm(pu[:, 0:256], k_t[ch * 64:(ch + 1) * 64, h * 128:(h + 1) * 128], v_b[ch * 64:(ch + 1) * 64, h * 256:(h + 1) * 256],
                         True, True, [KT, VB], [PKu])
                    K.tt("dve", tmpS[h], pu[:, 0:256], S32[h], ALU.add, [PKu, ("S32", h)], [("tmpS", h)])
                    K.done(PKu)
                    nv = (sver[h] + 1) % 3
                    K.act(Sbf[h][nv], tmpS[h], AF.Copy, [("tmpS", h), DK], [("Sbf", h, nv)], scale=dec[:, h * 2 + ch:h * 2 + ch + 1])
                    K.ts("dve", S32[h], tmpS[h], dec[:, h * 2 + ch:h * 2 + ch + 1], None, ALU.mult, None, [("tmpS", h), DK], [("S32", h)])
                    sver[h] = nv
                    if h % 2 == 1:
                        yield

            if not own:
                yield from state_update(0)
                yield from state_update(1)
                return
            q0 = q0T[b2]
            q1 = q1T[b2]
            Q0 = ("q0T", b2)
            Q1 = ("q1T", b2)
            a_m = attm[b2]
            AM = ("attm", b2)
            silur = silur2[b2]
            SR = ("silur", b2)
            po0, PKo0 = K.pb()
            po1, PKo1 = K.pb()
            sa = list(sver)
            for h in range(4):
                for vc in range(2):
                    idx = h * 2 + vc
                    po, PKo = (po0, PKo0) if idx < 4 else (po1, PKo1)
                    oc = (idx % 4) * 128
                    K.mm(po[:, oc:oc + 128], Sbf[h][sa[h]][:, vc * 128:(vc + 1) * 128], q0[:, h, :], idx % 4 == 0, False, [("Sbf", h, sa[h]), Q0], [PKo], skip=True)
                    K.mm(po[:, oc:oc + 128], v_b[:, h * 256 + vc * 128:h * 256 + (vc + 1) * 128], a_m[:, h, :], False, False, [VB, AM], [PKo], skip=True)
                if h % 2 == 1:
                    yield
            yield from state_update(0)
            for h in range(4):
                for vc in range(2):
                    idx = h * 2 + vc
                    po, PKo = (po0, PKo0) if idx < 4 else (po1, PKo1)
                    oc = (idx % 4) * 128
                    K.mm(po[:, oc:oc + 128], Sbf[h][sver[h]][:, vc * 128:(vc + 1) * 128], q1[:, h, :], False, idx % 4 == 3, [("Sbf", h, sver[h]), Q1], [PKo], skip=True)
            yield
            yield from state_update(1)
            sq_ = sqb
            SQ = "sqb"
            K.act(sq_[:, 0:512], po0[:, :], AF.Square, [PKo0], [SQ])
            K.act(sq_[:, 512:1024], po1[:, :], AF.Square, [PKo1], [SQ])
            pss, PKss = K.pb()
            for h in range(4):
                for vc in range(2):
                    K.mm(pss[:, h * 128:(h + 1) * 128], ones_b, sq_[:, (h * 2 + vc) * 128:(h * 2 + vc + 1) * 128], vc == 0, vc == 1, [SQ] + C0, [PKss])
            K.act(rstdg, pss[:, :], AF.Sqrt, [PKss] + C0, ["rstdg"], bias=c_rmseps, scale=1.0 / 256.0)
            K.done(PKss)
            K.recip(rstdg, rstdg, ["rstdg"], ["rstdg"])
            yield
            og = oglaT[b2]
            OG = ("oglaT", b2)
            for h in range(4):
                for vc in range(2):
                    idx = h * 2 + vc
                    po, PKo = (po0, PKo0) if idx < 4 else (po1, PKo1)
                    oc = (idx % 4) * 128
                    K.stt("dve", silur[:, idx * 128:(idx + 1) * 128], po[:, oc:oc + 128], gcol[:, vc:vc + 1], silur[:, idx * 128:(idx + 1) * 128],
                          ALU.mult, ALU.mult, [PKo, SR] + C0, [SR])
                    K.tt("pool", og[:, idx, :], silur[:, idx * 128:(idx + 1) * 128], rstdg[:, h * 128:(h + 1) * 128], ALU.mult, [SR, "rstdg"], [OG])
                if h % 2 == 1:
                    yield
            K.done(PKo0, PKo1)
            K.dma("sp", oglaT_d[:, :, j * 128:(j + 1) * 128], og, [OG], ["oglaT_d"])

        def drain(g):
            for _ in g:
                pass

        def interleave(ga, gb):
            ga_done = ga is None
            gb_done = gb is None
            while not (ga_done and gb_done):
                if not ga_done:
                    try:
                        next(ga)
                    except StopIteration:
                        ga_done = True
                if not gb_done:
                    try:
                        next(gb)
                    except StopIteration:
                        gb_done = True

        if "nopipe" in flags:
            for i in range(TT):
                drain(X(i))
                drain(Y(i))
        else:
            drain(X(0))
            for i in range(TT):
                interleave(X(i + 1) if i + 1 < TT else None, Y(i))

        CKV = [("ckvnT", i) for i in range(TT)]
        KRP = [("kropeT", i) for i in range(TT)]
        CQN = [("cqnT", j) for j in range(TO)]
        if debug:
            finals.append(K.dma("sp", dbg["ckvnT"], ckvnT, CKV, []))
            finals.append(K.dma("sp", dbg["kropeT"], kropeT, KRP, []))
            finals.append(K.dma("sp", dbg["cqnT"], cqnT, CQN, []))
        if stages >= 3:
            S.barrier()
            S.mute = only is not None and 3 not in only
            A.set(OMLA_OFF)
            omlaT = v3(A.B(8 * NOWN), NOWN)
            OMLA_END = A.off
            wuqt = A.B(3 * 16 * 192).rearrange("p (c h n) -> p c h n", c=3, h=16)
            wukt = A.B(2 * 16 * 96).rearrange("p (c h n) -> p c h n", c=2, h=16)
            wuvt = v3(A.B(2 * 1024), 1024)
            KTh = [A.B(NPOS, parts=96) for i in range(2)]
            Vh = [v3(A.B(TT * 128), 128) for i in range(2)]
            QTh = [A.B(NOWN, parts=96) for i in range(2)]
            PT = [A.B(512) for i in range(6)]
            csqt = v3(A.F(2 * NOWN, parts=96), NOWN)
            qa = [A.F(512, parts=96) for i in range(2)]
            rden = A.F(512)
            print("stage3 arena end", A.off)
            K.dma("pool", wuqt.rearrange("p c h n -> p c (h n)"), wuq2.rearrange("(c p) h a n -> p c (h a n)", p=128), (), ["wuqt"])
            K.dma("pool", wukt.rearrange("p c h n -> p c (h n)"), wukp.rearrange("(c p) h n -> p c (h n)", p=128), (), ["wukt"])
            K.dma("pool", wuvt, wuv.rearrange("(c p) n -> p c n", p=128), (), ["wuvt"])
            K.dma("sp", csqt, csq, (), ["csqt"])
            for p_ in range(2):
                K.cp("act", KTh[p_][64:96, :], kropeT, KRP, [("KTh", p_)])
            if sparse:
                Gz = G_d.rearrange("(p r) n -> p (r n)", p=128)
                for k_ in range(32):
                    K.dma("sp", Gz[:, k_ * 2048:(k_ + 1) * 2048], zt[:], ["zt"], ["G_d"])
            K.memset("pool", Vh[0][:, :, 64:128], 1.0, [("Vh", 0)])
            K.memset("pool", Vh[1][:, :, 0:64], 1.0, [("Vh", 1)])
            sm_scale = 96.0 ** -0.5

            def prep_chunks(h):
                par = h % 2
                KT_, KTK = KTh[par], ("KTh", par)
                V_, VK = Vh[par], ("Vh", par)
                Q_, QK = QTh[par], ("QTh", par)
                chunks = []
                nb = (NPOS + 511) // 512

                def m1(blk):
                    c0 = blk * 512
                    n = min(512, NPOS - c0)
                    pt, PK = K.pb()
                    K.mm(pt[0:64, 0:n], wukt[:, 0, h, 0:64], ckvnT[:, 0, c0:c0 + n], True, False, ["wukt"] + CKV, [PK])
                    K.mm(pt[0:64, 0:n], wukt[:, 1, h, 0:64], ckvnT[:, 1, c0:c0 + n], False, True, ["wukt"] + CKV, [PK])
                    K.cp("dve", KT_[0:64, c0:c0 + n], pt[0:64, 0:n], [PK], [KTK])
                    K.done(PK)

                def m2(t0):
                    vo = 0 if par == 0 else 64
                    nt = min(8, TT - t0)
                    pt, PK = K.pb()
                    for t in range(nt):
                        for c in range(2):
                            K.mm(pt[:, t * 64:(t + 1) * 64], ckvnT[:, c, (t0 + t) * 128:(t0 + t + 1) * 128], wuvt[:, c, h * 64:(h + 1) * 64], c == 0, c == 1,
                                 ["wuvt"] + CKV, [PK])
                    K.cp("dve", V_[:, t0:t0 + nt, vo:vo + 64], v3(pt[:, 0:nt * 64], 64), [PK], [VK])
                    K.done(PK)

                def m3(qb):
                    c0 = qb * 512
                    pA, PKA = K.pb()
                    pB, PKB = K.pb()
                    for c in range(3):
                        K.mm(pA[0:96, :], wuqt[:, c, h, 0:96], cqnT[:, c, c0:c0 + 512], c == 0, c == 2, ["wuqt"] + CQN, [PKA])
                    for c in range(3):
                        K.mm(pB[0:96, :], wuqt[:, c, h, 96:192], cqnT[:, c, c0:c0 + 512], c == 0, c == 2, ["wuqt"] + CQN, [PKB])
                    K.tt("dve", qa[0], pA[0:96, :], csqt[:, 0, c0:c0 + 512], ALU.mult, [PKA, "csqt"], ["qa0"])
                    K.tt("dve", qa[1], pB[0:96, :], csqt[:, 1, c0:c0 + 512], ALU.mult, [PKB, "csqt"], ["qa1"])
                    K.done(PKA, PKB)
                    K.tt("pool", Q_[:, c0:c0 + 512], qa[0], qa[1], ALU.add, ["qa0", "qa1"], [QK])

                for blk in range(nb):
                    chunks.append((m1, blk))
                for t0 in range(0, TT, 8):
                    chunks.append((m2, t0))
                for qb in range(4):
                    chunks.append((m3, qb))
                return chunks

            items = []
            for h in range(16):
                for qb in range(4):
                    full = [(t, 0) for t in range(TP)] + [(TP + 4 * qb2 + d, 0) for qb2 in range(qb) for d in range(4)]
                    diag = [(TP + 4 * qb + d, d * 128) for d in range(4)]
                    ktiles = full[0:1] + diag + full[1:]
                    for n_, (t, qo) in enumerate(ktiles):
                        items.append((h, qb, t, qo, n_ == 0, n_ == len(ktiles) - 1, t >= TP + 4 * qb))
            LA = 3
            precast = []
            for e_ in range(32):
                precast += [(ewg[e_], ewg_b[e_]), (ewu[e_], ewu_b[e_]), (ewd[e_], ewd_b[e_])]
            NPT = len(PT)
            pobank = {}
            pending = []
            for f_, a_ in prep_chunks(0):
                f_(a_)
            hcur = -1
            since = 0
            for idx in range(len(items) + LA):
                if idx < len(items):
                    h, qb, t, qo, first, last, isdiag = items[idx]
                    par = h % 2
                    if h != hcur:
                        for f_, a_ in pending:
                            f_(a_)
                        pending = prep_chunks(h + 1) if h + 1 < 16 else []
                        hcur = h
                        since = 0
                    since += 1
                    if sparse and idx % 18 == 0 and precast:
                        src_, dst_ = precast.pop(0)
                        K.dma("pool", dst_, src_, (), [])
                    if pending and since % 5 == 0:
                        f_, a_ = pending.pop(0)
                        f_(a_)
                    ps_, PKs = K.pb()
                    nq = 512 - qo
                    c0 = qb * 512
                    K.mm(ps_[:, 0:nq], KTh[par][:, t * 128:(t + 1) * 128], QTh[par][:, c0 + qo:c0 + 512], True, True, [("KTh", par), ("QTh", par)], [PKs])
                    pT = PT[idx % NPT]
                    PTK = ("PT", idx % NPT)
                    K.act(pT[:, 0:nq], ps_[:, 0:nq], AF.Exp, [PKs] + C0, [PTK], bias=vkt[:, TT + t:TT + t + 1], scale=sm_scale)
                    K.done(PKs)
                    if isdiag:
                        K.tt("pool", pT[:, 0:128], pT[:, 0:128], ut_b, ALU.mult, [PTK] + C0, [PTK])
                j_ = idx - LA
                if j_ >= 0:
                    h, qb, t, qo, first, last, isdiag = items[j_]
                    par = h % 2
                    c0 = qb * 512
                    nq = 512 - qo
                    if first:
                        pobank[(h, qb)] = K.pb()
                    po, PKo = pobank[(h, qb)]
                    pT = PT[j_ % NPT]
                    PTK = ("PT", j_ % NPT)
                    K.mm(po[:, qo:512], Vh[par][:, t, :], pT[:, 0:nq], first, last, [("Vh", par), PTK], [PKo])
                    if last:
                        hp = h // 2
                        if par == 0:
                            K.recip(rden[0:64, :], po[64:128, :], [PKo], ["rden"])
                            K.tt("dve", omlaT[0:64, hp, c0:c0 + 512], po[0:64, :], rden[0:64, :], ALU.mult, [PKo, "rden"], [("omlaT", qb)])
                        else:
                            K.recip(rden[64:128, :], po[0:64, :], [PKo], ["rden"])
                            K.tt("dve", omlaT[64:128, hp, c0:c0 + 512], po[64:128, :], rden[64:128, :], ALU.mult, [PKo, "rden"], [("omlaT", qb)])
                        K.done(PKo)
                        del pobank[(h, qb)]
            if sparse:
                for src_, dst_ in precast:
                    K.dma("pool", dst_, src_, (), [])
            OMK = [("omlaT", q) for q in range(4)]
            if debug:
                finals.append(K.dma("sp", dbg["omlaT"], omlaT, OMK, []))
        def ln_gen(x_t, XTL, stt_, ST, lnbc, LK, gi):
            JK = ("junk2", ST[1] % 2)
            jk = junk2[ST[1] % 2]
            K.act(jk[:], x_t, AF.Identity, XTL, [JK, ST], accum=stt_[:, 0:1]); yield
            K.act(jk[:], x_t, AF.Square, XTL, [JK, ST], accum=stt_[:, 1:2]); yield
            K.ts("dve", stt_[:, 2:3], stt_[:, 0:1], 1.0 / D, None, ALU.mult, None, [ST], [ST]); yield
            K.tt("dve", stt_[:, 3:4], stt_[:, 2:3], stt_[:, 2:3], ALU.mult, [ST], [ST]); yield
            K.stt("dve", stt_[:, 4:5], stt_[:, 1:2], 1.0 / D, stt_[:, 3:4], ALU.mult, ALU.subtract, [ST], [ST]); yield
            K.act(stt_[:, 5:6], stt_[:, 4:5], AF.Sqrt, [ST] + C0, [ST], bias=c_lneps, scale=1.0); yield
            K.recip(stt_[:, 5:6], stt_[:, 5:6], [ST], [ST]); yield
            K.stt("dve", stt_[:, 6:7], stt_[:, 2:3], -1.0, stt_[:, 5:6], ALU.mult, ALU.mult, [ST], [ST]); yield
            K.act(x_t, x_t, AF.Identity, XTL + [ST], XTL, bias=stt_[:, 6:7], scale=stt_[:, 5:6]); yield
            K.tt("dve", x_t, x_t, lnbc[:, gi, :], ALU.mult, XTL + [LK], XTL); yield
            K.tt("dve", x_t, x_t, lnbc[:, gi + 1, :], ALU.add, XTL + [LK], XTL); yield

        if stages >= 4:
            S.barrier()
            S.mute = only is not None and 4 not in only
            assert OMLA_OFF == 37632 and OMLA_END == 70400
            A.set(0)
            oglaTs = v3(A.B(8 * 512), 512)
            sTs = v3(A.B(8 * 512), 512)
            woutt = v3(A.B(8 * 1024), 1024)
            A.set(OMLA_END)
            wgat = v3(A.B(8 * 2048), 2048)
            wbgt = v3(A.B(8 * 1024), 1024)
            wbmt = v3(A.B(8 * 1024), 1024)
            W4_END = A.off
            mergedT = v3(A.B(8 * NOWN), NOWN)
            M_END = A.off
            sga = [A.B(512) for i in range(4)]
            print("stage4a arena end", A.off)
            for c in range(8):
                K.dma("pool", wgat[:, c, :], w_in_v[:, c, W_RES:5808], (), [("wgat", c)])
            for c in range(8):
                K.dma("pool", wbgt[:, c, :], wbg.rearrange("(c p) n -> p c n", p=128)[:, c, :], (), [("wbgt", c)])
            for c in range(8):
                K.dma("pool", wbmt[:, c, :], wbm.rearrange("(c p) n -> p c n", p=128)[:, c, :], (), [("wbmt", c)])
            K.dma("pool", woutt, wout.rearrange("(c p) n -> p c n", p=128), (), ["woutt"])
            WG8 = [("wgat", c) for c in range(8)]
            WB8 = [("wbgt", c) for c in range(8)]
            WM8 = [("wbmt", c) for c in range(8)]
            nsg = 0
            for blk in range(4):
                c0 = blk * 512
                K.dma("sp", oglaTs, oglaT_d[:, :, c0:c0 + 512], ["oglaT_d"], ["oglaTs"])
                K.dma("sp", sTs, sT_d[:, :, c0:c0 + 512], ["sT_d"], ["sTs"])
                for dc in range(8):
                    sa_ = sga[nsg % 4]; SA = ("sga", nsg % 4); nsg += 1
                    sb_ = sga[nsg % 4]; SB = ("sga", nsg % 4); nsg += 1
                    pga, PKga = K.pb()
                    pgb, PKgb = K.pb()
                    for c in range(8):
                        K.mm(pga[:, :], wgat[:, c, dc * 128:(dc + 1) * 128], sTs[:, c, :], c == 0, c == 7, [("wgat", c), "sTs"], [PKga])
                    for c in range(8):
                        K.mm(pgb[:, :], wgat[:, c, 1024 + dc * 128:1024 + (dc + 1) * 128], sTs[:, c, :], c == 0, c == 7, [("wgat", c), "sTs"], [PKgb])
                    K.act(sa_, pga[:, :], AF.Sigmoid, [PKga], [SA])
                    K.act(sb_, pgb[:, :], AF.Sigmoid, [PKgb], [SB])
                    K.done(PKga, PKgb)
                    pbg, PKbg = K.pb()
                    pbm, PKbm = K.pb()
                    for c in range(8):
                        K.mm(pbg[:, :], wbgt[:, c, dc * 128:(dc + 1) * 128], oglaTs[:, c, :], c == 0, c == 7, [("wbgt", c), "oglaTs"], [PKbg])
                    for c in range(8):
                        K.mm(pbm[:, :], wbmt[:, c, dc * 128:(dc + 1) * 128], omlaT[:, c, c0:c0 + 512], c == 0, c == 7, [("wbmt", c), ("omlaT", blk)], [PKbm])
                    K.tt("dve", sa_, sa_, pbg[:, :], ALU.mult, [SA, PKbg], [SA])
                    K.tt("dve", sb_, sb_, pbm[:, :], ALU.mult, [SB, PKbm], [SB])
                    K.done(PKbg, PKbm)
                    K.tt("pool", mergedT[:, dc, c0:c0 + 512], sa_, sb_, ALU.add, [SA, SB], [("mergedT", blk)])
            MT = [("mergedT", q) for q in range(4)]
            S.barrier()
            A.set(OMLA_OFF)
            s2T = v3(A.B(8 * NOWN), NOWN)
            lnbc = v3(A.F(2 * D), D)
            s2Tf = [A.F(1024)] * 2
            xt = [A.F(D) for i in range(4)]
            rlog = v3(A.F(TO * 36), 36)
            rb_bc = A.F(36)
            ixb = A.F(3616)
            wrt = v3(A.F(8 * 36), 36)
            s2b = [None] * TO
            for j in range(8, TO):
                s2b[j] = A.B(D)
            assert A.off <= W4_END, A.off
            end4b = A.off
            A.set(0)
            for j in range(8):
                s2b[j] = A.B(D)
            A.set(M_END)
            rtb = A.F(2560)
            A.set(end4b)
            print("stage4b arena end", A.off)
            K.dma("sp", lnbc[:, 0, :], lng[1, 0].partition_broadcast(128), (), ["lnbc"])
            K.dma("sp", lnbc[:, 1, :], lng[1, 1].partition_broadcast(128), (), ["lnbc"])
            K.dma("sp", rb_bc, rbias.partition_broadcast(128), (), ["rb_bc"])
            K.dma("sp", wrt, wr.rearrange("(c p) n -> p c n", p=128), (), ["wrt"])
            def P4(j):
                blk = j // 4
                x_t = xt[j % 4]
                XT = ("xt", j % 4)
                K.dma("sp", x_t, s_d[j * 128:(j + 1) * 128, :], ["s_d"], [XT])
                for hh in range(2):
                    ph, PKh = K.pb()
                    for c in range(8):
                        K.mm(ph[:, :], mergedT[:, c, j * 128:(j + 1) * 128], woutt[:, c, hh * 512:(hh + 1) * 512], c == 0, c == 7, [("mergedT", blk), "woutt"], [PKh])
                    K.stt("dve", x_t[:, hh * 512:(hh + 1) * 512], x_t[:, hh * 512:(hh + 1) * 512], ALPHA, ph[:, :], ALU.mult, ALU.add, [XT, PKh], [XT])
                    K.done(PKh)

            def L4(j):
                return ln_gen(xt[j % 4], [("xt", j % 4)], st8[j % 4], ("st8", j % 4), lnbc, "lnbc", 0)

            def Q4(j):
                x_t = xt[j % 4]
                XT = ("xt", j % 4)
                K.dma("sp", s2_d[j * 128:(j + 1) * 128, :], x_t, [XT], ["s2_d"])
                if sparse:
                    K.cp("act", s2b[j], x_t, [XT], [("s2b", j)])
                for half in range(2):
                    pt, PK = K.pb()
                    for cc in range(4):
                        c = half * 4 + cc
                        K.tr(pt[:, cc * 128:(cc + 1) * 128], x_t[:, c * 128:(c + 1) * 128], ident, [XT] + C0, [PK])
                    sf = s2Tf[j % 2]
                    K.cp("dve", sf[:, half * 512:(half + 1) * 512], pt[:, :], [PK], [("s2Tf", 0, half)])
                    K.done(PK)
                    if not sparse:
                        K.cp("act", s2T[:, half * 4:half * 4 + 4, j * 128:(j + 1) * 128], v3(sf[:, half * 512:(half + 1) * 512], 128), [("s2Tf", 0, half)], [("s2T", j)])
                prt, PKrt = K.pb()
                for c in range(8):
                    K.mm(prt[:, 0:36], s2Tf[j % 2][:, c * 128:(c + 1) * 128], wrt[:, c, :], c == 0, c == 7, [("s2Tf", 0, 0), ("s2Tf", 0, 1), "wrt"], [PKrt])
                K.tt("dve", rlog[:, j, :], prt[:, 0:36], rb_bc, ALU.add, [PKrt, "rb_bc"], [("rlog", j)])
                K.done(PKrt)

            P4(0)
            P4(1)
            for j0 in range(0, TO, 2):
                if j0 + 2 < TO:
                    P4(j0 + 2)
                    P4(j0 + 3)
                interleave(L4(j0), L4(j0 + 1))
                Q4(j0)
                Q4(j0 + 1)
            RL = [("rlog", j) for j in range(TO)]
            R = ["rtb"]
            l4 = rlog[:, :, 0:4]
            le = rtb[:, 0:512].rearrange("p (a e) -> p a e", e=8)
            K.cp("dve", rtb[:, 0:512].rearrange("p (t n) -> p t n", n=32), rlog[:, :, 4:36], RL, R)
            m4 = rtb[:, 512:528]
            K.red(m4, l4, ALU.max, RL, R)
            d4 = rtb[:, 528:592].rearrange("p (t g) -> p t g", g=4)
            K.tt("dve", d4, l4, m4.unsqueeze(2).broadcast_to([128, 16, 4]), ALU.subtract, RL + R, R)
            e4 = rtb[:, 592:656].rearrange("p (t g) -> p t g", g=4)
            K.act(e4, d4, AF.Exp, R, R)
            s4 = rtb[:, 656:672]
            K.red(s4, e4, ALU.add, R, R)
            K.recip(s4, s4, R, R)
            gw = rtb[:, 672:736].rearrange("p (t g) -> p t g", g=4)
            K.ts("dve", gw, d4, 0.0, None, ALU.is_equal, None, R, R)
            K.tt("dve", gw, gw, s4.unsqueeze(2).broadcast_to([128, 16, 4]), ALU.mult, R, R)
            m1 = rtb[:, 736:800]
            K.red(m1, le, ALU.max, R, R)
            eq1 = rtb[:, 800:1312].rearrange("p (a e) -> p a e", e=8)
            K.tt("dve", eq1, le, m1.unsqueeze(2).broadcast_to([128, 64, 8]), ALU.is_equal, R, R)
            l2 = rtb[:, 1312:1824].rearrange("p (a e) -> p a e", e=8)
            K.stt("dve", l2, eq1, -1e30, le, ALU.mult, ALU.add, R, R)
            m2 = rtb[:, 1824:1888]
            K.red(m2, l2, ALU.max, R, R)
            eq2 = rtb[:, 1888:2400].rearrange("p (a e) -> p a e", e=8)
            K.tt("dve", eq2, l2, m2.unsqueeze(2).broadcast_to([128, 64, 8]), ALU.is_equal, R, R)
            w1 = rtb[:, 2400:2464]
            K.tt("dve", w1, m1, m2, ALU.subtract, R, R)
            K.act(w1, w1, AF.Sigmoid, R, R)
            w2 = rtb[:, 2464:2528]
            K.ts("dve", w2, w1, -1.0, 1.0, ALU.mult, ALU.add, R, R)
            K.tt("dve", eq1, eq1, w1.unsqueeze(2).broadcast_to([128, 64, 8]), ALU.mult, R, R)
            K.tt("dve", eq2, eq2, w2.unsqueeze(2).broadcast_to([128, 64, 8]), ALU.mult, R, R)
            K.tt("dve", eq1, eq1, eq2, ALU.add, R, R)
            K.tt("dve", comb[:].rearrange("p t (g e) -> p (t g) e", e=8), eq1, gw.rearrange("p t g -> p (t g)").unsqueeze(2).broadcast_to([128, 64, 8]),
                 ALU.mult, R, [("comb", j) for j in range(TO)])
            if sparse:
                CM_ = [("comb", j) for j in range(TO)]
                IX = ["ixb"]
                comb3 = comb[:].rearrange("p t e -> p (t e)")
                Mk = ixb[:, 0:512]
                K.ts("dve", Mk, comb3, 0.0, None, ALU.is_gt, None, CM_, IX)
                prk, PKrk = K.pb()
                pcn, PKcn = K.pb()
                K.mm(prk[:, :], cmt[:, 610:738], Mk, True, True, IX + C0, [PKrk])
                K.mm(pcn[:, :], cmt[:, 482:610], Mk, True, True, IX + C0, [PKcn])
                cnts = v3(ixb[:, 512:1024], 32)
                K.cp("dve", ixb[:, 512:1024], pcn[:, :], [PKcn], IX)
                K.done(PKcn)
                offs = v3(ixb[:, 1024:1536], 32)
                K.memset("dve", offs[:, 0, :], 0.0, IX)
                for j in range(1, TO):
                    K.tt("dve", offs[:, j, :], offs[:, j - 1, :], cnts[:, j - 1, :], ALU.add, IX, IX)
                slot = ixb[:, 1536:2048]
                K.tt("dve", slot, prk[:, :], ixb[:, 1024:1536], ALU.add, [PKrk] + IX, IX)
                K.done(PKrk)
                valid_ = ixb[:, 2048:2560]
                K.ts("dve", valid_, slot, float(CAP), None, ALU.is_lt, None, IX, IX)
                K.tt("dve", valid_, valid_, Mk, ALU.mult, IX, IX)
                K.tt("dve", v3(slot, 32), v3(slot, 32), cmt[:, 738:770].unsqueeze(1).broadcast_to([128, TO, 32]), ALU.add, IX + C0, IX)
                rowv = ixb[:, 2560:3072]
                K.stt("dve", rowv, slot, 1.0, valid_, ALU.add, ALU.mult, IX, IX)
                K.ts("dve", rowv, rowv, -1.0, None, ALU.add, None, IX, IX)
                idxf = ixb[:, 3072:3104]
                K.red(idxf[:, 0:16], v3(rowv, 32), ALU.max, IX, IX)
                eqh = ixb[:, 3104:3616]
                K.tt("dve", v3(eqh, 32), v3(rowv, 32), idxf[:, 0:16].unsqueeze(2).broadcast_to([128, TO, 32]), ALU.is_equal, IX, IX)
                K.tt("dve", eqh, eqh, valid_, ALU.mult, IX, IX)
                K.stt("dve", rowv, eqh, -1e9, rowv, ALU.mult, ALU.add, IX, IX)
                K.red(idxf[:, 16:32], v3(rowv, 32), ALU.max, IX, IX)
                K.tt("dve", eqh, eqh, comb3, ALU.mult, IX + CM_, IX)
                K.red(wts[:, 0:16], v3(eqh, 32), ALU.add, IX, ["wts"])
                eql = ixb[:, 3104:3616]
                K.tt("dve", v3(eql, 32), v3(rowv, 32), idxf[:, 16:32].unsqueeze(2).broadcast_to([128, TO, 32]), ALU.is_equal, IX, IX)
                K.tt("dve", eql, eql, valid_, ALU.mult, IX, IX)
                K.tt("dve", eql, eql, comb3, ALU.mult, IX + CM_, IX)
                K.red(wts[:, 16:32], v3(eql, 32), ALU.add, IX, ["wts"])
                K.ts("dve", idxf, idxf, -1.0, None, ALU.max, None, IX, IX)
                K.cp("dve", idxi[:], idxf, IX, ["idxi"])
                for j in range(TO):
                    for k_ in range(2):
                        col = k_ * 16 + j
                        S.op("pool", (lambda j=j, col=col: (lambda e: e.indirect_dma_start(
                            out=G_d, out_offset=bass.IndirectOffsetOnAxis(ap=idxi[:, col:col + 1], axis=0), in_=s2b[j], in_offset=None,
                            bounds_check=bcreg(e), oob_is_err=False)))(), [("s2b", j), "idxi"], ["G_d"], dma=True)
            CMB = [("comb", j) for j in range(TO)]
            if debug:
                finals.append(K.dma("sp", dbg["comb"], comb[:], CMB, []))
        if stages >= 5 and not sparse:
            S.barrier()
            S.mute = only is not None and 5 not in only
            A.set(0)
            ewb = []
            for i in range(2):
                ewb.append((v3(A.B(2048), 256), v3(A.B(2048), 256), v3(A.B(2048), 1024)))
            lnbc = v3(A.F(2 * D), D)
            assert A.off <= OMLA_OFF
            A.set(OMLA_END)
            yacc = v3(A.F(TO * D), D)
            hidT = [v3(A.B(2 * NOWN), NOWN) for i in range(2)]
            silb = [A.B(512) for i in range(2)]
            print("stage5 arena end", A.off)
            S2K = [("s2T", j) for j in range(TO)]
            K.dma("sp", lnbc[:, 0, :], lng[2, 0].partition_broadcast(128), (), ["lnbc"])
            K.dma("sp", lnbc[:, 1, :], lng[2, 1].partition_broadcast(128), (), ["lnbc"])
            for j in range(TO):
                K.dma("sp", yacc[:, j, :], s2_d[j * 128:(j + 1) * 128, :], ["s2_d"], [("yacc", j, 0), ("yacc", j, 1)])
                K.act(yacc[:, j, :], yacc[:, j, :], AF.Copy, [("yacc", j, 0), ("yacc", j, 1)], [("yacc", j, 0), ("yacc", j, 1)], scale=ALPHA)
            for e_ in range(32):
                g_, u_, d_ = ewb[e_ % 2]
                EW = ("ew", e_ % 2)
                K.dma("pool", g_, ewg[e_].rearrange("(c p) n -> p c n", p=128), (), [EW])
                K.dma("pool", u_, ewu[e_].rearrange("(c p) n -> p c n", p=128), (), [EW])
                K.dma("pool", d_, ewd[e_].rearrange("(c p) n -> p c n", p=128), (), [EW])
                hT = hidT[e_ % 2]
                HT = ("hidT", e_ % 2)
                for fc in range(2):
                    for blk in range(4):
                        c0 = blk * 512
                        pg, PKg = K.pb()
                        pu, PKu = K.pb()
                        for c in range(8):
                            K.mm(pg[:, :], g_[:, c, fc * 128:(fc + 1) * 128], s2T[:, c, c0:c0 + 512], c == 0, c == 7, [EW] + S2K, [PKg])
                        for c in range(8):
                            K.mm(pu[:, :], u_[:, c, fc * 128:(fc + 1) * 128], s2T[:, c, c0:c0 + 512], c == 0, c == 7, [EW] + S2K, [PKu])
                        sl = silb[(fc * 4 + blk) % 2]
                        SL = ("silb", (fc * 4 + blk) % 2)
                        K.act(sl, pg[:, :], AF.Silu, [PKg], [SL])
                        K.tt("dve", hT[:, fc, c0:c0 + 512], sl, pu[:, :], ALU.mult, [SL, PKu], [HT])
                        K.done(PKg, PKu)
                for j in range(TO):
                    for half in range(2):
                        py, PKy = K.pb()
                        for fc in range(2):
                            K.mm(py[:, :], hT[:, fc, j * 128:(j + 1) * 128], d_[:, fc, half * 512:(half + 1) * 512], fc == 0, fc == 1, [HT, EW], [PKy])
                        ya = yacc[:, j, half * 512:(half + 1) * 512]
                        YK = ("yacc", j, half)
                        K.stt("dve", ya, py[:, :], comb[:, j, e_:e_ + 1], ya, ALU.mult, ALU.add, [PKy, ("comb", j), YK], [YK])
                        K.done(PKy)
            for j0 in range(0, TO, 2):
                gens = [ln_gen(yacc[:, j, :], [("yacc", j, 0), ("yacc", j, 1)], st8[j % 4], ("st8", j % 4), lnbc, "lnbc", 0) for j in (j0, j0 + 1)]
                alive = [True, True]
                while any(alive):
                    for k_, g_ in enumerate(gens):
                        if alive[k_]:
                            try:
                                next(g_)
                            except StopIteration:
                                alive[k_] = False
                for j in (j0, j0 + 1):
                    finals.append(K.dma("sp", out[j * 128:(j + 1) * 128, :], yacc[:, j, :], [("yacc", j, 0), ("yacc", j, 1)], []))
        if stages >= 5 and sparse:
            S.barrier()
            S.mute = only is not None and 5 not in only
            A.set(0)
            NW = 4
            ewb = []
            for i in range(NW):
                ewb.append((v3(A.B(2048), 256), v3(A.B(2048), 256), v3(A.B(2048), 1024)))
            lnbc = v3(A.F(2 * D), D)
            NST = CAP // 128
            gt = [v3(A.B(NST * D), D) for i in range(2)]
            gT = [v3(A.B(8 * CAP), CAP) for i in range(2)]
            hidT = [v3(A.B(2 * CAP), CAP) for i in range(2)]
            silb = [A.B(CAP) for i in range(2)]
            ysb = [v3(A.B(NST * D), D) for i in range(2)]
            NG = 8
            gth = [A.B(D) for i in range(NG)]
            xs2 = [A.F(D) for i in range(4)]
            print("stage5 sparse arena end", A.off)
            K.dma("sp", lnbc[:, 0, :], lng[2, 0].partition_broadcast(128), (), ["lnbc"])
            K.dma("sp", lnbc[:, 1, :], lng[2, 1].partition_broadcast(128), (), ["lnbc"])
            for i in range(NG):
                K.memset("pool", gth[i], 0.0, [("gth", i)])

            def wload(e_):
                g_, u_, d_ = ewb[e_ % NW]
                EW = ("ew", e_ % NW)
                K.dma("sp", g_, ewg_b[e_].rearrange("(c p) n -> p c n", p=128), (), [EW])
                K.dma("sp", u_, ewu_b[e_].rearrange("(c p) n -> p c n", p=128), (), [EW])
                K.dma("sp", d_, ewd_b[e_].rearrange("(c p) n -> p c n", p=128), (), [EW])

            for e_ in range(NW - 1):
                wload(e_)
            ncp = [0]

            def gload(e_):
                K.dma("sp", gt[e_ % 2], G_d[e_ * CAP:(e_ + 1) * CAP, :].rearrange("(s p) n -> p s n", p=128), ["G_d"], [("gt", e_ % 2)])

            def tpose(e_):
                g_t = gt[e_ % 2]
                GTK = ("gt", e_ % 2)
                gTe = gT[e_ % 2]
                GTT = ("gT", e_ % 2)
                for s_ in range(NST):
                    pt, PK = K.pb()
                    ptb = pt[:, :].bitcast(BF16)
                    for c in range(8):
                        K.tr(ptb[:, c * 128:(c + 1) * 128], g_t[:, s_, c * 128:(c + 1) * 128], cmb[:, 0:128], [GTK] + C0, [PK])
                    K.cp("dve" if ncp[0] % 2 == 0 else "act", gTe[:, :, s_ * 128:(s_ + 1) * 128], v3(ptb, 128), [PK], [GTT])
                    ncp[0] += 1
                    K.done(PK)

            def gate_up(e_):
                g_, u_, d_ = ewb[e_ % NW]
                EW = ("ew", e_ % NW)
                gTe = gT[e_ % 2]
                GTT = ("gT", e_ % 2)
                hT = hidT[e_ % 2]
                HT = ("hidT", e_ % 2)
                for fc in range(2):
                    pg, PKg = K.pb()
                    pu, PKu = K.pb()
                    for c in range(8):
                        K.mm(pg[:, 0:CAP], g_[:, c, fc * 128:(fc + 1) * 128], gTe[:, c, :], c == 0, c == 7, [EW, GTT], [PKg])
                    for c in range(8):
                        K.mm(pu[:, 0:CAP], u_[:, c, fc * 128:(fc + 1) * 128], gTe[:, c, :], c == 0, c == 7, [EW, GTT], [PKu])
                    sl = silb[fc]
                    SL = ("silb", fc)
                    K.act(sl, pg[:, 0:CAP], AF.Silu, [PKg], [SL])
                    K.tt("dve", hT[:, fc, :], sl, pu[:, 0:CAP], ALU.mult, [SL, PKu], [HT])
                    K.done(PKg, PKu)

            def down(e_):
                g_, u_, d_ = ewb[e_ % NW]
                EW = ("ew", e_ % NW)
                hT = hidT[e_ % 2]
                HT = ("hidT", e_ % 2)
                y_s = ysb[e_ % 2]
                YS = ("ysb", e_ % 2)
                for s_ in range(NST):
                    for half in range(2):
                        py, PKy = K.pb()
                        for fc in range(2):
                            K.mm(py[:, :], hT[:, fc, s_ * 128:(s_ + 1) * 128], d_[:, fc, half * 512:(half + 1) * 512], fc == 0, fc == 1, [HT, EW], [PKy])
                        K.cp("act" if ncp[0] % 2 == 0 else "dve", y_s[:, s_, half * 512:(half + 1) * 512], py[:, :], [PKy], [YS])
                        ncp[0] += 1
                        K.done(PKy)
                K.dma("sp", Y_d[e_ * CAP:(e_ + 1) * CAP, :].rearrange("(s p) n -> p s n", p=128), y_s, [YS], ["Y_d"])

            gload(0)
            gload(1)
            tpose(0)
            for e_ in range(32):
                gate_up(e_)
                if e_ + 1 < 32:
                    tpose(e_ + 1)
                if e_ + 2 < 32:
                    gload(e_ + 2)
                if e_ + NW - 1 < 32:
                    wload(e_ + NW - 1)
                down(e_)

            def fetch(j):
                x_t = xs2[j % 4]
                XT = ("xs2", j % 4)
                K.dma("sp", x_t, s2_d[j * 128:(j + 1) * 128, :], ["s2_d"], [XT])
                for k_ in range(2):
                    col = k_ * 16 + j
                    gi_ = (j * 2 + k_) % NG
                    gb = gth[gi_]
                    GK = ("gth", gi_)
                    S.op("pool", (lambda gb=gb, col=col: (lambda e: e.indirect_dma_start(
                        out=gb, out_offset=None, in_=Y_d, in_offset=bass.IndirectOffsetOnAxis(ap=idxi[:, col:col + 1], axis=0),
                        bounds_check=bcreg(e), oob_is_err=False)))(), ["Y_d", "idxi", GK], [GK], dma=True)

            def combine(j):
                x_t = xs2[j % 4]
                XT = ("xs2", j % 4)
                K.act(x_t, x_t, AF.Copy, [XT], [XT], scale=ALPHA)
                yield
                for k_ in range(2):
                    col = k_ * 16 + j
                    gi_ = (j * 2 + k_) % NG
                    K.stt("dve", x_t, gth[gi_], wts[:, col:col + 1], x_t, ALU.mult, ALU.add, [("gth", gi_), XT, "wts"], [XT])
                    yield
                yield from ln_gen(x_t, [XT], st8[j % 4], ("st8", j % 4), lnbc, "lnbc", 0)
                finals.append(K.dma("sp", out[j * 128:(j + 1) * 128, :], x_t, [XT], []))

            for j in range(4):
                fetch(j)
            for j0 in range(0, TO, 2):
                interleave(combine(j0), combine(j0 + 1))
                for j in (j0 + 4, j0 + 5):
                    if j < TO:
                        fetch(j)
        finals = [f_ for f_ in finals if f_.idx is not None and f_.idx < len(S.ops) and S.ops[f_.idx] is f_] + [o for o in S.ops if o.is_dma and o.eng == "sp"][-NDSEM:]
        counts = S.emit(finals)
        print("op counts", counts)
    return nc, counts


def _const_mats():
    cm = np.zeros((128, 776), np.float32)
    cm[:, 0:128] = np.eye(128, dtype=np.float32)
    p = np.arange(128)
    tri = ((p[:, None] // 64 == p[None, :] // 64) & (p[:, None] <= p[None, :])).astype(np.float32)
    cm[:, 128:256] = tri
    cm[:, 256:384] = (p[:, None] <= p[None, :]).astype(np.float32)
    cm[0:64, 384] = 1.0
    cm[64:128, 385] = 1.0
    for i in range(32):
        cm[i, 386 + 64 + i] = 1.0
    cm[:, 482:610] = 1.0
    cm[:, 610:738] = (p[:, None] < p[None, :]).astype(np.float32)
    cm[:, 738:770] = (np.arange(32) * CAP)[None, :]
    cv = np.zeros((128, 16), np.float32)
    cv[:, 0] = 1.0
    cv[:, 1] = LN_EPS
    cv[:, 2] = RMS_EPS
    return cm, cv


def _core_inputs(x, meta_tokens, half):
    xin = np.zeros((NPOS, D), np.float32)
    valid = np.zeros((NPOS,), np.float32)
    pos = np.zeros((NPOS,), np.float64)
    if half == 0:
        xin[NPREV - 16:NPREV] = meta_tokens
        valid[NPREV - 16:] = 1.0
        pos[NPREV - 16:NPREV] = np.arange(16)
        xin[NPREV:] = x[0:2048]
        pos[NPREV:] = 16 + np.arange(2048)
    else:
        m0 = NPREV - 2048 - 16
        xin[m0:m0 + 16] = meta_tokens
        xin[m0 + 16:NPREV] = x[0:2048]
        valid[m0:] = 1.0
        pos[m0:m0 + 16] = np.arange(16)
        pos[m0 + 16:NPREV] = 16 + np.arange(2048)
        xin[NPREV:] = x[2048:4096]
        pos[NPREV:] = 16 + 2048 + np.arange(2048)
    vk = np.zeros((128, 2 * TT), np.float32)
    vk[:, 0:TT] = valid.reshape(TT, 128).T
    vk[:, TT:] = ((valid - 1.0) * (-NEG)).reshape(TT, 128).T
    inv_freq = (10000.0 ** (-np.arange(0, 32, 2, dtype=np.float32) / 32)).astype(np.float32)
    ang = (pos.astype(np.float32)[:, None] * inv_freq[None, :]).astype(np.float32)
    cos = np.cos(ang).T.astype(np.float32)
    sin = np.sin(ang).T.astype(np.float32)
    cskv = np.zeros((32, 2, NPOS), np.float32)
    cskv[0:16, 0] = cos
    cskv[16:32, 0] = cos
    cskv[0:16, 1] = -sin
    cskv[16:32, 1] = sin
    csq = np.zeros((96, 2, NOWN), np.float32)
    csq[0:64, 0] = 1.0
    csq[64:96, 0] = cskv[:, 0, NPREV:]
    csq[64:96, 1] = cskv[:, 1, NPREV:]
    return xin, vk, cskv, csq


def _shared_inputs(inp):
    f = lambda a: np.ascontiguousarray(np.asarray(a, dtype=np.float32))
    cm, cv = _const_mats()
    w_in = f(inp["w_in"][0])
    kr0 = 512 + 512 + 1024 + 1024 + 16 + 384 + 256
    wkr2 = np.zeros((D, 2, 32), np.float32)
    wkr2[:, 0] = w_in[:, kr0:kr0 + 32]
    wkr2[:, 1, 0:16] = w_in[:, kr0 + 16:kr0 + 32]
    wkr2[:, 1, 16:32] = w_in[:, kr0:kr0 + 16]
    w2a = np.zeros((33, 512), np.float32)
    w2a[0:16] = f(inp["gla_gate_w2"][0])
    w2a[32] = f(inp["gla_gate_b"][0])
    wuq = f(inp["mla_w_uq"][0]).reshape(384, 16, 96)
    wuq2 = np.zeros((384, 16, 2, 96), np.float32)
    wuq2[:, :, 0] = wuq
    wuq2[:, :, 1, 64:80] = wuq[:, :, 80:96]
    wuq2[:, :, 1, 80:96] = wuq[:, :, 64:80]
    wukp = np.zeros((256, 16, 96), np.float32)
    wukp[:, :, 0:64] = f(inp["mla_w_uk"][0]).reshape(256, 16, 64)
    lng = np.stack([np.stack([f(inp["ln_emb_g"]), f(inp["ln_emb_b"])]),
                    np.stack([f(inp["ln_mix_g"][0]), f(inp["ln_mix_b"][0])]),
                    np.stack([f(inp["ln_ffn_g"][0]), f(inp["ln_ffn_b"][0])])])
    gcols = np.zeros((128, 8), np.float32)
    gcols[:, 0:2] = f(inp["gla_norm_g"][0]).reshape(2, 128).T
    gcols[:, 2:5] = f(inp["mla_q_norm_g"][0]).reshape(3, 128).T
    gcols[:, 5:7] = f(inp["mla_kv_norm_g"][0]).reshape(2, 128).T
    sh = {
        "cm": cm, "cv": cv, "lng": f(lng), "w_in": w_in, "wkr2": wkr2, "w2a": w2a, "gcols": gcols,
        "wuq2": wuq2, "wukp": wukp, "wuv": f(inp["mla_w_uv"][0]),
        "wbg": f(inp["w_branch_gla"][0]), "wbm": f(inp["w_branch_mla"][0]), "wout": f(inp["w_out"][0]),
        "wr": f(np.concatenate([inp["router_group_w"][0], inp["router_expert_w"][0]], axis=1)),
        "rbias": f(np.concatenate([inp["router_group_b"][0], inp["router_expert_b"][0]], axis=0)),
        "ewg": f(np.asarray(inp["expert_w_gate"][0]).reshape(32, D, 256)),
        "ewu": f(np.asarray(inp["expert_w_up"][0]).reshape(32, D, 256)),
        "ewd": f(np.asarray(inp["expert_w_down"][0]).reshape(32, 256, D)),
    }
    return sh


_NC_CACHE = {}


def kernel(**inputs):
    inp = {k: np.asarray(v) for k, v in inputs.items()}
    x = inp["x"].astype(np.float32)
    meta = inp["meta_tokens"].astype(np.float32)
    sh = _shared_inputs(inp)
    in_maps = []
    for c in range(8):
        b, half = c // 2, c % 2
        xin, vk, cskv, csq = _core_inputs(x[b], meta, half)
        m = dict(sh)
        m.update({"xin": xin, "vk": vk, "cskv": cskv, "csq": csq})
        in_maps.append(m)
    if "nc" not in _NC_CACHE:
        _NC_CACHE["nc"] = build(False)[0]
    res = run_bass_kernel_spmd(_NC_CACHE["nc"], in_maps, core_ids=list(range(8)))
    out = np.zeros((4, 4096, D), np.float32)
    for c in range(8):
        b, half = c // 2, c % 2
        out[b, half * 2048:(half + 1) * 2048] = res.results[c]["out"]
    return out
```
